# Optimizing a Trainium2 kernel written in Bass

```python
import math
import jax, jax.numpy as jnp
from jax import lax
import numpy as np

D_MODEL = 2048
BATCH = 4
SEQ = 2048
DEPTH = 1

DA_HEADS = 8
DA_HEAD_DIM = 64
DA_V_DIM = 2 * DA_HEAD_DIM
DA_WIDTH = DA_HEADS * DA_V_DIM

MLA_HEADS = 8
MLA_Q_RANK = 512
MLA_KV_RANK = 256
MLA_NOPE = 64
MLA_ROPE = 32
MLA_V = 128
MLA_WIDTH = MLA_HEADS * MLA_V

IN_SIZES = (
    DA_HEADS * 2 * DA_HEAD_DIM,
    DA_HEADS * 2 * DA_HEAD_DIM,
    DA_HEADS * DA_V_DIM,
    MLA_Q_RANK,
    MLA_KV_RANK,
    MLA_ROPE,
    D_MODEL,
    D_MODEL,
)
IN_WIDTH = sum(IN_SIZES)

N_GROUPS = 4
EXPERTS_PER_GROUP = 8
N_EXPERTS = N_GROUPS * EXPERTS_PER_GROUP
TOP_K_IN_GROUP = 2
EXPERT_FF = 512
EXPERT_BLOCK = 128

ROPE_THETA = 10000.0
Q_BLOCK = 128
NORM_EPS = 1e-6
NEG_INF = -1e30

kernel_name = "hybrid_diffattn_mla_hiermoe_block"


def rms_norm(x, g):
    xf = x.astype(jnp.float32)
    y = xf * lax.rsqrt(jnp.mean(xf * xf, axis=-1, keepdims=True) + NORM_EPS)
    return (y * g.astype(jnp.float32)).astype(x.dtype)


def rope(x, positions):
    d = x.shape[-1]
    inv_freq = ROPE_THETA ** (-jnp.arange(0, d, 2, dtype=jnp.float32) / d)
    ang = positions.astype(jnp.float32)[..., None] * inv_freq
    cos = jnp.cos(ang)[:, :, None, :]
    sin = jnp.sin(ang)[:, :, None, :]
    x1, x2 = jnp.split(x.astype(jnp.float32), 2, axis=-1)
    return jnp.concatenate([x1 * cos - x2 * sin, x1 * sin + x2 * cos], axis=-1).astype(x.dtype)


def causal_mask(i, seq_len):
    q_idx = i * Q_BLOCK + jnp.arange(Q_BLOCK)
    k_idx = jnp.arange(seq_len)
    return k_idx[None, :] <= q_idx[:, None]


def sweep_query_blocks(block_fn, seq_len):
    out = lax.map(block_fn, jnp.arange(seq_len // Q_BLOCK))
    out = jnp.moveaxis(out, 0, 1)
    return out.reshape(out.shape[0], seq_len, *out.shape[3:])


def diff_attention(q, k, v, lam):
    seq_len = q.shape[1]
    scale = DA_HEAD_DIM ** -0.5

    def block(i):
        q_blk = lax.dynamic_slice_in_dim(q, i * Q_BLOCK, Q_BLOCK, axis=1)
        s = jnp.einsum('bqhcd,bkhcd->bhcqk', q_blk, k,
                       preferred_element_type=jnp.float32) * scale
        s = jnp.where(causal_mask(i, seq_len), s, NEG_INF)
        p = jax.nn.softmax(s, axis=-1)
        p = p[:, :, 0] - lam * p[:, :, 1]
        return jnp.einsum('bhqk,bkhe->bqhe', p.astype(v.dtype), v)

    return sweep_query_blocks(block, seq_len)


def softmax_attention(q, k, v, scale):
    seq_len = q.shape[1]

    def block(i):
        q_blk = lax.dynamic_slice_in_dim(q, i * Q_BLOCK, Q_BLOCK, axis=1)
        s = jnp.einsum('bqhd,bkhd->bhqk', q_blk, k,
                       preferred_element_type=jnp.float32) * scale
        s = jnp.where(causal_mask(i, seq_len), s, NEG_INF)
        p = jax.nn.softmax(s, axis=-1)
        return jnp.einsum('bhqk,bkhe->bqhe', p.astype(v.dtype), v)

    return sweep_query_blocks(block, seq_len)


def hierarchical_route(h, w_group, b_group, w_router, b_router):
    g_logits = jnp.dot(h, w_group, preferred_element_type=jnp.float32) + b_group.astype(jnp.float32)
    p_group = jax.nn.softmax(g_logits, axis=-1)
    g_sel = jnp.argmax(p_group, axis=-1)
    p_g = jnp.take_along_axis(p_group, g_sel[:, None], axis=-1)
    e_logits = (jnp.dot(h, w_router, preferred_element_type=jnp.float32)
                + b_router.astype(jnp.float32)).reshape(-1, N_GROUPS, EXPERTS_PER_GROUP)
    e_logits = jnp.take_along_axis(e_logits, g_sel[:, None, None], axis=1)[:, 0]
    p_e = jax.nn.softmax(e_logits, axis=-1)
    top_w, top_i = lax.top_k(p_e, TOP_K_IN_GROUP)
    top_w = top_w / jnp.sum(top_w, axis=-1, keepdims=True)
    expert_id = g_sel[:, None] * EXPERTS_PER_GROUP + top_i
    return expert_id, p_g * top_w


def routed_experts(h, expert_id, weight, w_gate, w_up, w_down):
    T, D = h.shape
    n_assign = expert_id.size
    flat_e = expert_id.reshape(-1)
    flat_tok = jnp.repeat(jnp.arange(T, dtype=jnp.int32), TOP_K_IN_GROUP)
    flat_w = weight.reshape(-1)
    order = jnp.argsort(flat_e)
    sorted_e = flat_e[order]
    counts = jnp.bincount(flat_e, length=N_EXPERTS)
    starts = jnp.cumsum(counts) - counts
    padded = (counts + EXPERT_BLOCK - 1) // EXPERT_BLOCK * EXPERT_BLOCK
    padded_ends = jnp.cumsum(padded)
    padded_starts = padded_ends - padded
    dest = padded_starts[sorted_e] + (jnp.arange(n_assign) - starts[sorted_e])
    n_rows = (-(-n_assign // EXPERT_BLOCK) + N_EXPERTS) * EXPERT_BLOCK
    row_tok = jnp.full((n_rows,), T, jnp.int32).at[dest].set(flat_tok[order])
    row_w = jnp.zeros((n_rows,), h.dtype).at[dest].set(flat_w[order].astype(h.dtype))
    block_start = jnp.arange(n_rows // EXPERT_BLOCK) * EXPERT_BLOCK
    block_expert = jnp.minimum(jnp.searchsorted(padded_ends, block_start, side='right'),
                               N_EXPERTS - 1)
    h_pad = jnp.concatenate([h, jnp.zeros((1, D), h.dtype)], axis=0)

    def expert_block(args):
        tok, e = args
        rows = h_pad[tok]
        return (jax.nn.silu(rows @ w_gate[e]) * (rows @ w_up[e])) @ w_down[e]

    y = lax.map(expert_block, (row_tok.reshape(-1, EXPERT_BLOCK), block_expert))
    y = y.reshape(n_rows, D) * row_w[:, None]
    return jnp.zeros((T + 1, D), h.dtype).at[row_tok].add(y)[:T]


def setup_inputs(seed: int = 0) -> dict:
    key = jax.random.key(seed)
    ks = jax.random.split(key, 24)

    def nrm(k, shape, fan_in):
        return jax.random.normal(k, shape, jnp.float32) * fan_in ** -0.5

    def gain(k, shape):
        return 1.0 + 0.02 * jax.random.normal(k, shape, jnp.float32)

    L = DEPTH
    x = jax.random.normal(ks[0], (BATCH, SEQ, D_MODEL), jnp.float32)
    positions = jnp.broadcast_to(jnp.arange(SEQ, dtype=jnp.int32), (BATCH, SEQ))
    return {
        "x": x,
        "positions": positions,
        "attn_norm": gain(ks[1], (L, D_MODEL)),
        "w_in": nrm(ks[2], (L, D_MODEL, IN_WIDTH), D_MODEL),
        "da_lambda": 0.1 * jax.random.normal(ks[3], (L, 4, DA_HEAD_DIM), jnp.float32),
        "da_subln": gain(ks[4], (L, DA_V_DIM)),
        "mla_q_norm": gain(ks[5], (L, MLA_Q_RANK)),
        "mla_w_uq": nrm(ks[6], (L, MLA_Q_RANK, MLA_HEADS * (MLA_NOPE + MLA_ROPE)), MLA_Q_RANK),
        "mla_kv_norm": gain(ks[7], (L, MLA_KV_RANK)),
        "mla_w_ukv": nrm(ks[8], (L, MLA_KV_RANK, MLA_HEADS * (MLA_NOPE + MLA_V)), MLA_KV_RANK),
        "w_branch_a": nrm(ks[9], (L, DA_WIDTH, D_MODEL), DA_WIDTH),
        "w_branch_b": nrm(ks[10], (L, MLA_WIDTH, D_MODEL), MLA_WIDTH),
        "w_out": nrm(ks[11], (L, D_MODEL, D_MODEL), D_MODEL),
        "ffn_norm": gain(ks[12], (L, D_MODEL)),
        "w_group": nrm(ks[13], (L, D_MODEL, N_GROUPS), D_MODEL),
        "b_group": 0.01 * jax.random.normal(ks[14], (L, N_GROUPS), jnp.float32),
        "w_router": nrm(ks[15], (L, D_MODEL, N_EXPERTS), D_MODEL),
        "b_router": 0.01 * jax.random.normal(ks[16], (L, N_EXPERTS), jnp.float32),
        "w_exp_gate": nrm(ks[17], (L, N_EXPERTS, D_MODEL, EXPERT_FF), D_MODEL),
        "w_exp_up": nrm(ks[18], (L, N_EXPERTS, D_MODEL, EXPERT_FF), D_MODEL),
        "w_exp_down": nrm(ks[19], (L, N_EXPERTS, EXPERT_FF, D_MODEL), EXPERT_FF),
        "final_norm": gain(ks[20], (D_MODEL,)),
    }


def reference(x, positions, attn_norm, w_in, da_lambda, da_subln, mla_q_norm, mla_w_uq,
              mla_kv_norm, mla_w_ukv, w_branch_a, w_branch_b, w_out, ffn_norm, w_group,
              b_group, w_router, b_router, w_exp_gate, w_exp_up, w_exp_down, final_norm):
    B, S, _ = x.shape
    split_points = np.cumsum(IN_SIZES)[:-1]
    for l in range(DEPTH):
        h = rms_norm(x, attn_norm[l])
        proj = h @ w_in[l]
        qa, ka, va, cq, ckv, krope, ga, gb = jnp.split(proj, split_points, axis=-1)

        lam_init = 0.8 - 0.6 * math.exp(-0.3 * l)
        qa = rope(qa.reshape(B, S, 2 * DA_HEADS, DA_HEAD_DIM), positions)
        ka = rope(ka.reshape(B, S, 2 * DA_HEADS, DA_HEAD_DIM), positions)
        qa = qa.reshape(B, S, DA_HEADS, 2, DA_HEAD_DIM)
        ka = ka.reshape(B, S, DA_HEADS, 2, DA_HEAD_DIM)
        va = va.reshape(B, S, DA_HEADS, DA_V_DIM)
        lq1, lk1, lq2, lk2 = da_lambda[l].astype(jnp.float32)
        lam = jnp.exp(jnp.sum(lq1 * lk1)) - jnp.exp(jnp.sum(lq2 * lk2)) + lam_init
        oa = diff_attention(qa, ka, va, lam)
        oa = rms_norm(oa, da_subln[l]) * (1.0 - lam_init)
        ya = oa.reshape(B, S, DA_WIDTH) @ w_branch_a[l]

        qb = (rms_norm(cq, mla_q_norm[l]) @ mla_w_uq[l]).reshape(B, S, MLA_HEADS, MLA_NOPE + MLA_ROPE)
        q_nope, q_rot = jnp.split(qb, [MLA_NOPE], axis=-1)
        q_rot = rope(q_rot, positions)
        kv = (rms_norm(ckv, mla_kv_norm[l]) @ mla_w_ukv[l]).reshape(B, S, MLA_HEADS, MLA_NOPE + MLA_V)
        k_nope, vb = jnp.split(kv, [MLA_NOPE], axis=-1)
        k_rot = rope(krope[:, :, None, :], positions)
        q_full = jnp.concatenate([q_nope, q_rot], axis=-1)
        k_full = jnp.concatenate([k_nope, jnp.broadcast_to(k_rot, (B, S, MLA_HEADS, MLA_ROPE))], axis=-1)
        ob = softmax_attention(q_full, k_full, vb, (MLA_NOPE + MLA_ROPE) ** -0.5)
        yb = ob.reshape(B, S, MLA_WIDTH) @ w_branch_b[l]

        mixed = jax.nn.sigmoid(ga) * ya + jax.nn.sigmoid(gb) * yb
        x = x + mixed @ w_out[l]

        hf = rms_norm(x, ffn_norm[l]).reshape(B * S, D_MODEL)
        expert_id, comb_w = hierarchical_route(hf, w_group[l], b_group[l], w_router[l], b_router[l])
        moe = routed_experts(hf, expert_id, comb_w, w_exp_gate[l], w_exp_up[l], w_exp_down[l])
        x = x + moe.reshape(B, S, D_MODEL)
    return rms_norm(x, final_norm)
```

```python
import contextlib
import numpy as np
import concourse.bass as bass
import concourse.mybir as mybir
from concourse.bass_utils import run_bass_kernel_spmd

F32 = mybir.dt.float32
BF16 = mybir.dt.bfloat16
I32 = mybir.dt.int32
AF = mybir.ActivationFunctionType
ALU = mybir.AluOpType
AX = mybir.AxisListType

EPS = 1e-6
NEXP = 32
STAGE_ALL = 99


class Sched:
    ENG = ("pe", "act", "dve", "pool", "sp")

    def __init__(self, nc):
        self.nc = nc
        self.q = {e: [] for e in self.ENG}
        self.cnt = {}
        self.seen = {e: {} for e in self.ENG}
        self.lastw = {}
        self.readers = {}
        self.sems = {}
        self.sem_names = []

    def sem(self, name):
        if name not in self.cnt:
            self.cnt[name] = 0
            self.sem_names.append(name)
        return name

    def op(self, eng, fn, reads=(), writes=(), inc=True, dsem=None):
        psr = [r for r in reads if isinstance(r, tuple) and r[0] == "ps"]
        if psr:
            reads = [r for r in reads if r not in psr]
            writes = list(writes) + psr
        deps = {}

        def add(tok):
            if tok is None:
                return
            s, v = tok
            if deps.get(s, 0) < v:
                deps[s] = v

        for r in reads:
            add(self.lastw.get(r))
        for w in writes:
            add(self.lastw.get(w))
            for t in self.readers.get(w, ()):
                add(t)
        for s, v in deps.items():
            if s == "pe" and eng == "pe":
                continue
            if self.seen[eng].get(s, 0) >= v:
                continue
            self.seen[eng][s] = v
            self.q[eng].append(("wait", s, v))
        if dsem is not None:
            self.sem(dsem)
            self.cnt[dsem] += 16
            tok = (dsem, self.cnt[dsem])
            self.q[eng].append(("dma", fn, dsem))
        else:
            self.sem(eng)
            if inc:
                self.cnt[eng] += 1
                tok = (eng, self.cnt[eng])
                self.q[eng].append(("inc", fn, eng))
            else:
                tok = (eng, self.cnt[eng] + 1)
                self.q[eng].append(("noinc", fn, None))
        for r in reads:
            self.readers.setdefault(r, []).append(tok)
        for w in writes:
            self.lastw[w] = tok
            self.readers[w] = []
        return tok

    def barrier(self):
        for e in self.ENG:
            for s in list(self.cnt.keys()):
                if s.startswith("d_pc"):
                    continue
                if s != e and self.cnt[s] > 0:
                    self.wait_tok(e, (s, self.cnt[s]))

    def wait_tok(self, eng, tok):
        s, v = tok
        if self.seen[eng].get(s, 0) >= v:
            return
        self.seen[eng][s] = v
        self.q[eng].append(("wait", s, v))

    def emit(self, eng, E):
        for item in self.q[eng]:
            if item[0] == "wait":
                E.wait_ge(self.sems[item[1]], item[2])
            elif item[0] == "dma":
                item[1](E).then_inc(self.sems[item[2]], 16)
            elif item[0] == "inc":
                item[1](E).then_inc(self.sems[item[2]], 1)
            else:
                item[1](E)


def build_nc(stage=STAGE_ALL, debug=False):
    nc = bass.Bass("TRN2", target_bir_lowering=False)
    S = Sched(nc)

    def din(name, shape, dt=F32):
        return nc.dram_tensor(name, list(shape), dt, kind="ExternalInput").ap()

    def dout(name, shape, dt=F32):
        return nc.dram_tensor(name, list(shape), dt, kind="ExternalOutput").ap()

    x_own = din("x_own", [1024, 2048])
    x_ctx = din("x_ctx", [1024, 2048])
    pos_d = din("pos", [2048], I32)
    cstf_d = din("cst_f", [128, 360])
    cstb_d = din("cst_b", [128, 768])
    wda_d = din("w_da", [8, 128, 6144])
    wmA_d = din("w_mA", [128, 4096])
    wmB_d = din("w_mB", [128, 4096])
    wmC_d = din("w_mC", [128, 4096])
    wmD_d = din("w_mD", [128, 2048])
    wuq_d = din("w_uq", [128, 3072])
    wukv_d = din("w_ukv", [128, 3072])
    subln_d = din("subln", [128])
    lam_d = din("lam", [256])
    if stage >= 4:
        wg_d = din("w_g", [16, 128, 4096])
        wba_d = din("w_ba", [16, 128, 1024])
        wbb_d = din("w_bb", [16, 128, 1024])
    if stage >= 5:
        wout_d = din("w_out", [4, 128, 8192])
    if stage >= 6:
        ffn_d = din("ffn_norm", [2048])
        wr_d = din("w_r", [128, 576])
        rb_d = din("r_bias", [36])
    if stage >= 7:
        weg_d = din("w_eg", [NEXP, 128, 8192])
        weu_d = din("w_eu", [NEXP, 128, 8192])
        wed_d = din("w_ed", [NEXP, 128, 8192])
        fin_d = din("final_norm", [2048])
    out_d = dout("out", [1024, 2048])
    def ch(ap2, b):
        return ap2.rearrange("p (a b) -> p a b", b=b)

    def fl(t3):
        return t3[:].rearrange("p c n -> p (c n)")

    PC = {"i": 0, "list": []}
    if stage >= 7:
        hf_s = nc.dram_tensor("hf_s", [1025, 2048], BF16, kind="Internal").ap()
        y_s = nc.dram_tensor("y_s", [4097, 2048], BF16, kind="Internal").ap()
        weg_s = nc.dram_tensor("weg_s", [NEXP, 128, 8192], BF16, kind="Internal").ap()
        weu_s = nc.dram_tensor("weu_s", [NEXP, 128, 8192], BF16, kind="Internal").ap()
        wed_s = nc.dram_tensor("wed_s", [NEXP, 128, 8192], BF16, kind="Internal").ap()
        for e in range(NEXP):
            if e % 6 == 5:
                continue
            PC["list"].append((e, "g", ch(weg_d[e], 2048), ch(weg_s[e], 2048)))
            PC["list"].append((e, "u", ch(weu_d[e], 2048), ch(weu_s[e], 2048)))
            PC["list"].append((e, "d", ch(wed_d[e], 2048), ch(wed_s[e], 2048)))

    def precast(n, after=()):
        for _ in range(n):
            if PC["i"] >= len(PC["list"]):
                return
            e, kind, src_ap, dst_ap = PC["list"][PC["i"]]
            PC["i"] += 1
            S.op("pool", lambda E, src_ap=src_ap, dst_ap=dst_ap: E.dma_start(out=dst_ap, in_=src_ap),
                 reads=list(after), writes=[("pc", e, kind)], dsem=f"d_pc{e}")
    dbg = {}
    if debug:
        dbg["oaT"] = dout("dbg_oaT", [128, 8 * 1024], BF16)
        dbg["obT"] = dout("dbg_obT", [128, 8 * 1024], BF16)
        dbg["xmid"] = dout("dbg_xmid", [128, 8 * 2048])

    es = contextlib.ExitStack()
    with es:
        def sb(name, shape, dt):
            return es.enter_context(nc.sbuf_tensor(name, list(shape), dt))

        ps = [es.enter_context(nc.psum_tensor(f"ps{i}", [128, 512], F32)) for i in range(8)]

        def psb(i):
            return ps[i][:].bitcast(BF16)

        cstf = sb("cstf", [128, 360], F32)
        cstb = sb("cstb", [128, 768], BF16)
        iota_f = cstf[:, 0:128]
        invf_da = cstf[:, 128:129]
        invf_m = cstf[:, 129:130]
        vis = cstf[:, 352:360]
        gattn = cstf[:, 132:148]
        gq = cstf[:, 148:152]
        gkv = cstf[:, 152:154]
        ident_f = cstf[:, 160:288]
        ebase = cstf[:, 288:320]
        Rc = cstf[:, 320:352]
        ident_b = cstb[:, 0:128]
        R_da = cstb[:, 128:256]
        R_m = cstb[:, 256:384]
        cmask = cstb[:, 384:512]
        ustrict = cstb[:, 512:640]
        ones_b = cstb[:, 640:768]

        hTc = sb("hTc", [128, 16, 1024], BF16)
        hTo = sb("hTo", [128, 16, 1024], BF16)
        bufO = sb("bufO", [128, 16384], BF16)
        oaT = bufO[:, 0:8192].rearrange("p (h t) -> p h t", t=1024)
        obT = bufO[:, 8192:16384].rearrange("p (h t) -> p h t", t=1024)
        xmA = bufO[:].bitcast(F32).rearrange("p (a b) -> p a b", b=2048)
        xmB = hTo[:].rearrange("p a b -> p (a b)").bitcast(F32).rearrange("p (a b) -> p a b", b=2048)
        hfb = hTc[:].rearrange("p a b -> p (a b)").rearrange("p (t f) -> p t f", f=2048)

        def hTx(c, t0, n):
            if t0 < 1024:
                return hTc[:, c, t0:t0 + n]
            return hTo[:, c, t0 - 1024:t0 - 1024 + n]

        def xm(t):
            return (xmA if t < 4 else xmB)[:, t % 4, :]
        small = sb("small", [128, 64], F32)
        lamv = small[:, 0:1]
        neglam = small[:, 1:2]

        S.op("sp", lambda E: E.dma_start(out=cstf[:], in_=cstf_d), writes=["cstf"], dsem="d_cf")
        S.op("pool", lambda E: E.dma_start(out=cstb[:], in_=cstb_d), writes=["cstb"], dsem="d_cb")

        HT_ALL = [("hT", t) for t in range(16)]
        HT_OWN = [("hT", t) for t in range(8, 16)]
        AB = {}
        state = {"sbank": 0, "pt": 0, "rope": 0, "onb": 0}
        NPT = 4

        def alloc_attn_bufs(es_, sfx):
            def a(name, shape, dt):
                return es_.enter_context(nc.sbuf_tensor(name + sfx, list(shape), dt))
            AB["qT"] = a("qT", [128, 1024], BF16)
            AB["kT"] = a("kT", [128, 2048], BF16)
            AB["Vt"] = a("Vt", [128, 16, 130], BF16)
            AB["xq"] = [a(f"xq{i}", [128, 512], BF16) for i in range(2)]
            AB["t1"] = [a(f"t1_{i}", [128, 512], F32) for i in range(2)]
            AB["t2"] = [a(f"t2_{i}", [128, 512], F32) for i in range(2)]
            AB["pt"] = [a(f"pt{i}", [128, 512], BF16) for i in range(NPT)]
            AB["onb"] = [a(f"onb{i}", [128, 128], BF16) for i in range(2)]
            Vt_ = AB["Vt"]
            S.op("dve", lambda E: E.memset(Vt_[:, :, 128:130], 1.0), writes=["Vones"])

        def rope_evac(psrc_bank, nrows, dst_ap, tok0, ctab, stab, Rm, dst_res, tabres):
            i = state["rope"] % 2
            state["rope"] += 1
            xq, t1, t2 = AB["xq"][i], AB["t1"][i], AB["t2"][i]
            S.op("act", lambda E: E.activation(out=xq[0:nrows, :], in_=ps[psrc_bank][0:nrows, :], func=AF.Copy),
                 reads=[("ps", psrc_bank)], writes=[("xq", i)])
            S.op("dve", lambda E: E.tensor_tensor(out=t1[0:nrows, :], in0=ps[psrc_bank][0:nrows, :],
                                                  in1=ctab[0:nrows, tok0:tok0 + 512], op=ALU.mult),
                 reads=[("ps", psrc_bank), tabres], writes=[("t1", i)])

            def part_b():
                S.op("pe", lambda E: E.matmul(ps[3][0:nrows, :], lhsT=Rm[0:nrows, 0:nrows], rhs=xq[0:nrows, :], start=True, stop=True),
                     reads=[("xq", i), "cstb"], writes=[("ps", 3)])
                S.op("dve", lambda E: E.tensor_tensor(out=t2[0:nrows, :], in0=ps[3][0:nrows, :],
                                                      in1=stab[0:nrows, tok0:tok0 + 512], op=ALU.mult),
                     reads=[("ps", 3), tabres], writes=[("t2", i)])
                S.op("dve", lambda E: E.tensor_tensor(out=dst_ap, in0=t1[0:nrows, :], in1=t2[0:nrows, :], op=ALU.add),
                     reads=[("t1", i), ("t2", i)], writes=[dst_res])
            return part_b

        class Pipe:
            def __init__(self):
                self.pend = None

            def push(self, fn):
                if self.pend is not None:
                    self.pend()
                self.pend = fn

            def flush(self):
                if self.pend is not None:
                    self.pend()
                self.pend = None

        pipe = Pipe()

        def attn_unit(K, p0, scale, g, on_done):
            qT, kT, Vt, pt = AB["qT"], AB["kT"], AB["Vt"], AB["pt"]
            qt0 = 4 * g
            ktiles = list(range(4 * g + 4)) + [8 + j for j in range(4 * g + 4)]
            DEPTH = 2
            info = {}

            def emit_s(kt):
                visk = None
                if kt < 8:
                    first_q, diag, bias = max(qt0, kt), False, 0.0
                    if kt >= qt0:
                        visk = vis[:, kt:kt + 1]
                else:
                    j = kt - 8
                    first_q, diag, bias = max(qt0, j), (j >= qt0), 0.0
                ncol = (qt0 + 4 - first_q) * 128
                sbk = state["sbank"] % 3
                state["sbank"] += 1
                S.op("pe", lambda E: E.matmul(
                    ps[sbk][:, 0:ncol], lhsT=kT[p0:p0 + K, kt * 128:(kt + 1) * 128],
                    rhs=qT[p0:p0 + K, first_q * 128:first_q * 128 + ncol], start=True, stop=True),
                    reads=["kT", "qT"], writes=[("ps", sbk)])
                sl = state["pt"] % NPT
                state["pt"] += 1
                S.op("act", lambda E: E.activation(
                    out=pt[sl][:, 0:ncol], in_=ps[sbk][:, 0:ncol], func=AF.Exp, bias=bias, scale=float(scale)),
                    reads=[("ps", sbk), "cstf"], writes=[("pt", sl)])
                if diag:
                    S.op("dve", lambda E: E.tensor_tensor(out=pt[sl][:, 0:128], in0=pt[sl][:, 0:128], in1=cmask, op=ALU.mult),
                         reads=[("pt", sl), "cstb"], writes=[("pt", sl)])
                if visk is not None:
                    S.op("dve", lambda E: E.tensor_scalar(out=pt[sl][:, 0:128], in0=pt[sl][:, 0:128], scalar1=visk, scalar2=None, op0=ALU.mult),
                         reads=[("pt", sl), "cstf"], writes=[("pt", sl)])
                info[kt] = (first_q, sl)

            def emit_pv(kt):
                first_q, sl = info[kt]
                for i in range(first_q, qt0 + 4):
                    ob = 4 + (i - qt0)
                    start = (kt == 0)
                    stop = (kt == 8 + i)
                    last = (i == qt0 + 3)
                    S.op("pe", lambda E, ob=ob, i=i, start=start, stop=stop: E.matmul(
                        ps[ob][:, 0:129], lhsT=pt[sl][:, (i - first_q) * 128:(i - first_q + 1) * 128],
                        rhs=Vt[:, kt, 0:129], start=start, stop=stop),
                        reads=[("pt", sl), "Vt", "Vones"], writes=[("ps", ob)], inc=(stop or last))

            n = len(ktiles)
            for idx in range(n + DEPTH):
                if idx < n:
                    emit_s(ktiles[idx])
                if idx - DEPTH >= 0:
                    emit_pv(ktiles[idx - DEPTH])
            for i in range(4):
                on_done(qt0 + i, 4 + i)

        def v_evac(bank, tq):
            Vt = AB["Vt"]
            S.op("act", lambda E: E.activation(
                out=Vt[:, tq * 4:(tq + 1) * 4, 0:128], in_=ps[bank][:, :].rearrange("p (a b) -> p a b", b=128), func=AF.Copy),
                reads=[("ps", bank)], writes=["Vt"])

        with contextlib.ExitStack() as es1:
            def sb1(name, shape, dt):
                return es1.enter_context(nc.sbuf_tensor(name, list(shape), dt))

            cos_m = sb1("cos_m", [128, 2048], BF16)
            sin_m = sb1("sin_m", [128, 2048], BF16)
            subw = sb1("subw", [128, 128], F32)
            S.op("sp", lambda E: E.dma_start(out=subw[:], in_=subln_d.partition_broadcast(128)),
                 writes=["subw"], dsem="d_sub")
            S.op("dve", lambda E: E.tensor_scalar(out=subw[:], in0=subw[:], scalar1=0.8, scalar2=None, op0=ALU.mult),
                 reads=["subw"], writes=["subw"])

            with contextlib.ExitStack() as esda:
                cos_da = esda.enter_context(nc.sbuf_tensor("cos_da", [128, 2048], BF16))
                sin_da = esda.enter_context(nc.sbuf_tensor("sin_da", [128, 2048], BF16))
                lamt = esda.enter_context(nc.sbuf_tensor("lamt", [128, 256], F32))
                S.op("sp", lambda E: E.dma_start(out=lamt[:], in_=lam_d.partition_broadcast(128)),
                     writes=["lamt"], dsem="d_lam")
                S.op("dve", lambda E: E.tensor_tensor(out=lamt[:, 0:64], in0=lamt[:, 0:64], in1=lamt[:, 64:128], op=ALU.mult),
                     reads=["lamt"], writes=["lamt"])
                S.op("dve", lambda E: E.tensor_tensor(out=lamt[:, 128:192], in0=lamt[:, 128:192], in1=lamt[:, 192:256], op=ALU.mult),
                     reads=["lamt"], writes=["lamt"])
                S.op("dve", lambda E: E.tensor_reduce(out=small[:, 2:3], in_=lamt[:, 0:64], axis=AX.X, op=ALU.add),
                     reads=["lamt"], writes=["sm2"])
                S.op("dve", lambda E: E.tensor_reduce(out=small[:, 3:4], in_=lamt[:, 128:192], axis=AX.X, op=ALU.add),
                     reads=["lamt"], writes=["sm3"])
                S.op("act", lambda E: E.activation(out=small[:, 4:6], in_=small[:, 2:4], func=AF.Exp),
                     reads=["sm2", "sm3"], writes=["sm4"])
                S.op("dve", lambda E: E.tensor_tensor(out=small[:, 6:7], in0=small[:, 4:5], in1=small[:, 5:6], op=ALU.subtract),
                     reads=["sm4"], writes=["sm6"])
                S.op("dve", lambda E: E.tensor_scalar(out=lamv, in0=small[:, 6:7], scalar1=0.2, scalar2=None, op0=ALU.add),
                     reads=["sm6"], writes=["lamv"])
                S.op("dve", lambda E: E.tensor_scalar(out=neglam, in0=lamv, scalar1=-1.0, scalar2=None, op0=ALU.mult),
                     reads=["lamv"], writes=["neglam"])

                with contextlib.ExitStack() as es0:
                    posi = es0.enter_context(nc.sbuf_tensor("posi", [128, 2048], I32))
                    posf = es0.enter_context(nc.sbuf_tensor("posf", [128, 2048], F32))
                    ang = es0.enter_context(nc.sbuf_tensor("ang", [128, 2048], F32))
                    kk = es0.enter_context(nc.sbuf_tensor("kk", [128, 2048], F32))
                    ki = es0.enter_context(nc.sbuf_tensor("ki", [128, 2048], I32))
                    S.op("sp", lambda E: E.dma_start(out=posi[:], in_=pos_d.partition_broadcast(128)),
                         writes=["posi"], dsem="d_pos")
                    S.op("dve", lambda E: E.tensor_copy(out=posf[:], in_=posi[:]), reads=["posi"], writes=["posf"])
                    TWO_PI = 2.0 * np.pi
                    for (invf, ctab, stab, nm) in ((invf_da, cos_da, sin_da, "da"), (invf_m, cos_m, sin_m, "m")):
                        for (shift, tab) in ((0.0, stab), (np.pi / 2.0, ctab)):
                            S.op("dve", lambda E, invf=invf, shift=shift: E.tensor_scalar(
                                out=ang[:], in0=posf[:], scalar1=invf, scalar2=float(shift), op0=ALU.mult, op1=ALU.add),
                                reads=["posf", "cstf"], writes=["ang"])
                            S.op("dve", lambda E: E.tensor_scalar(
                                out=kk[:], in0=ang[:], scalar1=float(1.0 / TWO_PI), scalar2=None, op0=ALU.mult),
                                reads=["ang"], writes=["kk"])
                            S.op("dve", lambda E: E.tensor_copy(out=ki[:], in_=kk[:]), reads=["kk"], writes=["ki"])
                            S.op("dve", lambda E: E.tensor_copy(out=kk[:], in_=ki[:]), reads=["ki"], writes=["kk"])
                            S.op("dve", lambda E: E.scalar_tensor_tensor(
                                out=ang[:], in0=kk[:], scalar=float(-TWO_PI), in1=ang[:], op0=ALU.mult, op1=ALU.add),
                                reads=["kk", "ang"], writes=["ang"])
                            S.op("dve", lambda E: E.tensor_scalar(
                                out=kk[:], in0=ang[:], scalar1=float(np.pi), scalar2=float(TWO_PI), op0=ALU.is_gt, op1=ALU.mult),
                                reads=["ang"], writes=["kk"])
                            S.op("dve", lambda E: E.tensor_tensor(out=ang[:], in0=ang[:], in1=kk[:], op=ALU.subtract),
                                 reads=["ang", "kk"], writes=["ang"])
                            S.op("dve", lambda E: E.tensor_scalar(
                                out=kk[:], in0=ang[:], scalar1=float(-np.pi), scalar2=float(TWO_PI), op0=ALU.is_lt, op1=ALU.mult),
                                reads=["ang"], writes=["kk"])
                            S.op("dve", lambda E: E.tensor_tensor(out=ang[:], in0=ang[:], in1=kk[:], op=ALU.add),
                                 reads=["ang", "kk"], writes=["ang"])
                            S.op("dve", lambda E: E.tensor_scalar(
                                out=ang[:], in0=ang[:], scalar1=3.141592, scalar2=-3.141592, op0=ALU.min, op1=ALU.max),
                                reads=["ang"], writes=["ang"])
                            S.op("act", lambda E, tab=tab: E.activation(out=tab[:], in_=ang[:], func=AF.Sin),
                                 reads=["ang"], writes=["tab_" + nm])

                    xst = [es0.enter_context(nc.sbuf_tensor(f"xst{i}", [128, 2048], F32)) for i in range(2)]
                    xsb = [es0.enter_context(nc.sbuf_tensor(f"xsb{i}", [128, 2048], BF16)) for i in range(2)]
                    for t in range(16):
                        b = t % 2
                        src = x_ctx if t < 8 else x_own
                        r0 = (t % 8) * 128
                        S.op("sp", lambda E, b=b, src=src, r0=r0: E.dma_start(out=xst[b][:], in_=src[r0:r0 + 128, :]),
                             writes=[("xst", b)], dsem=f"d_x{b}")
                        ss = small[:, 8 + b:9 + b]
                        S.op("act", lambda E, b=b, ss=ss: E.activation(out=xsb[b][:], in_=xst[b][:], func=AF.Square, accum_out=ss),
                             reads=[("xst", b)], writes=[("xsb", b), ("ss", b)])
                        S.op("act", lambda E, ss=ss: E.activation(out=ss, in_=ss, func=AF.Ln, bias=EPS, scale=1.0 / 2048.0),
                             reads=[("ss", b)], writes=[("ss", b)])
                        S.op("act", lambda E, ss=ss: E.activation(out=ss, in_=ss, func=AF.Exp, scale=-0.5),
                             reads=[("ss", b)], writes=[("ss", b)])
                        S.op("dve", lambda E, b=b, ss=ss: E.tensor_scalar(out=xsb[b][:], in0=xst[b][:], scalar1=ss, scalar2=None, op0=ALU.mult),
                             reads=[("xst", b), ("ss", b)], writes=[("xsb", b)])
                        for hb in range(2):
                            bank = 2 * b + hb
                            for j in range(8):
                                c = hb * 8 + j
                                S.op("pe", lambda E, bank=bank, j=j, c=c, b=b: E.transpose(
                                    out=psb(bank)[:, j * 128:(j + 1) * 128], in_=xsb[b][:, c * 128:(c + 1) * 128], identity=ident_b),
                                    reads=[("xsb", b), "cstb"], writes=[("ps", bank)], inc=(j == 7))
                            S.op("dve", lambda E, bank=bank, hb=hb, t=t: E.tensor_tensor(
                                out=(hTc if t < 8 else hTo)[:, hb * 8:(hb + 1) * 8, (t % 8) * 128:(t % 8 + 1) * 128],
                                in0=psb(bank).rearrange("p (a b) -> p a b", b=128),
                                in1=gattn[:, hb * 8:(hb + 1) * 8].unsqueeze(2).broadcast_to([128, 8, 128]),
                                op=ALU.mult),
                                reads=[("ps", bank), "cstf"], writes=[("hT", t)])
                S.barrier()

                with contextlib.ExitStack() as es2:
                    alloc_attn_bufs(es2, "_a")
                    qT, kT = AB["qT"], AB["kT"]
                    acc = es2.enter_context(nc.sbuf_tensor("acc", [128, 4, 128], F32))
                    ocomb = es2.enter_context(nc.sbuf_tensor("ocomb", [128, 128], F32))
                    osq = es2.enter_context(nc.sbuf_tensor("osq", [128, 128], F32))
                    wda = [es2.enter_context(nc.sbuf_tensor(f"wda{i}", [128, 16, 384], BF16)) for i in range(2)]
                    onb = AB["onb"]
                    for h in range(8 if stage >= 2 else 0):
                        wb = h % 2
                        S.op("pool", lambda E, wb=wb, h=h: E.dma_start(out=ch(fl(wda[wb]), 2048), in_=ch(wda_d[h], 2048)),
                             writes=[("wda", wb)], dsem=f"d_wda{wb}")
                        precast(6)
                        for tg in range(2):
                            bank = tg % 2
                            for c in range(16):
                                S.op("pe", lambda E, bank=bank, c=c, wb=wb, tg=tg: E.matmul(
                                    ps[bank][:, :], lhsT=wda[wb][:, c, 0:128], rhs=hTo[:, c, tg * 512:(tg + 1) * 512],
                                    start=(c == 0), stop=(c == 15)),
                                    reads=[("wda", wb)] + HT_OWN, writes=[("ps", bank)], inc=(c == 15))
                            pipe.push(rope_evac(bank, 128, qT[:, tg * 512:(tg + 1) * 512], 1024 + tg * 512, cos_da, sin_da, R_da, "qT", "tab_da"))
                        for tg in range(4):
                            bank = tg % 2
                            for c in range(16):
                                S.op("pe", lambda E, bank=bank, c=c, wb=wb, tg=tg: E.matmul(
                                    ps[bank][:, :], lhsT=wda[wb][:, c, 128:256], rhs=hTx(c, tg * 512, 512),
                                    start=(c == 0), stop=(c == 15)),
                                    reads=[("wda", wb)] + HT_ALL, writes=[("ps", bank)], inc=(c == 15))
                            pipe.push(rope_evac(bank, 128, kT[:, tg * 512:(tg + 1) * 512], tg * 512, cos_da, sin_da, R_da, "kT", "tab_da"))
                        pipe.flush()
                        for tq in range(4):
                            bank = tq % 2
                            for ti in range(4):
                                tt = tq * 4 + ti
                                for c in range(16):
                                    S.op("pe", lambda E, bank=bank, ti=ti, tt=tt, c=c, wb=wb: E.matmul(
                                        ps[bank][:, ti * 128:(ti + 1) * 128], lhsT=hTx(c, tt * 128, 128),
                                        rhs=wda[wb][:, c, 256:384], start=(c == 0), stop=(c == 15)),
                                        reads=[("wda", wb)] + HT_ALL, writes=[("ps", bank)], inc=(c == 15))
                            v_evac(bank, tq)
                        for g in range(2):
                            def done0(qtile, ob):
                                i = qtile % 4
                                sm = small[:, 16 + i:17 + i]
                                S.op("dve", lambda E: E.reciprocal(out=sm, in_=ps[ob][:, 128:129]), reads=[("ps", ob)], writes=[("r1", i)])
                                S.op("dve", lambda E: E.tensor_scalar(out=acc[:, i, :], in0=ps[ob][:, 0:128], scalar1=sm, scalar2=None, op0=ALU.mult),
                                     reads=[("ps", ob), ("r1", i)], writes=[("acc", i)])

                            def done1(qtile, ob, h=h):
                                i = qtile % 4
                                sm = small[:, 20 + i:21 + i]
                                sm2 = small[:, 24 + i:25 + i]
                                S.op("dve", lambda E: E.reciprocal(out=sm, in_=ps[ob][:, 128:129]), reads=[("ps", ob)], writes=[("r2", i)])
                                S.op("dve", lambda E: E.tensor_tensor(out=sm, in0=sm, in1=neglam, op=ALU.mult),
                                     reads=[("r2", i), "neglam"], writes=[("r2", i)])
                                S.op("dve", lambda E: E.scalar_tensor_tensor(out=ocomb[:], in0=ps[ob][:, 0:128], scalar=sm, in1=acc[:, i, :],
                                                                             op0=ALU.mult, op1=ALU.add),
                                     reads=[("ps", ob), ("r2", i), ("acc", i)], writes=["ocomb"])
                                S.op("dve", lambda E: E.tensor_tensor(out=osq[:], in0=ocomb[:], in1=ocomb[:], op=ALU.mult),
                                     reads=["ocomb"], writes=["osq"])
                                S.op("dve", lambda E: E.tensor_reduce(out=sm2, in_=osq[:], axis=AX.X, op=ALU.add),
                                     reads=["osq"], writes=[("ssq", i)])
                                S.op("act", lambda E: E.activation(out=sm2, in_=sm2, func=AF.Ln, bias=EPS, scale=1.0 / 128.0),
                                     reads=[("ssq", i)], writes=[("ssq", i)])
                                S.op("act", lambda E: E.activation(out=sm2, in_=sm2, func=AF.Exp, scale=-0.5),
                                     reads=[("ssq", i)], writes=[("ssq", i)])
                                ob_i = state["onb"] % 2
                                state["onb"] += 1
                                S.op("dve", lambda E: E.scalar_tensor_tensor(out=onb[ob_i][:], in0=ocomb[:], scalar=sm2, in1=subw[:],
                                                                             op0=ALU.mult, op1=ALU.mult),
                                     reads=["ocomb", ("ssq", i), "subw"], writes=[("onb", ob_i)])
                                S.op("pe", lambda E: E.transpose(out=psb(3)[:, 0:128], in_=onb[ob_i][:], identity=ident_b),
                                     reads=[("onb", ob_i), "cstb"], writes=[("ps", 3)])
                                S.op("act", lambda E: E.activation(out=oaT[:, h, qtile * 128:(qtile + 1) * 128], in_=psb(3)[:, 0:128], func=AF.Copy),
                                     reads=[("ps", 3)], writes=[("oaT", h)])

                            attn_unit(64, 0, 0.125, g, done0)
                            attn_unit(64, 64, 0.125, g, done1)
                S.barrier()
            S.barrier()

            if stage >= 3:
                with contextlib.ExitStack() as es3:
                    def sb3(name, shape, dt):
                        return es3.enter_context(nc.sbuf_tensor(name, list(shape), dt))
                    alloc_attn_bufs(es3, "_b")
                    qT, kT = AB["qT"], AB["kT"]
                    onb = AB["onb"]
                    cqn = sb3("cqn", [128, 4, 1024], BF16)
                    ckvn = sb3("ckvn", [128, 2, 2048], BF16)
                    krT = sb3("krT", [128, 2048], BF16)
                    wuq = sb3("wuq", [128, 4, 768], BF16)
                    wukv = sb3("wukv", [128, 2, 1536], BF16)
                    cf = sb3("cf", [128, 4, 512], BF16)
                    csq = sb3("csq", [128, 4, 512], BF16)
                    rbc = sb3("rbc", [128, 512], F32)
                    S.op("pool", lambda E: E.dma_start(out=ch(fl(wuq), 1024), in_=ch(wuq_d, 1024)),
                         writes=["wuq"], dsem="d_wuq")
                    S.op("pool", lambda E: E.dma_start(out=ch(fl(wukv), 1024), in_=ch(wukv_d, 1024)),
                         writes=["wukv"], dsem="d_wukv")
                    with contextlib.ExitStack() as es3b:
                        wmA = es3b.enter_context(nc.sbuf_tensor("wmA", [128, 16, 256], BF16))
                        wmB = es3b.enter_context(nc.sbuf_tensor("wmB", [128, 16, 256], BF16))

                        def latent_norm(nchunk, wts, tok0, gvec, dst, dst_res, width):
                            for k in range(nchunk):
                                wt, off, wres = wts[k]
                                bank = k % 2
                                for c in range(16):
                                    S.op("pe", lambda E, wt=wt, off=off, c=c, bank=bank: E.matmul(
                                        ps[bank][:, :], lhsT=wt[:, c, off:off + 128], rhs=hTx(c, tok0, 512),
                                        start=(c == 0), stop=(c == 15)),
                                        reads=[wres] + HT_ALL, writes=[("ps", bank)], inc=(c == 15))
                                S.op("act", lambda E, k=k, bank=bank: E.activation(out=cf[:, k, :], in_=ps[bank][:, :], func=AF.Copy),
                                     reads=[("ps", bank)], writes=[("cf", k)])
                                S.op("act", lambda E, k=k, bank=bank: E.activation(out=csq[:, k, :], in_=ps[bank][:, :], func=AF.Square),
                                     reads=[("ps", bank)], writes=[("csq", k)])
                            for k in range(nchunk):
                                S.op("pe", lambda E, k=k: E.matmul(ps[2][:, :], lhsT=ones_b, rhs=csq[:, k, :], start=(k == 0), stop=(k == nchunk - 1)),
                                     reads=[("csq", k), "cstb"], writes=[("ps", 2)], inc=(k == nchunk - 1))
                            S.op("act", lambda E: E.activation(out=rbc[:], in_=ps[2][:, :], func=AF.Ln, bias=EPS, scale=1.0 / width),
                                 reads=[("ps", 2)], writes=["rbc"])
                            S.op("act", lambda E: E.activation(out=rbc[:], in_=rbc[:], func=AF.Exp, scale=-0.5),
                                 reads=["rbc"], writes=["rbc"])
                            for k in range(nchunk):
                                S.op("dve", lambda E, k=k: E.scalar_tensor_tensor(
                                    out=dst(k), in0=cf[:, k, :], scalar=gvec[:, k:k + 1], in1=rbc[:], op0=ALU.mult, op1=ALU.mult),
                                    reads=[("cf", k), "rbc", "cstf"], writes=[dst_res])

                        S.op("pool", lambda E: E.dma_start(out=ch(fl(wmA), 2048), in_=ch(wmA_d, 2048)),
                             writes=["wmA"], dsem="d_wmA")
                        S.op("pool", lambda E: E.dma_start(out=ch(fl(wmB), 2048), in_=ch(wmB_d, 2048)),
                             writes=["wmB"], dsem="d_wmB")
                        precast(3)
                        cq_w = [(wmA, 0, "wmA"), (wmA, 128, "wmA"), (wmB, 0, "wmB"), (wmB, 128, "wmB")]
                        for tg in range(2):
                            latent_norm(4, cq_w, 1024 + tg * 512, gq, lambda k, tg=tg: cqn[:, k, tg * 512:(tg + 1) * 512], "cqn", 512.0)
                        S.op("pool", lambda E: E.dma_start(out=ch(fl(wmA), 2048), in_=ch(wmC_d, 2048)),
                             writes=["wmA"], dsem="d_wmA")
                        S.op("pool", lambda E: E.dma_start(out=wmB[:, :, 0:128], in_=wmD_d.rearrange("p (c n) -> p c n", n=128)),
                             writes=["wmB"], dsem="d_wmB")
                        ckv_w = [(wmA, 0, "wmA"), (wmA, 128, "wmA")]
                        for tg in range(4):
                            latent_norm(2, ckv_w, tg * 512, gkv, lambda k, tg=tg: ckvn[:, k, tg * 512:(tg + 1) * 512], "ckvn", 256.0)
                        for tg in range(4):
                            bank = tg % 2
                            for c in range(16):
                                S.op("pe", lambda E, c=c, bank=bank, tg=tg: E.matmul(
                                    ps[bank][:, :], lhsT=wmB[:, c, 0:128], rhs=hTx(c, tg * 512, 512),
                                    start=(c == 0), stop=(c == 15)),
                                    reads=["wmB"] + HT_ALL, writes=[("ps", bank)], inc=(c == 15))
                            pipe.push(rope_evac(bank, 128, krT[:, tg * 512:(tg + 1) * 512], tg * 512, cos_m, sin_m, R_m, "krT", "tab_m"))
                        pipe.flush()
                    S.barrier()
                    for h in range(8):
                        precast(3, after=([("obT", h - 1)] if h >= 1 else []))
                        for tg in range(2):
                            bank = tg % 2
                            for c in range(4):
                                S.op("pe", lambda E, c=c, bank=bank, tg=tg, h=h: E.matmul(
                                    ps[bank][0:96, :], lhsT=wuq[:, c, h * 96:(h + 1) * 96], rhs=cqn[:, c, tg * 512:(tg + 1) * 512],
                                    start=(c == 0), stop=(c == 3)),
                                    reads=["wuq", "cqn"], writes=[("ps", bank)], inc=(c == 3))
                            pipe.push(rope_evac(bank, 96, qT[0:96, tg * 512:(tg + 1) * 512], 1024 + tg * 512, cos_m, sin_m, R_m, "qT", "tab_m"))
                        pipe.flush()
                        for tg in range(4):
                            bank = tg % 2
                            for c in range(2):
                                S.op("pe", lambda E, c=c, bank=bank, tg=tg, h=h: E.matmul(
                                    ps[bank][0:64, :], lhsT=wukv[:, c, h * 192:h * 192 + 64], rhs=ckvn[:, c, tg * 512:(tg + 1) * 512],
                                    start=(c == 0), stop=(c == 1)),
                                    reads=["wukv", "ckvn"], writes=[("ps", bank)], inc=(c == 1))
                            S.op("act", lambda E, bank=bank, tg=tg: E.activation(out=kT[0:64, tg * 512:(tg + 1) * 512], in_=ps[bank][0:64, :], func=AF.Copy),
                                 reads=[("ps", bank)], writes=["kT"])
                        S.op("dve", lambda E: E.tensor_copy(out=kT[64:96, :], in_=krT[64:96, :]), reads=["krT"], writes=["kT"])
                        for tq in range(4):
                            bank = tq % 2
                            for ti in range(4):
                                tt = tq * 4 + ti
                                for c in range(2):
                                    S.op("pe", lambda E, bank=bank, ti=ti, tt=tt, c=c, h=h: E.matmul(
                                        ps[bank][:, ti * 128:(ti + 1) * 128], lhsT=ckvn[:, c, tt * 128:(tt + 1) * 128],
                                        rhs=wukv[:, c, h * 192 + 64:h * 192 + 192], start=(c == 0), stop=(c == 1)),
                                        reads=["wukv", "ckvn"], writes=[("ps", bank)], inc=(c == 1))
                            v_evac(bank, tq)
                        for g in range(2):
                            def donem(qtile, ob, h=h):
                                i = qtile % 4
                                sm = small[:, 28 + i:29 + i]
                                S.op("dve", lambda E: E.reciprocal(out=sm, in_=ps[ob][:, 128:129]), reads=[("ps", ob)], writes=[("r3", i)])
                                ob_i = state["onb"] % 2
                                state["onb"] += 1
                                S.op("dve", lambda E: E.tensor_scalar(out=onb[ob_i][:], in0=ps[ob][:, 0:128], scalar1=sm, scalar2=None, op0=ALU.mult),
                                     reads=[("ps", ob), ("r3", i)], writes=[("onb", ob_i)])
                                S.op("pe", lambda E: E.transpose(out=psb(3)[:, 0:128], in_=onb[ob_i][:], identity=ident_b),
                                     reads=[("onb", ob_i), "cstb"], writes=[("ps", 3)])
                                S.op("act", lambda E: E.activation(out=obT[:, h, qtile * 128:(qtile + 1) * 128], in_=psb(3)[:, 0:128], func=AF.Copy),
                                     reads=[("ps", 3)], writes=[("obT", h)])
                            attn_unit(96, 0, float(96.0 ** -0.5), g, donem)
                S.barrier()
        S.barrier()

        OAT = [("oaT", h) for h in range(8)]
        OBT = [("obT", h) for h in range(8)]
        if debug:
            S.op("sp", lambda E: E.dma_start(out=dbg["oaT"], in_=bufO[:, 0:8192]), reads=OAT, dsem="d_dbg2")
            S.op("sp", lambda E: E.dma_start(out=dbg["obT"], in_=bufO[:, 8192:16384]), reads=OBT, dsem="d_dbg3")

        MIX = [("hT", t) for t in range(8)]
        if stage >= 4:
            with contextlib.ExitStack() as es4:
                def sb4(name, shape, dt):
                    return es4.enter_context(nc.sbuf_tensor(name, list(shape), dt))
                wgt = [sb4(f"wgt{i}", [128, 16, 256], BF16) for i in range(2)]
                wat = [sb4(f"wat{i}", [128, 8, 128], BF16) for i in range(2)]
                wbt = [sb4(f"wbt{i}", [128, 8, 128], BF16) for i in range(2)]
                sga = [sb4(f"sga{i}", [128, 512], F32) for i in range(2)]
                sgb = [sb4(f"sgb{i}", [128, 512], F32) for i in range(2)]
                m1 = [sb4(f"m1_{i}", [128, 512], F32) for i in range(2)]
                m2 = [sb4(f"m2_{i}", [128, 512], F32) for i in range(2)]
                it = 0
                for j in range(16):
                    wb = j % 2
                    S.op("pool", lambda E, wb=wb, j=j: E.dma_start(out=ch(fl(wgt[wb]), 2048), in_=ch(wg_d[j], 2048)),
                         writes=[("wgt", wb)], dsem=f"d_wg{wb}")
                    S.op("pool", lambda E, wb=wb, j=j: E.dma_start(out=fl(wat[wb]), in_=wba_d[j]),
                         writes=[("wat", wb)], dsem=f"d_wa{wb}")
                    S.op("pool", lambda E, wb=wb, j=j: E.dma_start(out=fl(wbt[wb]), in_=wbb_d[j]),
                         writes=[("wbt", wb)], dsem=f"d_wb{wb}")
                    for tg in range(2):
                        pb = 4 * (it % 2)
                        ib = it % 2
                        it += 1
                        tsl = slice(tg * 512, (tg + 1) * 512)
                        for c in range(16):
                            S.op("pe", lambda E, c=c, pb=pb, wb=wb, tsl=tsl: E.matmul(
                                ps[pb][:, :], lhsT=wgt[wb][:, c, 0:128], rhs=hTo[:, c, tsl], start=(c == 0), stop=(c == 15)),
                                reads=[("wgt", wb)] + HT_OWN, writes=[("ps", pb)], inc=(c == 15))
                        for c in range(16):
                            S.op("pe", lambda E, c=c, pb=pb, wb=wb, tsl=tsl: E.matmul(
                                ps[pb + 1][:, :], lhsT=wgt[wb][:, c, 128:256], rhs=hTo[:, c, tsl], start=(c == 0), stop=(c == 15)),
                                reads=[("wgt", wb)] + HT_OWN, writes=[("ps", pb + 1)], inc=(c == 15))
                        for hh in range(8):
                            S.op("pe", lambda E, hh=hh, pb=pb, wb=wb, tg=tg: E.matmul(
                                ps[pb + 2][:, :], lhsT=wat[wb][:, hh, :], rhs=oaT[:, hh, tg * 512:(tg + 1) * 512], start=(hh == 0), stop=(hh == 7)),
                                reads=[("wat", wb)] + OAT, writes=[("ps", pb + 2)], inc=(hh == 7))
                        for hh in range(8):
                            S.op("pe", lambda E, hh=hh, pb=pb, wb=wb, tg=tg: E.matmul(
                                ps[pb + 3][:, :], lhsT=wbt[wb][:, hh, :], rhs=obT[:, hh, tg * 512:(tg + 1) * 512], start=(hh == 0), stop=(hh == 7)),
                                reads=[("wbt", wb)] + OBT, writes=[("ps", pb + 3)], inc=(hh == 7))
                        S.op("act", lambda E, pb=pb, ib=ib: E.activation(out=sga[ib][:], in_=ps[pb][:, :], func=AF.Sigmoid),
                             reads=[("ps", pb)], writes=[("sga", ib)])
                        S.op("act", lambda E, pb=pb, ib=ib: E.activation(out=sgb[ib][:], in_=ps[pb + 1][:, :], func=AF.Sigmoid),
                             reads=[("ps", pb + 1)], writes=[("sgb", ib)])
                        S.op("dve", lambda E, pb=pb, ib=ib: E.tensor_tensor(out=m1[ib][:], in0=ps[pb + 2][:, :], in1=sga[ib][:], op=ALU.mult),
                             reads=[("ps", pb + 2), ("sga", ib)], writes=[("m1", ib)])
                        S.op("dve", lambda E, pb=pb, ib=ib: E.tensor_tensor(out=m2[ib][:], in0=ps[pb + 3][:, :], in1=sgb[ib][:], op=ALU.mult),
                             reads=[("ps", pb + 3), ("sgb", ib)], writes=[("m2", ib)])
                        S.op("dve", lambda E, ib=ib, j=j, tg=tg: E.tensor_tensor(out=hTc[:, j, tg * 512:(tg + 1) * 512], in0=m1[ib][:], in1=m2[ib][:], op=ALU.add),
                             reads=[("m1", ib), ("m2", ib)], writes=[("hT", 4 * tg + k) for k in range(4)])
            S.barrier()

        if stage >= 5:
            with contextlib.ExitStack() as es5:
                def sb5(name, shape, dt):
                    return es5.enter_context(nc.sbuf_tensor(name, list(shape), dt))
                XM = [("xm", t) for t in range(8)]
                for t in range(8):
                    S.op("sp", lambda E, t=t: E.dma_start(out=xm(t)[:, :], in_=x_own[t * 128:(t + 1) * 128, :]),
                         writes=[("xm", t)], dsem=f"d_xm{t}")
                with contextlib.ExitStack() as es5a:
                    wo = [es5a.enter_context(nc.sbuf_tensor(f"wo{i}", [128, 16, 512], BF16)) for i in range(2)]
                    it = 0
                    for n in range(4):
                        wb = n % 2
                        S.op("pool", lambda E, wb=wb, n=n: E.dma_start(out=ch(fl(wo[wb]), 2048), in_=ch(wout_d[n], 2048)),
                             writes=[("wo", wb)], dsem=f"d_wo{wb}")
                        for t in range(8):
                            bank = it % 4
                            it += 1
                            for c in range(16):
                                S.op("pe", lambda E, c=c, bank=bank, t=t, wb=wb: E.matmul(
                                    ps[bank][:, :], lhsT=hTc[:, c, t * 128:(t + 1) * 128], rhs=wo[wb][:, c, :], start=(c == 0), stop=(c == 15)),
                                    reads=[("wo", wb)] + MIX, writes=[("ps", bank)], inc=(c == 15))
                            S.op("dve", lambda E, bank=bank, t=t, n=n: E.tensor_tensor(
                                out=xm(t)[:, n * 512:(n + 1) * 512], in0=ps[bank][:, :], in1=xm(t)[:, n * 512:(n + 1) * 512], op=ALU.add),
                                reads=[("ps", bank), ("xm", t)], writes=[("xm", t)])
                S.barrier()
                if debug:
                    S.op("sp", lambda E: E.dma_start(out=dbg["xmid"][:, 0:8192], in_=xmA.rearrange("p a b -> p (a b)")), reads=XM, dsem="d_dbg4")
                    S.op("sp", lambda E: E.dma_start(out=dbg["xmid"][:, 8192:16384], in_=xmB.rearrange("p a b -> p (a b)")), reads=XM, dsem="d_dbg5")

                if stage >= 6:
                    Wc = sb5("Wc", [128, 8, 32], F32)
                    Ab = sb5("Ab", [128, 8, 32], BF16)
                    Af = sb5("Af", [128, 8, 32], F32)
                    A1 = sb5("A1", [128, 8, 32], F32)
                    A2 = sb5("A2", [128, 8, 32], F32)
                    sidx = sb5("sidx", [128, 8, 2], I32)
                    sidf = sb5("sidf", [128, 8, 2], F32)
                    rankf = sb5("rankf", [128, 8, 32], F32)
                    with contextlib.ExitStack() as es6:
                        def sb6(name, shape, dt):
                            return es6.enter_context(nc.sbuf_tensor(name, list(shape), dt))
                        gbc = sb6("gbc", [128, 2048], F32)
                        hff = sb6("hff", [128, 2048], F32)
                        hfT = sb6("hfT", [128, 16, 128], F32)
                        wr = sb6("wr", [128, 16, 36], F32)
                        rbias = sb6("rbias", [128, 36], F32)
                        lg = sb6("lg", [128, 36], F32)
                        rt = sb6("rt", [128, 64], F32)
                        elm = sb6("elm", [128, 32], F32)
                        RB = sb6("RB", [128, 8, 32], F32)
                        RT = sb6("RT", [128, 8, 32], F32)
                        junk = sb6("junk", [128, 2048], BF16)
                        S.op("sp", lambda E: E.dma_start(out=gbc[:], in_=ffn_d.partition_broadcast(128)), writes=["gbc"], dsem="d_gbc")
                        S.op("sp", lambda E: E.dma_start(out=fl(wr), in_=wr_d), writes=["wr"], dsem="d_wr")
                        S.op("sp", lambda E: E.dma_start(out=rbias[:], in_=rb_d.partition_broadcast(128)), writes=["rbias"], dsem="d_rb")
                        for t in range(8):
                            a1 = A1[:, t, :]
                            a2 = A2[:, t, :]
                            ss = small[:, 40:41]
                            S.op("act", lambda E, t=t: E.activation(out=junk[:], in_=xm(t)[:, :], func=AF.Square, accum_out=ss),
                                 reads=[("xm", t)], writes=["junk", "ss6"])
                            S.op("act", lambda E: E.activation(out=ss, in_=ss, func=AF.Ln, bias=EPS, scale=1.0 / 2048.0), reads=["ss6"], writes=["ss6"])
                            S.op("act", lambda E: E.activation(out=ss, in_=ss, func=AF.Exp, scale=-0.5), reads=["ss6"], writes=["ss6"])
                            S.op("dve", lambda E, t=t: E.scalar_tensor_tensor(out=hff[:], in0=xm(t)[:, :], scalar=ss, in1=gbc[:], op0=ALU.mult, op1=ALU.mult),
                                 reads=[("xm", t), "ss6", "gbc"], writes=["hff"])
                            S.op("act", lambda E, t=t: E.activation(out=hfb[:, t, :], in_=hff[:], func=AF.Copy), reads=["hff"], writes=[("hfb", t)])
                            for q4 in range(4):
                                bank = q4
                                for j in range(4):
                                    c = q4 * 4 + j
                                    S.op("pe", lambda E, bank=bank, j=j, c=c: E.transpose(
                                        out=ps[bank][:, j * 128:(j + 1) * 128], in_=hff[:, c * 128:(c + 1) * 128], identity=ident_f),
                                        reads=["hff", "cstf"], writes=[("ps", bank)], inc=(j == 3))
                                S.op("act" if q4 % 2 else "dve",
                                     (lambda E, bank=bank, q4=q4: E.activation(out=hfT[:, q4 * 4:(q4 + 1) * 4, :], in_=ps[bank][:, :].rearrange("p (a b) -> p a b", b=128), func=AF.Copy))
                                     if q4 % 2 else
                                     (lambda E, bank=bank, q4=q4: E.tensor_copy(out=hfT[:, q4 * 4:(q4 + 1) * 4, :], in_=ps[bank][:, :].rearrange("p (a b) -> p a b", b=128))),
                                     reads=[("ps", bank)], writes=[("hfT", q4)])
                            for c in range(16):
                                S.op("pe", lambda E, c=c: E.matmul(ps[4][:, 0:36], lhsT=hfT[:, c, :], rhs=wr[:, c, :], start=(c == 0), stop=(c == 15)),
                                     reads=[("hfT", c // 4), "wr"], writes=[("ps", 4)], inc=(c == 15))
                            S.op("dve", lambda E: E.tensor_tensor(out=lg[:], in0=ps[4][:, 0:36], in1=rbias[:], op=ALU.add),
                                 reads=[("ps", 4), "rbias"], writes=["lg"])
                            S.op("dve", lambda E: E.tensor_reduce(out=rt[:, 0:1], in_=lg[:, 0:4], axis=AX.X, op=ALU.max), reads=["lg"], writes=["rt0"])
                            S.op("dve", lambda E: E.tensor_scalar(out=rt[:, 4:8], in0=lg[:, 0:4], scalar1=rt[:, 0:1], scalar2=None, op0=ALU.is_equal),
                                 reads=["lg", "rt0"], writes=["gm"])
                            S.op("dve", lambda E: E.tensor_scalar(out=rt[:, 1:2], in0=rt[:, 0:1], scalar1=-1.0, scalar2=None, op0=ALU.mult),
                                 reads=["rt0"], writes=["rt1"])
                            S.op("act", lambda E: E.activation(out=rt[:, 8:12], in_=lg[:, 0:4], func=AF.Exp, bias=rt[:, 1:2], scale=1.0, accum_out=rt[:, 2:3]),
                                 reads=["lg", "rt1"], writes=["rt2", "rt8"])
                            S.op("dve", lambda E: E.reciprocal(out=rt[:, 3:4], in_=rt[:, 2:3]), reads=["rt2"], writes=["pg"])
                            S.op("dve", lambda E: E.tensor_scalar(out=rt[:, 4:8], in0=rt[:, 4:8], scalar1=-1.0, scalar2=1e30, op0=ALU.add, op1=ALU.mult),
                                 reads=["gm"], writes=["gm"])
                            S.op("dve", lambda E: E.tensor_tensor(out=elm[:].rearrange("p (a b) -> p a b", b=8), in0=lg[:, 4:36].rearrange("p (a b) -> p a b", b=8),
                                                                  in1=rt[:, 4:8].unsqueeze(2).broadcast_to([128, 4, 8]), op=ALU.add),
                                 reads=["lg", "gm"], writes=["elm"])
                            S.op("dve", lambda E: E.max(out=rt[:, 16:24], in_=elm[:]), reads=["elm"], writes=["top8"])
                            S.op("dve", lambda E, a1=a1: E.tensor_scalar(out=a1, in0=elm[:], scalar1=rt[:, 16:17], scalar2=None, op0=ALU.is_equal),
                                 reads=["elm", "top8"], writes=["a1"])
                            S.op("dve", lambda E, a2=a2: E.tensor_scalar(out=a2, in0=elm[:], scalar1=rt[:, 17:18], scalar2=None, op0=ALU.is_equal),
                                 reads=["elm", "top8"], writes=["a2"])
                            S.op("dve", lambda E: E.tensor_tensor(out=rt[:, 24:25], in0=rt[:, 16:17], in1=rt[:, 17:18], op=ALU.subtract),
                                 reads=["top8"], writes=["dd"])
                            S.op("act", lambda E: E.activation(out=rt[:, 25:26], in_=rt[:, 24:25], func=AF.Sigmoid), reads=["dd"], writes=["w1"])
                            S.op("dve", lambda E: E.tensor_tensor(out=rt[:, 26:27], in0=rt[:, 25:26], in1=rt[:, 3:4], op=ALU.mult),
                                 reads=["w1", "pg"], writes=["cw1"])
                            S.op("dve", lambda E: E.tensor_tensor(out=rt[:, 27:28], in0=rt[:, 3:4], in1=rt[:, 26:27], op=ALU.subtract),
                                 reads=["cw1", "pg"], writes=["cw2"])
                            S.op("dve", lambda E, t=t, a1=a1: E.tensor_scalar(out=Wc[:, t, :], in0=a1, scalar1=rt[:, 26:27], scalar2=None, op0=ALU.mult),
                                 reads=["a1", "cw1"], writes=[("Wc", t)])
                            S.op("dve", lambda E, t=t, a2=a2: E.scalar_tensor_tensor(out=Wc[:, t, :], in0=a2, scalar=rt[:, 27:28], in1=Wc[:, t, :], op0=ALU.mult, op1=ALU.add),
                                 reads=["a2", "cw2", ("Wc", t)], writes=[("Wc", t)])
                            S.op("dve", lambda E, t=t, a1=a1, a2=a2: E.tensor_tensor(out=Ab[:, t, :], in0=a1, in1=a2, op=ALU.add),
                                 reads=["a1", "a2"], writes=[("Ab", t)])
                            S.op("dve", lambda E, t=t, a1=a1, a2=a2: E.tensor_tensor(out=Af[:, t, :], in0=a1, in1=a2, op=ALU.add),
                                 reads=["a1", "a2"], writes=[("Af", t)])
                        for t in range(8):
                            for tp in range(t + 1):
                                S.op("pe", lambda E, t=t, tp=tp: E.matmul(ps[5][:, 0:32], lhsT=(ustrict if tp == t else ones_b), rhs=Ab[:, tp, :],
                                                                          start=(tp == 0), stop=(tp == t)),
                                     reads=[("Ab", tp), "cstb"], writes=[("ps", 5)], inc=(tp == t))
                            S.op("dve", lambda E, t=t: E.tensor_copy(out=rankf[:, t, :], in_=ps[5][:, 0:32]), reads=[("ps", 5)], writes=[("rank", t)])
                        RANKS = [("rank", t) for t in range(8)]
                        S.op("dve", lambda E: E.tensor_tensor(out=RB[:], in0=rankf[:], in1=ebase.unsqueeze(1).broadcast_to([128, 8, 32]), op=ALU.add),
                             reads=RANKS + ["cstf"], writes=["RB"])
                        for k, Ak in ((0, A1), (1, A2)):
                            S.op("dve", lambda E, Ak=Ak: E.tensor_tensor(out=RT[:], in0=RB[:], in1=Ak[:], op=ALU.mult), reads=["RB", "a1", "a2"], writes=["RT"])
                            S.op("dve", lambda E, k=k: E.tensor_reduce(out=sidf[:, :, k], in_=RT[:], axis=AX.X, op=ALU.add), reads=["RT"], writes=[("sidf", k)])
                            S.op("dve", lambda E, Ak=Ak: E.tensor_tensor(out=RT[:], in0=rankf[:], in1=Ak[:], op=ALU.mult), reads=RANKS + ["a1", "a2"], writes=["RT"])
                            S.op("dve", lambda E, k=k: E.tensor_reduce(out=rt[:, 32 + 8 * k:40 + 8 * k], in_=RT[:], axis=AX.X, op=ALU.add), reads=["RT"], writes=[("rk", k)])
                            S.op("dve", lambda E, k=k: E.tensor_scalar(out=rt[:, 32 + 8 * k:40 + 8 * k], in0=rt[:, 32 + 8 * k:40 + 8 * k], scalar1=127.5, scalar2=1e6,
                                                                      op0=ALU.is_gt, op1=ALU.mult), reads=[("rk", k)], writes=[("rk", k)])
                            S.op("dve", lambda E, k=k: E.tensor_tensor(out=sidf[:, :, k], in0=sidf[:, :, k], in1=rt[:, 32 + 8 * k:40 + 8 * k], op=ALU.add),
                                 reads=[("sidf", k), ("rk", k)], writes=[("sidf", k)])
                        S.op("dve", lambda E: E.tensor_scalar(out=sidf[:], in0=sidf[:], scalar1=4096.0, scalar2=None, op0=ALU.min),
                             reads=[("sidf", 0), ("sidf", 1)], writes=["sidf2"])
                        S.op("dve", lambda E: E.tensor_copy(out=sidx[:], in_=sidf[:]), reads=["sidf2"], writes=["sidx"])

                if stage >= 6:
                    S.barrier()
                if stage >= 7:
                    with contextlib.ExitStack() as es7:
                        def sb7(name, shape, dt):
                            return es7.enter_context(nc.sbuf_tensor(name, list(shape), dt))
                        wring = [sb7(f"wring{i}", [128, 16, 512], BF16) for i in range(4)]
                        hTc_flat = hTc[:].rearrange("p a b -> p (a b)")
                        wring.append(hTc_flat[:, 0:8192].rearrange("p (c n) -> p c n", n=512))
                        wring.append(hTc_flat[:, 8192:16384].rearrange("p (c n) -> p c n", n=512))
                        NRING = 6
                        HFB_ALL = [("hfb", t) for t in range(8)]

                        def ring_res(s):
                            return [("wring", s)] + (HFB_ALL if s >= 4 else [])

                        def fl2(t3):
                            return (t3[:] if hasattr(t3, "alloc_name") else t3).rearrange("p c n -> p (c n)")
                        Pd = [sb7("Pd0", [128, 8, 128], BF16)] * 2
                        NXG = 3
                        xg = [sb7(f"xg{i}", [128, 2048], BF16) for i in range(NXG)]
                        ye = [sb7(f"ye{i}", [128, 2048], BF16) for i in range(2)]
                        Rfull = sb7("Rfull", [128, 8, 32, 4], BF16)
                        idx_i = sb7("idx_i", [128, 32], I32)
                        idx_f = sb7("idx_f", [128, 32], F32)
                        wsl = sb7("wsl", [128, 32], F32)
                        hfe = sb7("hfe", [128, 16, 128], BF16)
                        sg = sb7("sg", [128, 512], F32)
                        ae = sb7("ae", [128, 512], BF16)
                        aT = sb7("aT", [128, 4, 128], BF16)
                        precast(96)
                        WCS = [("Wc", t) for t in range(8)]
                        Rc3 = Rc.rearrange("p (t k) -> p t k", k=4)
                        for k in range(3):
                            S.op("dve", lambda E, k=k: E.tensor_copy(out=Rfull[:, :, :, k], in_=Rc3[:, :, k].unsqueeze(2).broadcast_to([128, 8, 32])),
                                 reads=["cstf"], writes=["Rfull"])
                        S.op("dve", lambda E: E.tensor_copy(out=Rfull[:, :, :, 3], in_=Wc[:]), reads=WCS, writes=["Rfull"])
                        S.op("dve", lambda E: E.memset(xg[0][:], 0.0), writes=[("xg", 0)])
                        S.op("sp", lambda E: E.dma_start(out=hf_s[1024:1025, :], in_=xg[0][0:1, :]), reads=[("xg", 0)], writes=["hfz"], dsem="d_hfz")
                        S.op("sp", lambda E: E.dma_start(out=y_s[4096:4097, :], in_=xg[0][0:1, :]), reads=[("xg", 0)], writes=["yz"], dsem="d_yz")
                        S.op("sp", lambda E: E.dma_start(out=hf_s[0:1024, :].rearrange("(t p) f -> p t f", p=128), in_=hfb),
                             reads=[("hfb", t) for t in range(8)], writes=["hfd"], dsem="d_hfd")

                        def prep_a(e):
                            pb_ = 0
                            for t in range(8):
                                S.op("dve", lambda E, t=t: E.tensor_scalar(
                                    out=Pd[pb_][:, t, :], in0=iota_f, scalar1=rankf[:, t, e:e + 1], scalar2=Af[:, t, e:e + 1],
                                    op0=ALU.is_equal, op1=ALU.mult),
                                    reads=[("rank", t), ("Af", t), "cstf"], writes=[("Pd", pb_)])

                        def prep_b(e):
                            pb_ = 0
                            xb_ = e % NXG
                            for t in range(8):
                                S.op("pe", lambda E, t=t: E.matmul(ps[6][:, 0:4], lhsT=Pd[pb_][:, t, :], rhs=Rfull[:, t, e, :], start=(t == 0), stop=(t == 7)),
                                     reads=[("Pd", pb_), "Rfull"], writes=[("ps", 6)], inc=(t == 7))
                            S.op("dve", lambda E: E.tensor_copy(out=small[:, 48:52], in_=ps[6][:, 0:4]),
                                 reads=[("ps", 6)], writes=["ixr"])
                            S.op("dve", lambda E: E.scalar_tensor_tensor(out=idx_f[:, e:e + 1], in0=small[:, 49:50], scalar=128.0, in1=small[:, 48:49],
                                                                         op0=ALU.mult, op1=ALU.add), reads=["ixr"], writes=[("idxf", e)])
                            S.op("dve", lambda E: E.tensor_scalar(out=small[:, 52:53], in0=small[:, 50:51], scalar1=-1024.0, scalar2=1024.0, op0=ALU.mult, op1=ALU.add),
                                 reads=["ixr"], writes=["ixo"])
                            S.op("dve", lambda E: E.tensor_tensor(out=idx_f[:, e:e + 1], in0=idx_f[:, e:e + 1], in1=small[:, 52:53], op=ALU.add),
                                 reads=[("idxf", e), "ixo"], writes=[("idxf", e)])
                            S.op("dve", lambda E: E.tensor_copy(out=idx_i[:, e:e + 1], in_=idx_f[:, e:e + 1]), reads=[("idxf", e)], writes=[("idxi", e)])
                            S.op("dve", lambda E: E.tensor_copy(out=wsl[:, e:e + 1], in_=small[:, 51:52]), reads=["ixr"], writes=[("wsl", e)])
                            S.op("pool", lambda E: E.indirect_dma_start(out=xg[xb_][:], out_offset=None, in_=hf_s,
                                                                        in_offset=bass.IndirectOffsetOnAxis(ap=idx_i[:, e:e + 1], axis=0)),
                                 reads=[("idxi", e), "hfd", "hfz"], writes=[("xg", xb_)], dsem=f"d_xg{xb_}")

                        def wslots(e):
                            sg_, su_, sd_ = (3 * e) % NRING, (3 * e + 1) % NRING, (3 * e + 2) % NRING
                            wdt_ = fl2(wring[sd_]).rearrange("p (f n) -> p f n", n=2048)
                            return sg_, su_, sd_, wring[sg_], wring[su_], wdt_

                        def loads(e):
                            pcr = [("pc", e, "g"), ("pc", e, "u"), ("pc", e, "d")]
                            sg_, su_, sd_, wgt_, wut_, wdt_ = wslots(e)
                            if e % 6 == 5:
                                S.op("pool", lambda E: E.dma_start(out=ch(fl2(wgt_), 2048), in_=ch(weg_d[e], 2048)),
                                     writes=ring_res(sg_), dsem=f"d_wrp{sg_}")
                                S.op("pool", lambda E: E.dma_start(out=ch(fl2(wut_), 2048), in_=ch(weu_d[e], 2048)),
                                     writes=ring_res(su_), dsem=f"d_wrp{su_}")
                                S.op("pool", lambda E: E.dma_start(out=ch(fl2(wring[sd_]), 2048), in_=ch(wed_d[e], 2048)),
                                     writes=ring_res(sd_), dsem=f"d_wrp{sd_}")
                                return
                            S.op("sp", lambda E: E.dma_start(out=fl2(wgt_), in_=weg_s[e]),
                                 reads=pcr, writes=ring_res(sg_), dsem=f"d_wr{sg_}")
                            S.op("sp", lambda E: E.dma_start(out=fl2(wut_), in_=weu_s[e]),
                                 reads=pcr, writes=ring_res(su_), dsem=f"d_wr{su_}")
                            S.op("sp", lambda E: E.dma_start(out=fl2(wring[sd_]), in_=wed_s[e]),
                                 reads=pcr, writes=ring_res(sd_), dsem=f"d_wr{sd_}")

                        def moe_t(e):
                            pb_ = e % NXG
                            for hb in range(2):
                                for j in range(8):
                                    c = hb * 8 + j
                                    S.op("pe", lambda E, hb=hb, j=j, c=c: E.transpose(out=psb(hb)[:, j * 128:(j + 1) * 128], in_=xg[pb_][:, c * 128:(c + 1) * 128], identity=ident_b),
                                         reads=[("xg", pb_), "cstb"], writes=[("ps", hb)], inc=(j == 7))
                                if hb == 0:
                                    S.op("act", lambda E: E.activation(out=hfe[:, 0:8, :], in_=psb(0).rearrange("p (a b) -> p a b", b=128), func=AF.Copy),
                                         reads=[("ps", 0)], writes=[("hfe", 0)])
                                else:
                                    S.op("act", lambda E: E.activation(out=hfe[:, 8:16, :], in_=psb(1).rearrange("p (a b) -> p a b", b=128), func=AF.Copy),
                                         reads=[("ps", 1)], writes=[("hfe", 1)])

                        def moe_gu(e):
                            sg_, su_, sd_, wgt_, wut_, wdt_ = wslots(e)
                            for c in range(16):
                                S.op("pe", lambda E, c=c: E.matmul(ps[2][:, :], lhsT=hfe[:, c, :], rhs=wgt_[:, c, :], start=(c == 0), stop=(c == 15)),
                                     reads=[("hfe", c // 8), ("wring", sg_)], writes=[("ps", 2)], inc=(c == 15))
                            for c in range(16):
                                S.op("pe", lambda E, c=c: E.matmul(ps[3][:, :], lhsT=hfe[:, c, :], rhs=wut_[:, c, :], start=(c == 0), stop=(c == 15)),
                                     reads=[("hfe", c // 8), ("wring", su_)], writes=[("ps", 3)], inc=(c == 15))
                            S.op("act", lambda E: E.activation(out=sg[:], in_=ps[2][:, :], func=AF.Silu), reads=[("ps", 2)], writes=["sg"])
                            S.op("dve", lambda E: E.tensor_tensor(out=ae[:], in0=ps[3][:, :], in1=sg[:], op=ALU.mult), reads=[("ps", 3), "sg"], writes=["ae"])

                        def moe_at(e):
                            for j in range(4):
                                S.op("pe", lambda E, j=j: E.transpose(out=psb(7)[:, j * 128:(j + 1) * 128], in_=ae[:, j * 128:(j + 1) * 128], identity=ident_b),
                                     reads=["ae", "cstb"], writes=[("ps", 7)], inc=(j == 3))
                            S.op("act", lambda E: E.activation(out=aT[:], in_=psb(7)[:, 0:512].rearrange("p (a b) -> p a b", b=128), func=AF.Copy),
                                 reads=[("ps", 7)], writes=["aT"])

                        def moe_dn(e):
                            pb_ = e % 2
                            sg_, su_, sd_, wgt_, wut_, wdt_ = wslots(e)
                            for n in range(4):
                                bank = 4 + (n % 2)
                                for f in range(4):
                                    S.op("pe", lambda E, bank=bank, f=f, n=n: E.matmul(
                                        ps[bank][:, :], lhsT=aT[:, f, :], rhs=wdt_[:, f, n * 512:(n + 1) * 512], start=(f == 0), stop=(f == 3)),
                                        reads=["aT", ("wring", sd_)], writes=[("ps", bank)], inc=(f == 3))
                                if n % 2 == 0:
                                    S.op("act", lambda E, bank=bank, n=n: E.activation(out=ye[pb_][:, n * 512:(n + 1) * 512], in_=ps[bank][:, :], func=AF.Copy, scale=wsl[:, e:e + 1]),
                                         reads=[("ps", bank), ("wsl", e)], writes=[("ye", pb_)])
                                else:
                                    S.op("dve", lambda E, bank=bank, n=n: E.tensor_scalar(out=ye[pb_][:, n * 512:(n + 1) * 512], in0=ps[bank][:, :], scalar1=wsl[:, e:e + 1], scalar2=None, op0=ALU.mult),
                                         reads=[("ps", bank), ("wsl", e)], writes=[("ye", pb_)])
                            S.op("act", lambda E: E.dma_start(out=y_s[e * 128:(e + 1) * 128, :], in_=ye[pb_][:]),
                                 reads=[("ye", pb_)], writes=[("Yd", e)], dsem=f"d_y{pb_}")

                        loads(0)
                        loads(1)
                        for e0 in range(3):
                            prep_a(e0)
                            prep_b(e0)
                        moe_t(0)
                        moe_gu(0)
                        for e in range(NEXP):
                            if e + 1 < NEXP:
                                moe_t(e + 1)
                            if e + 3 < NEXP:
                                prep_a(e + 3)
                            moe_at(e)
                            if e + 1 < NEXP:
                                moe_gu(e + 1)
                            moe_dn(e)
                            if e + 2 < NEXP:
                                loads(e + 2)
                            if e + 3 < NEXP:
                                prep_b(e + 3)

                S.barrier()
                with contextlib.ExitStack() as es8:
                    fbc = es8.enter_context(nc.sbuf_tensor("fbc", [128, 2048], F32))
                    ot = [es8.enter_context(nc.sbuf_tensor(f"ot{i}", [128, 2048], F32)) for i in range(2)]
                    jk = es8.enter_context(nc.sbuf_tensor("jk", [128, 2048], BF16))
                    if stage >= 7:
                        yg = [[es8.enter_context(nc.sbuf_tensor(f"yg{i}_{k}", [128, 2048], BF16)) for k in range(2)] for i in range(2)]
                        S.op("sp", lambda E: E.dma_start(out=fbc[:], in_=fin_d.partition_broadcast(128)), writes=["fbc"], dsem="d_fbc")
                        YD = [("Yd", e) for e in range(NEXP)] + ["yz"]
                    for t in range(8):
                        ob_ = t % 2
                        if stage >= 7:
                            for k in range(2):
                                S.op("pool", lambda E, t=t, k=k, ob_=ob_: E.indirect_dma_start(
                                    out=yg[ob_][k][:], out_offset=None, in_=y_s, in_offset=bass.IndirectOffsetOnAxis(ap=sidx[:, t, k:k + 1], axis=0)),
                                    reads=YD + ["sidx"], writes=[("yg", ob_, k)], dsem=f"d_yg{ob_}{k}")
                                S.op("dve", lambda E, t=t, k=k, ob_=ob_: E.tensor_tensor(out=xm(t)[:, :], in0=xm(t)[:, :], in1=yg[ob_][k][:], op=ALU.add),
                                     reads=[("xm", t), ("yg", ob_, k)], writes=[("xm", t)])
                            ss = small[:, 44 + ob_:45 + ob_]
                            S.op("act", lambda E, t=t, ss=ss: E.activation(out=jk[:], in_=xm(t)[:, :], func=AF.Square, accum_out=ss),
                                 reads=[("xm", t)], writes=["jk", ("ss8", ob_)])
                            S.op("act", lambda E, ss=ss: E.activation(out=ss, in_=ss, func=AF.Ln, bias=EPS, scale=1.0 / 2048.0), reads=[("ss8", ob_)], writes=[("ss8", ob_)])
                            S.op("act", lambda E, ss=ss: E.activation(out=ss, in_=ss, func=AF.Exp, scale=-0.5), reads=[("ss8", ob_)], writes=[("ss8", ob_)])
                            S.op("dve", lambda E, t=t, ob_=ob_, ss=ss: E.scalar_tensor_tensor(out=ot[ob_][:], in0=xm(t)[:, :], scalar=ss, in1=fbc[:], op0=ALU.mult, op1=ALU.mult),
                                 reads=[("xm", t), ("ss8", ob_), "fbc"], writes=[("ot", ob_)])
                        else:
                            S.op("dve", lambda E, t=t, ob_=ob_: E.tensor_copy(out=ot[ob_][:], in_=xm(t)[:, :]), reads=[("xm", t)], writes=[("ot", ob_)])
                        S.op("sp", lambda E, t=t, ob_=ob_: E.dma_start(out=out_d[t * 128:(t + 1) * 128, :], in_=ot[ob_][:]),
                             reads=[("ot", ob_)], dsem=f"d_out{ob_}")
        for name in list(S.cnt.keys()):
            if name.startswith("d_"):
                S.wait_tok("sp", (name, S.cnt[name]))

        with contextlib.ExitStack() as esem:
            for name in S.sem_names:
                S.sems[name] = esem.enter_context(nc.semaphore(name))
            with nc.Block() as block:
                @block.tensor
                def _(E):
                    S.emit("pe", E)

                @block.scalar
                def _(E):
                    S.emit("act", E)

                @block.vector
                def _(E):
                    S.emit("dve", E)

                @block.gpsimd
                def _(E):
                    S.emit("pool", E)

                @block.sync
                def _(E):
                    S.emit("sp", E)
    return nc


GA = [0, 3, 4, 7, 8, 11, 12, 15]
GB = [1, 2, 5, 6, 9, 10, 13, 14]


def _consts():
    p = np.arange(128)
    cf = np.zeros((128, 360), np.float32)
    cf[:, 0:128] = np.arange(128, dtype=np.float32)[None, :]
    cf[:, 128] = (10000.0 ** (-(2.0 * (p % 32)) / 64.0)).astype(np.float32)
    invm = np.zeros(128, np.float32)
    for q in range(64, 96):
        invm[q] = 10000.0 ** (-(2.0 * ((q - 64) % 16)) / 32.0)
    cf[:, 129] = invm
    cf[:, 160:288] = np.eye(128, dtype=np.float32)
    cf[:, 288:320] = (128.0 * np.arange(32, dtype=np.float32))[None, :]
    rc = np.zeros((128, 8, 4), np.float32)
    rc[:, :, 0] = p[:, None]
    rc[:, :, 1] = np.arange(8, dtype=np.float32)[None, :]
    rc[:, :, 2] = 1.0
    cf[:, 320:352] = rc.reshape(128, 32)
    cb = np.zeros((128, 768), np.float32)
    cb[:, 0:128] = np.eye(128)
    R = np.zeros((128, 128), np.float32)
    for m in range(128):
        if (m % 64) < 32:
            R[m + 32, m] = -1.0
        else:
            R[m - 32, m] = 1.0
    cb[:, 128:256] = R
    Rm = np.zeros((128, 128), np.float32)
    for m in range(64, 80):
        Rm[m + 16, m] = -1.0
    for m in range(80, 96):
        Rm[m - 16, m] = 1.0
    cb[:, 256:384] = Rm
    cb[:, 384:512] = (p[:, None] <= p[None, :]).astype(np.float32)
    cb[:, 512:640] = (p[:, None] < p[None, :]).astype(np.float32)
    cb[:, 640:768] = 1.0
    return cf, cb


def make_in_maps(inp, stage=STAGE_ALL, cores=range(8)):
    f32 = np.float32
    x = np.asarray(inp["x"], f32)
    pos = np.asarray(inp["positions"]).astype(np.int32)
    w_in = np.asarray(inp["w_in"], f32)[0]
    cf0, cb = _consts()
    def pm(w):
        K, N = w.shape
        return np.ascontiguousarray(w.reshape(K // 128, 128, N).transpose(1, 0, 2).reshape(128, (K // 128) * N))

    w_da = np.ascontiguousarray(np.stack([
        pm(np.concatenate([w_in[:, h * 128:(h + 1) * 128], w_in[:, 1024 + h * 128:1024 + (h + 1) * 128],
                           w_in[:, 2048 + h * 128:2048 + (h + 1) * 128]], axis=1)) for h in range(8)]))
    w_kr = np.zeros((2048, 128), f32)
    w_kr[:, 64:96] = w_in[:, 3840:3872]
    shared = {
        "cst_b": cb, "w_da": w_da,
        "w_mA": pm(w_in[:, 3072:3328]), "w_mB": pm(w_in[:, 3328:3584]), "w_mC": pm(w_in[:, 3584:3840]), "w_mD": pm(w_kr),
        "w_uq": pm(np.asarray(inp["mla_w_uq"], f32)[0]),
        "w_ukv": pm(np.asarray(inp["mla_w_ukv"], f32)[0]),
        "subln": np.ascontiguousarray(np.asarray(inp["da_subln"], f32)[0]),
        "lam": np.ascontiguousarray(np.asarray(inp["da_lambda"], f32)[0].reshape(256)),
    }
    if stage >= 4:
        ga = w_in[:, 3872:5920]
        gb = w_in[:, 5920:7968]
        shared["w_g"] = np.ascontiguousarray(np.stack([
            pm(np.concatenate([ga[:, j * 128:(j + 1) * 128], gb[:, j * 128:(j + 1) * 128]], axis=1)) for j in range(16)]))
        wba = np.asarray(inp["w_branch_a"], f32)[0]
        wbb = np.asarray(inp["w_branch_b"], f32)[0]
        shared["w_ba"] = np.ascontiguousarray(np.stack([pm(wba[:, j * 128:(j + 1) * 128]) for j in range(16)]))
        shared["w_bb"] = np.ascontiguousarray(np.stack([pm(wbb[:, j * 128:(j + 1) * 128]) for j in range(16)]))
    if stage >= 5:
        wout = np.asarray(inp["w_out"], f32)[0]
        shared["w_out"] = np.ascontiguousarray(np.stack([pm(wout[:, n * 512:(n + 1) * 512]) for n in range(4)]))
    if stage >= 6:
        shared["ffn_norm"] = np.ascontiguousarray(np.asarray(inp["ffn_norm"], f32)[0])
        shared["w_r"] = pm(np.concatenate([np.asarray(inp["w_group"], f32)[0], np.asarray(inp["w_router"], f32)[0]], axis=1))
        shared["r_bias"] = np.ascontiguousarray(np.concatenate([np.asarray(inp["b_group"], f32)[0], np.asarray(inp["b_router"], f32)[0]]))
    if stage >= 7:
        def pm3(w):
            E_, K, N = w.shape
            return np.ascontiguousarray(w.reshape(E_, K // 128, 128, N).transpose(0, 2, 1, 3).reshape(E_, 128, (K // 128) * N))
        shared["w_eg"] = pm3(np.asarray(inp["w_exp_gate"], f32)[0])
        shared["w_eu"] = pm3(np.asarray(inp["w_exp_up"], f32)[0])
        shared["w_ed"] = pm3(np.asarray(inp["w_exp_down"], f32)[0])
        shared["final_norm"] = np.ascontiguousarray(np.asarray(inp["final_norm"], f32))
    an = np.asarray(inp["attn_norm"], f32)[0].reshape(16, 128).T
    qn = np.asarray(inp["mla_q_norm"], f32)[0].reshape(4, 128).T
    kvn = np.asarray(inp["mla_kv_norm"], f32)[0].reshape(2, 128).T
    maps = []
    for c in cores:
        b, hf = divmod(c, 2)
        cf = cf0.copy()
        own = GA if hf == 0 else GB
        ctx = GB if hf == 0 else GA
        cf[:, 352:360] = np.array([1.0 if own[j] > ctx[j] else 0.0 for j in range(8)], np.float32)[None, :]
        cf[:, 132:148] = an
        cf[:, 148:152] = qn
        cf[:, 152:154] = kvn
        m = dict(shared)
        m["cst_f"] = cf
        xb = x[b].reshape(16, 128, 2048)
        pb = pos[b].reshape(16, 128)
        m["x_own"] = np.ascontiguousarray(xb[own].reshape(1024, 2048))
        m["x_ctx"] = np.ascontiguousarray(xb[ctx].reshape(1024, 2048))
        m["pos"] = np.ascontiguousarray(np.concatenate([pb[ctx].reshape(-1), pb[own].reshape(-1)]))
        maps.append(m)
    return maps


def kernel(**inp):
    nc = build_nc()
    maps = make_in_maps(inp)
    res = run_bass_kernel_spmd(nc, maps, core_ids=list(range(8)))
    out = np.zeros((4, 2048, 2048), np.float32)
    for c in range(8):
        b, hf = divmod(c, 2)
        own = GA if hf == 0 else GB
        o = res.results[c]["out"].reshape(8, 128, 2048)
        for j in range(8):
            out[b, own[j] * 128:(own[j] + 1) * 128] = o[j]
    return out
```

```python
import contextlib
import numpy as np
import concourse.bass as bass
import concourse.mybir as mybir
from concourse.bass_utils import run_bass_kernel_spmd

F32 = mybir.dt.float32
BF16 = mybir.dt.bfloat16
I32 = mybir.dt.int32
AF = mybir.ActivationFunctionType
ALU = mybir.AluOpType
AX = mybir.AxisListType

EPS = 1e-6
NEXP = 32
STAGE_ALL = 99


class Sched:
    ENG = ("pe", "act", "dve", "pool", "sp")

    def __init__(self, nc):
        self.nc = nc
        self.q = {e: [] for e in self.ENG}
        self.cnt = {}
        self.seen = {e: {} for e in self.ENG}
        self.lastw = {}
        self.readers = {}
        self.sems = {}
        self.sem_names = []

    def sem(self, name):
        if name not in self.cnt:
            self.cnt[name] = 0
            self.sem_names.append(name)
        return name

    def op(self, eng, fn, reads=(), writes=(), inc=True, dsem=None):
        psr = [r for r in reads if isinstance(r, tuple) and r[0] == "ps"]
        if psr:
            reads = [r for r in reads if r not in psr]
            writes = list(writes) + psr
        deps = {}

        def add(tok):
            if tok is None:
                return
            s, v = tok
            if deps.get(s, 0) < v:
                deps[s] = v

        for r in reads:
            add(self.lastw.get(r))
        for w in writes:
            add(self.lastw.get(w))
            for t in self.readers.get(w, ()):
                add(t)
        for s, v in deps.items():
            if s == "pe" and eng == "pe":
                continue
            if self.seen[eng].get(s, 0) >= v:
                continue
            self.seen[eng][s] = v
            self.q[eng].append(("wait", s, v))
        if dsem is not None:
            self.sem(dsem)
            self.cnt[dsem] += 16
            tok = (dsem, self.cnt[dsem])
            self.q[eng].append(("dma", fn, dsem))
        else:
            self.sem(eng)
            if inc:
                self.cnt[eng] += 1
                tok = (eng, self.cnt[eng])
                self.q[eng].append(("inc", fn, eng))
            else:
                tok = (eng, self.cnt[eng] + 1)
                self.q[eng].append(("noinc", fn, None))
        for r in reads:
            self.readers.setdefault(r, []).append(tok)
        for w in writes:
            self.lastw[w] = tok
            self.readers[w] = []
        return tok

    def barrier(self):
        for e in self.ENG:
            for s in list(self.cnt.keys()):
                if s.startswith("d_pc"):
                    continue
                if s != e and self.cnt[s] > 0:
                    self.wait_tok(e, (s, self.cnt[s]))

    def wait_tok(self, eng, tok):
        s, v = tok
        if self.seen[eng].get(s, 0) >= v:
            return
        self.seen[eng][s] = v
        self.q[eng].append(("wait", s, v))

    def emit(self, eng, E):
        for item in self.q[eng]:
            if item[0] == "wait":
                E.wait_ge(self.sems[item[1]], item[2])
            elif item[0] == "dma":
                item[1](E).then_inc(self.sems[item[2]], 16)
            elif item[0] == "inc":
                item[1](E).then_inc(self.sems[item[2]], 1)
            else:
                item[1](E)


def build_nc(stage=STAGE_ALL, debug=False):
    nc = bass.Bass("TRN2", target_bir_lowering=False)
    S = Sched(nc)

    def din(name, shape, dt=F32):
        return nc.dram_tensor(name, list(shape), dt, kind="ExternalInput").ap()

    def dout(name, shape, dt=F32):
        return nc.dram_tensor(name, list(shape), dt, kind="ExternalOutput").ap()

    x_own = din("x_own", [1024, 2048])
    x_ctx = din("x_ctx", [1024, 2048])
    pos_d = din("pos", [2048], I32)
    cstf_d = din("cst_f", [128, 360])
    cstb_d = din("cst_b", [128, 768])
    wda_d = din("w_da", [8, 128, 6144])
    wmA_d = din("w_mA", [128, 4096])
    wmB_d = din("w_mB", [128, 4096])
    wmC_d = din("w_mC", [128, 4096])
    wmD_d = din("w_mD", [128, 2048])
    wuq_d = din("w_uq", [128, 3072])
    wukv_d = din("w_ukv", [128, 3072])
    subln_d = din("subln", [128])
    lam_d = din("lam", [256])
    if stage >= 4:
        wg_d = din("w_g", [16, 128, 4096])
        wba_d = din("w_ba", [16, 128, 1024])
        wbb_d = din("w_bb", [16, 128, 1024])
    if stage >= 5:
        wout_d = din("w_out", [4, 128, 8192])
    if stage >= 6:
        ffn_d = din("ffn_norm", [2048])
        wr_d = din("w_r", [128, 576])
        rb_d = din("r_bias", [36])
    if stage >= 7:
        weg_d = din("w_eg", [NEXP, 128, 8192])
        weu_d = din("w_eu", [NEXP, 128, 8192])
        wed_d = din("w_ed", [NEXP, 128, 8192])
        fin_d = din("final_norm", [2048])
    out_d = dout("out", [1024, 2048])
    def ch(ap2, b):
        return ap2.rearrange("p (a b) -> p a b", b=b)

    def fl(t3):
        return t3[:].rearrange("p c n -> p (c n)")

    PC = {"i": 0, "list": []}
    if stage >= 7:
        hf_s = nc.dram_tensor("hf_s", [1025, 2048], BF16, kind="Internal").ap()
        y_s = nc.dram_tensor("y_s", [4097, 2048], BF16, kind="Internal").ap()
        weg_s = nc.dram_tensor("weg_s", [NEXP, 128, 8192], BF16, kind="Internal").ap()
        weu_s = nc.dram_tensor("weu_s", [NEXP, 128, 8192], BF16, kind="Internal").ap()
        wed_s = nc.dram_tensor("wed_s", [NEXP, 128, 8192], BF16, kind="Internal").ap()
        for e in range(NEXP):
            if e % 6 == 5:
                continue
            PC["list"].append((e, "g", ch(weg_d[e], 2048), ch(weg_s[e], 2048)))
            PC["list"].append((e, "u", ch(weu_d[e], 2048), ch(weu_s[e], 2048)))
            PC["list"].append((e, "d", ch(wed_d[e], 2048), ch(wed_s[e], 2048)))

    def precast(n, after=()):
        for _ in range(n):
            if PC["i"] >= len(PC["list"]):
                return
            e, kind, src_ap, dst_ap = PC["list"][PC["i"]]
            PC["i"] += 1
            S.op("pool", lambda E, src_ap=src_ap, dst_ap=dst_ap: E.dma_start(out=dst_ap, in_=src_ap),
                 reads=list(after), writes=[("pc", e, kind)], dsem=f"d_pc{e}")
    dbg = {}
    if debug:
        dbg["oaT"] = dout("dbg_oaT", [128, 8 * 1024], BF16)
        dbg["obT"] = dout("dbg_obT", [128, 8 * 1024], BF16)
        dbg["xmid"] = dout("dbg_xmid", [128, 8 * 2048])

    es = contextlib.ExitStack()
    with es:
        def sb(name, shape, dt):
            return es.enter_context(nc.sbuf_tensor(name, list(shape), dt))

        ps = [es.enter_context(nc.psum_tensor(f"ps{i}", [128, 512], F32)) for i in range(8)]

        def psb(i):
            return ps[i][:].bitcast(BF16)

        cstf = sb("cstf", [128, 360], F32)
        cstb = sb("cstb", [128, 768], BF16)
        iota_f = cstf[:, 0:128]
        invf_da = cstf[:, 128:129]
        invf_m = cstf[:, 129:130]
        vis = cstf[:, 352:360]
        gattn = cstf[:, 132:148]
        gq = cstf[:, 148:152]
        gkv = cstf[:, 152:154]
        ident_f = cstf[:, 160:288]
        ebase = cstf[:, 288:320]
        Rc = cstf[:, 320:352]
        ident_b = cstb[:, 0:128]
        R_da = cstb[:, 128:256]
        R_m = cstb[:, 256:384]
        cmask = cstb[:, 384:512]
        ustrict = cstb[:, 512:640]
        ones_b = cstb[:, 640:768]

        hTc = sb("hTc", [128, 16, 1024], BF16)
        hTo = sb("hTo", [128, 16, 1024], BF16)
        bufO = sb("bufO", [128, 16384], BF16)
        oaT = bufO[:, 0:8192].rearrange("p (h t) -> p h t", t=1024)
        obT = bufO[:, 8192:16384].rearrange("p (h t) -> p h t", t=1024)
        xmA = bufO[:].bitcast(F32).rearrange("p (a b) -> p a b", b=2048)
        xmB = hTo[:].rearrange("p a b -> p (a b)").bitcast(F32).rearrange("p (a b) -> p a b", b=2048)
        hfb = hTc[:].rearrange("p a b -> p (a b)").rearrange("p (t f) -> p t f", f=2048)

        def hTx(c, t0, n):
            if t0 < 1024:
                return hTc[:, c, t0:t0 + n]
            return hTo[:, c, t0 - 1024:t0 - 1024 + n]

        def xm(t):
            return (xmA if t < 4 else xmB)[:, t % 4, :]
        small = sb("small", [128, 64], F32)
        lamv = small[:, 0:1]
        neglam = small[:, 1:2]

        S.op("sp", lambda E: E.dma_start(out=cstf[:], in_=cstf_d), writes=["cstf"], dsem="d_cf")
        S.op("pool", lambda E: E.dma_start(out=cstb[:], in_=cstb_d), writes=["cstb"], dsem="d_cb")

        HT_ALL = [("hT", t) for t in range(16)]
        HT_OWN = [("hT", t) for t in range(8, 16)]
        AB = {}
        state = {"sbank": 0, "pt": 0, "rope": 0, "onb": 0}
        NPT = 4

        def alloc_attn_bufs(es_, sfx):
            def a(name, shape, dt):
                return es_.enter_context(nc.sbuf_tensor(name + sfx, list(shape), dt))
            AB["qT"] = a("qT", [128, 1024], BF16)
            AB["kT"] = a("kT", [128, 2048], BF16)
            AB["Vt"] = a("Vt", [128, 16, 130], BF16)
            AB["xq"] = [a(f"xq{i}", [128, 512], BF16) for i in range(2)]
            AB["t1"] = [a(f"t1_{i}", [128, 512], F32) for i in range(2)]
            AB["t2"] = [a(f"t2_{i}", [128, 512], F32) for i in range(2)]
            AB["pt"] = [a(f"pt{i}", [128, 512], BF16) for i in range(NPT)]
            AB["onb"] = [a(f"onb{i}", [128, 128], BF16) for i in range(2)]
            Vt_ = AB["Vt"]
            S.op("dve", lambda E: E.memset(Vt_[:, :, 128:130], 1.0), writes=["Vones"])

        def rope_evac(psrc_bank, nrows, dst_ap, tok0, ctab, stab, Rm, dst_res, tabres):
            i = state["rope"] % 2
            state["rope"] += 1
            xq, t1, t2 = AB["xq"][i], AB["t1"][i], AB["t2"][i]
            S.op("act", lambda E: E.activation(out=xq[0:nrows, :], in_=ps[psrc_bank][0:nrows, :], func=AF.Copy),
                 reads=[("ps", psrc_bank)], writes=[("xq", i)])
            S.op("dve", lambda E: E.tensor_tensor(out=t1[0:nrows, :], in0=ps[psrc_bank][0:nrows, :],
                                                  in1=ctab[0:nrows, tok0:tok0 + 512], op=ALU.mult),
                 reads=[("ps", psrc_bank), tabres], writes=[("t1", i)])

            def part_b():
                S.op("pe", lambda E: E.matmul(ps[3][0:nrows, :], lhsT=Rm[0:nrows, 0:nrows], rhs=xq[0:nrows, :], start=True, stop=True),
                     reads=[("xq", i), "cstb"], writes=[("ps", 3)])
                S.op("dve", lambda E: E.tensor_tensor(out=t2[0:nrows, :], in0=ps[3][0:nrows, :],
                                                      in1=stab[0:nrows, tok0:tok0 + 512], op=ALU.mult),
                     reads=[("ps", 3), tabres], writes=[("t2", i)])
                S.op("dve", lambda E: E.tensor_tensor(out=dst_ap, in0=t1[0:nrows, :], in1=t2[0:nrows, :], op=ALU.add),
                     reads=[("t1", i), ("t2", i)], writes=[dst_res])
            return part_b

        class Pipe:
            def __init__(self):
                self.pend = None

            def push(self, fn):
                if self.pend is not None:
                    self.pend()
                self.pend = fn

            def flush(self):
                if self.pend is not None:
                    self.pend()
                self.pend = None

        pipe = Pipe()

        def attn_unit(K, p0, scale, g, on_done):
            qT, kT, Vt, pt = AB["qT"], AB["kT"], AB["Vt"], AB["pt"]
            qt0 = 4 * g
            ktiles = list(range(4 * g + 4)) + [8 + j for j in range(4 * g + 4)]
            DEPTH = 2
            info = {}

            def emit_s(kt):
                visk = None
                if kt < 8:
                    first_q, diag, bias = max(qt0, kt), False, 0.0
                    if kt >= qt0:
                        visk = vis[:, kt:kt + 1]
                else:
                    j = kt - 8
                    first_q, diag, bias = max(qt0, j), (j >= qt0), 0.0
                ncol = (qt0 + 4 - first_q) * 128
                sbk = state["sbank"] % 3
                state["sbank"] += 1
                S.op("pe", lambda E: E.matmul(
                    ps[sbk][:, 0:ncol], lhsT=kT[p0:p0 + K, kt * 128:(kt + 1) * 128],
                    rhs=qT[p0:p0 + K, first_q * 128:first_q * 128 + ncol], start=True, stop=True),
                    reads=["kT", "qT"], writes=[("ps", sbk)])
                sl = state["pt"] % NPT
                state["pt"] += 1
                S.op("act", lambda E: E.activation(
                    out=pt[sl][:, 0:ncol], in_=ps[sbk][:, 0:ncol], func=AF.Exp, bias=bias, scale=float(scale)),
                    reads=[("ps", sbk), "cstf"], writes=[("pt", sl)])
                if diag:
                    S.op("dve", lambda E: E.tensor_tensor(out=pt[sl][:, 0:128], in0=pt[sl][:, 0:128], in1=cmask, op=ALU.mult),
                         reads=[("pt", sl), "cstb"], writes=[("pt", sl)])
                if visk is not None:
                    S.op("dve", lambda E: E.tensor_scalar(out=pt[sl][:, 0:128], in0=pt[sl][:, 0:128], scalar1=visk, scalar2=None, op0=ALU.mult),
                         reads=[("pt", sl), "cstf"], writes=[("pt", sl)])
                info[kt] = (first_q, sl)

            def emit_pv(kt):
                first_q, sl = info[kt]
                for i in range(first_q, qt0 + 4):
                    ob = 4 + (i - qt0)
                    start = (kt == 0)
                    stop = (kt == 8 + i)
                    last = (i == qt0 + 3)
                    S.op("pe", lambda E, ob=ob, i=i, start=start, stop=stop: E.matmul(
                        ps[ob][:, 0:129], lhsT=pt[sl][:, (i - first_q) * 128:(i - first_q + 1) * 128],
                        rhs=Vt[:, kt, 0:129], start=start, stop=stop),
                        reads=[("pt", sl), "Vt", "Vones"], writes=[("ps", ob)], inc=(stop or last))

            n = len(ktiles)
            for idx in range(n + DEPTH):
                if idx < n:
                    emit_s(ktiles[idx])
                if idx - DEPTH >= 0:
                    emit_pv(ktiles[idx - DEPTH])
            for i in range(4):
                on_done(qt0 + i, 4 + i)

        def v_evac(bank, tq):
            Vt = AB["Vt"]
            S.op("act", lambda E: E.activation(
                out=Vt[:, tq * 4:(tq + 1) * 4, 0:128], in_=ps[bank][:, :].rearrange("p (a b) -> p a b", b=128), func=AF.Copy),
                reads=[("ps", bank)], writes=["Vt"])

        with contextlib.ExitStack() as es1:
            def sb1(name, shape, dt):
                return es1.enter_context(nc.sbuf_tensor(name, list(shape), dt))

            cos_m = sb1("cos_m", [128, 2048], BF16)
            sin_m = sb1("sin_m", [128, 2048], BF16)
            subw = sb1("subw", [128, 128], F32)
            S.op("sp", lambda E: E.dma_start(out=subw[:], in_=subln_d.partition_broadcast(128)),
                 writes=["subw"], dsem="d_sub")
            S.op("dve", lambda E: E.tensor_scalar(out=subw[:], in0=subw[:], scalar1=0.8, scalar2=None, op0=ALU.mult),
                 reads=["subw"], writes=["subw"])

            with contextlib.ExitStack() as esda:
                cos_da = esda.enter_context(nc.sbuf_tensor("cos_da", [128, 2048], BF16))
                sin_da = esda.enter_context(nc.sbuf_tensor("sin_da", [128, 2048], BF16))
                lamt = esda.enter_context(nc.sbuf_tensor("lamt", [128, 256], F32))
                S.op("sp", lambda E: E.dma_start(out=lamt[:], in_=lam_d.partition_broadcast(128)),
                     writes=["lamt"], dsem="d_lam")
                S.op("dve", lambda E: E.tensor_tensor(out=lamt[:, 0:64], in0=lamt[:, 0:64], in1=lamt[:, 64:128], op=ALU.mult),
                     reads=["lamt"], writes=["lamt"])
                S.op("dve", lambda E: E.tensor_tensor(out=lamt[:, 128:192], in0=lamt[:, 128:192], in1=lamt[:, 192:256], op=ALU.mult),
                     reads=["lamt"], writes=["lamt"])
                S.op("dve", lambda E: E.tensor_reduce(out=small[:, 2:3], in_=lamt[:, 0:64], axis=AX.X, op=ALU.add),
                     reads=["lamt"], writes=["sm2"])
                S.op("dve", lambda E: E.tensor_reduce(out=small[:, 3:4], in_=lamt[:, 128:192], axis=AX.X, op=ALU.add),
                     reads=["lamt"], writes=["sm3"])
                S.op("act", lambda E: E.activation(out=small[:, 4:6], in_=small[:, 2:4], func=AF.Exp),
                     reads=["sm2", "sm3"], writes=["sm4"])
                S.op("dve", lambda E: E.tensor_tensor(out=small[:, 6:7], in0=small[:, 4:5], in1=small[:, 5:6], op=ALU.subtract),
                     reads=["sm4"], writes=["sm6"])
                S.op("dve", lambda E: E.tensor_scalar(out=lamv, in0=small[:, 6:7], scalar1=0.2, scalar2=None, op0=ALU.add),
                     reads=["sm6"], writes=["lamv"])
                S.op("dve", lambda E: E.tensor_scalar(out=neglam, in0=lamv, scalar1=-1.0, scalar2=None, op0=ALU.mult),
                     reads=["lamv"], writes=["neglam"])

                with contextlib.ExitStack() as es0:
                    posi = es0.enter_context(nc.sbuf_tensor("posi", [128, 2048], I32))
                    posf = es0.enter_context(nc.sbuf_tensor("posf", [128, 2048], F32))
                    ang = es0.enter_context(nc.sbuf_tensor("ang", [128, 2048], F32))
                    kk = es0.enter_context(nc.sbuf_tensor("kk", [128, 2048], F32))
                    ki = es0.enter_context(nc.sbuf_tensor("ki", [128, 2048], I32))
                    S.op("sp", lambda E: E.dma_start(out=posi[:], in_=pos_d.partition_broadcast(128)),
                         writes=["posi"], dsem="d_pos")
                    S.op("dve", lambda E: E.tensor_copy(out=posf[:], in_=posi[:]), reads=["posi"], writes=["posf"])
                    TWO_PI = 2.0 * np.pi
                    for (invf, ctab, stab, nm) in ((invf_da, cos_da, sin_da, "da"), (invf_m, cos_m, sin_m, "m")):
                        for (shift, tab) in ((0.0, stab), (np.pi / 2.0, ctab)):
                            S.op("dve", lambda E, invf=invf, shift=shift: E.tensor_scalar(
                                out=ang[:], in0=posf[:], scalar1=invf, scalar2=float(shift), op0=ALU.mult, op1=ALU.add),
                                reads=["posf", "cstf"], writes=["ang"])
                            S.op("dve", lambda E: E.tensor_scalar(
                                out=kk[:], in0=ang[:], scalar1=float(1.0 / TWO_PI), scalar2=None, op0=ALU.mult),
                                reads=["ang"], writes=["kk"])
                            S.op("dve", lambda E: E.tensor_copy(out=ki[:], in_=kk[:]), reads=["kk"], writes=["ki"])
                            S.op("dve", lambda E: E.tensor_copy(out=kk[:], in_=ki[:]), reads=["ki"], writes=["kk"])
                            S.op("dve", lambda E: E.scalar_tensor_tensor(
                                out=ang[:], in0=kk[:], scalar=float(-TWO_PI), in1=ang[:], op0=ALU.mult, op1=ALU.add),
                                reads=["kk", "ang"], writes=["ang"])
                            S.op("dve", lambda E: E.tensor_scalar(
                                out=kk[:], in0=ang[:], scalar1=float(np.pi), scalar2=float(TWO_PI), op0=ALU.is_gt, op1=ALU.mult),
                                reads=["ang"], writes=["kk"])
                            S.op("dve", lambda E: E.tensor_tensor(out=ang[:], in0=ang[:], in1=kk[:], op=ALU.subtract),
                                 reads=["ang", "kk"], writes=["ang"])
                            S.op("dve", lambda E: E.tensor_scalar(
                                out=kk[:], in0=ang[:], scalar1=float(-np.pi), scalar2=float(TWO_PI), op0=ALU.is_lt, op1=ALU.mult),
                                reads=["ang"], writes=["kk"])
                            S.op("dve", lambda E: E.tensor_tensor(out=ang[:], in0=ang[:], in1=kk[:], op=ALU.add),
                                 reads=["ang", "kk"], writes=["ang"])
                            S.op("dve", lambda E: E.tensor_scalar(
                                out=ang[:], in0=ang[:], scalar1=3.141592, scalar2=-3.141592, op0=ALU.min, op1=ALU.max),
                                reads=["ang"], writes=["ang"])
                            S.op("act", lambda E, tab=tab: E.activation(out=tab[:], in_=ang[:], func=AF.Sin),
                                 reads=["ang"], writes=["tab_" + nm])

                    xst = [es0.enter_context(nc.sbuf_tensor(f"xst{i}", [128, 2048], F32)) for i in range(2)]
                    xsb = [es0.enter_context(nc.sbuf_tensor(f"xsb{i}", [128, 2048], BF16)) for i in range(2)]
                    for t in range(16):
                        b = t % 2
                        src = x_ctx if t < 8 else x_own
                        r0 = (t % 8) * 128
                        S.op("sp", lambda E, b=b, src=src, r0=r0: E.dma_start(out=xst[b][:], in_=src[r0:r0 + 128, :]),
                             writes=[("xst", b)], dsem=f"d_x{b}")
                        ss = small[:, 8 + b:9 + b]
                        S.op("act", lambda E, b=b, ss=ss: E.activation(out=xsb[b][:], in_=xst[b][:], func=AF.Square, accum_out=ss),
                             reads=[("xst", b)], writes=[("xsb", b), ("ss", b)])
                        S.op("act", lambda E, ss=ss: E.activation(out=ss, in_=ss, func=AF.Ln, bias=EPS, scale=1.0 / 2048.0),
                             reads=[("ss", b)], writes=[("ss", b)])
                        S.op("act", lambda E, ss=ss: E.activation(out=ss, in_=ss, func=AF.Exp, scale=-0.5),
                             reads=[("ss", b)], writes=[("ss", b)])
                        S.op("dve", lambda E, b=b, ss=ss: E.tensor_scalar(out=xsb[b][:], in0=xst[b][:], scalar1=ss, scalar2=None, op0=ALU.mult),
                             reads=[("xst", b), ("ss", b)], writes=[("xsb", b)])
                        for hb in range(2):
                            bank = 2 * b + hb
                            for j in range(8):
                                c = hb * 8 + j
                                S.op("pe", lambda E, bank=bank, j=j, c=c, b=b: E.transpose(
                                    out=psb(bank)[:, j * 128:(j + 1) * 128], in_=xsb[b][:, c * 128:(c + 1) * 128], identity=ident_b),
                                    reads=[("xsb", b), "cstb"], writes=[("ps", bank)], inc=(j == 7))
                            S.op("dve", lambda E, bank=bank, hb=hb, t=t: E.tensor_tensor(
                                out=(hTc if t < 8 else hTo)[:, hb * 8:(hb + 1) * 8, (t % 8) * 128:(t % 8 + 1) * 128],
                                in0=psb(bank).rearrange("p (a b) -> p a b", b=128),
                                in1=gattn[:, hb * 8:(hb + 1) * 8].unsqueeze(2).broadcast_to([128, 8, 128]),
                                op=ALU.mult),
                                reads=[("ps", bank), "cstf"], writes=[("hT", t)])
                S.barrier()

                with contextlib.ExitStack() as es2:
                    alloc_attn_bufs(es2, "_a")
                    qT, kT = AB["qT"], AB["kT"]
                    acc = es2.enter_context(nc.sbuf_tensor("acc", [128, 4, 128], F32))
                    ocomb = es2.enter_context(nc.sbuf_tensor("ocomb", [128, 128], F32))
                    osq = es2.enter_context(nc.sbuf_tensor("osq", [128, 128], F32))
                    wda = [es2.enter_context(nc.sbuf_tensor(f"wda{i}", [128, 16, 384], BF16)) for i in range(2)]
                    onb = AB["onb"]
                    for h in range(8 if stage >= 2 else 0):
                        wb = h % 2
                        S.op("pool", lambda E, wb=wb, h=h: E.dma_start(out=ch(fl(wda[wb]), 2048), in_=ch(wda_d[h], 2048)),
                             writes=[("wda", wb)], dsem=f"d_wda{wb}")
                        precast(5)
                        for tg in range(2):
                            bank = tg % 2
                            for c in range(16):
                                S.op("pe", lambda E, bank=bank, c=c, wb=wb, tg=tg: E.matmul(
                                    ps[bank][:, :], lhsT=wda[wb][:, c, 0:128], rhs=hTo[:, c, tg * 512:(tg + 1) * 512],
                                    start=(c == 0), stop=(c == 15)),
                                    reads=[("wda", wb)] + HT_OWN, writes=[("ps", bank)], inc=(c == 15))
                            pipe.push(rope_evac(bank, 128, qT[:, tg * 512:(tg + 1) * 512], 1024 + tg * 512, cos_da, sin_da, R_da, "qT", "tab_da"))
                        for tg in range(4):
                            bank = tg % 2
                            for c in range(16):
                                S.op("pe", lambda E, bank=bank, c=c, wb=wb, tg=tg: E.matmul(
                                    ps[bank][:, :], lhsT=wda[wb][:, c, 128:256], rhs=hTx(c, tg * 512, 512),
                                    start=(c == 0), stop=(c == 15)),
                                    reads=[("wda", wb)] + HT_ALL, writes=[("ps", bank)], inc=(c == 15))
                            pipe.push(rope_evac(bank, 128, kT[:, tg * 512:(tg + 1) * 512], tg * 512, cos_da, sin_da, R_da, "kT", "tab_da"))
                        pipe.flush()
                        for tq in range(4):
                            bank = tq % 2
                            for ti in range(4):
                                tt = tq * 4 + ti
                                for c in range(16):
                                    S.op("pe", lambda E, bank=bank, ti=ti, tt=tt, c=c, wb=wb: E.matmul(
                                        ps[bank][:, ti * 128:(ti + 1) * 128], lhsT=hTx(c, tt * 128, 128),
                                        rhs=wda[wb][:, c, 256:384], start=(c == 0), stop=(c == 15)),
                                        reads=[("wda", wb)] + HT_ALL, writes=[("ps", bank)], inc=(c == 15))
                            v_evac(bank, tq)
                        for g in range(2):
                            def done0(qtile, ob):
                                i = qtile % 4
                                sm = small[:, 16 + i:17 + i]
                                S.op("dve", lambda E: E.reciprocal(out=sm, in_=ps[ob][:, 128:129]), reads=[("ps", ob)], writes=[("r1", i)])
                                S.op("dve", lambda E: E.tensor_scalar(out=acc[:, i, :], in0=ps[ob][:, 0:128], scalar1=sm, scalar2=None, op0=ALU.mult),
                                     reads=[("ps", ob), ("r1", i)], writes=[("acc", i)])

                            def done1(qtile, ob, h=h):
                                i = qtile % 4
                                sm = small[:, 20 + i:21 + i]
                                sm2 = small[:, 24 + i:25 + i]
                                S.op("dve", lambda E: E.reciprocal(out=sm, in_=ps[ob][:, 128:129]), reads=[("ps", ob)], writes=[("r2", i)])
                                S.op("dve", lambda E: E.tensor_tensor(out=sm, in0=sm, in1=neglam, op=ALU.mult),
                                     reads=[("r2", i), "neglam"], writes=[("r2", i)])
                                S.op("dve", lambda E: E.scalar_tensor_tensor(out=ocomb[:], in0=ps[ob][:, 0:128], scalar=sm, in1=acc[:, i, :],
                                                                             op0=ALU.mult, op1=ALU.add),
                                     reads=[("ps", ob), ("r2", i), ("acc", i)], writes=["ocomb"])
                                S.op("dve", lambda E: E.tensor_tensor(out=osq[:], in0=ocomb[:], in1=ocomb[:], op=ALU.mult),
                                     reads=["ocomb"], writes=["osq"])
                                S.op("dve", lambda E: E.tensor_reduce(out=sm2, in_=osq[:], axis=AX.X, op=ALU.add),
                                     reads=["osq"], writes=[("ssq", i)])
                                S.op("act", lambda E: E.activation(out=sm2, in_=sm2, func=AF.Ln, bias=EPS, scale=1.0 / 128.0),
                                     reads=[("ssq", i)], writes=[("ssq", i)])
                                S.op("act", lambda E: E.activation(out=sm2, in_=sm2, func=AF.Exp, scale=-0.5),
                                     reads=[("ssq", i)], writes=[("ssq", i)])
                                ob_i = state["onb"] % 2
                                state["onb"] += 1
                                S.op("dve", lambda E: E.scalar_tensor_tensor(out=onb[ob_i][:], in0=ocomb[:], scalar=sm2, in1=subw[:],
                                                                             op0=ALU.mult, op1=ALU.mult),
                                     reads=["ocomb", ("ssq", i), "subw"], writes=[("onb", ob_i)])
                                S.op("pe", lambda E: E.transpose(out=psb(3)[:, 0:128], in_=onb[ob_i][:], identity=ident_b),
                                     reads=[("onb", ob_i), "cstb"], writes=[("ps", 3)])
                                S.op("act", lambda E: E.activation(out=oaT[:, h, qtile * 128:(qtile + 1) * 128], in_=psb(3)[:, 0:128], func=AF.Copy),
                                     reads=[("ps", 3)], writes=[("oaT", h)])

                            attn_unit(64, 0, 0.125, g, done0)
                            attn_unit(64, 64, 0.125, g, done1)
                S.barrier()
            S.barrier()

            if stage >= 3:
                with contextlib.ExitStack() as es3:
                    def sb3(name, shape, dt):
                        return es3.enter_context(nc.sbuf_tensor(name, list(shape), dt))
                    alloc_attn_bufs(es3, "_b")
                    qT, kT = AB["qT"], AB["kT"]
                    onb = AB["onb"]
                    cqn = sb3("cqn", [128, 4, 1024], BF16)
                    ckvn = sb3("ckvn", [128, 2, 2048], BF16)
                    krT = sb3("krT", [128, 2048], BF16)
                    wuq = sb3("wuq", [128, 4, 768], BF16)
                    wukv = sb3("wukv", [128, 2, 1536], BF16)
                    cf = sb3("cf", [128, 4, 512], BF16)
                    csq = sb3("csq", [128, 4, 512], BF16)
                    rbc = sb3("rbc", [128, 512], F32)
                    S.op("pool", lambda E: E.dma_start(out=ch(fl(wuq), 1024), in_=ch(wuq_d, 1024)),
                         writes=["wuq"], dsem="d_wuq")
                    S.op("pool", lambda E: E.dma_start(out=ch(fl(wukv), 1024), in_=ch(wukv_d, 1024)),
                         writes=["wukv"], dsem="d_wukv")
                    with contextlib.ExitStack() as es3b:
                        wmA = es3b.enter_context(nc.sbuf_tensor("wmA", [128, 16, 256], BF16))
                        wmB = es3b.enter_context(nc.sbuf_tensor("wmB", [128, 16, 256], BF16))

                        def latent_norm(nchunk, wts, tok0, gvec, dst, dst_res, width):
                            for k in range(nchunk):
                                wt, off, wres = wts[k]
                                bank = k % 2
                                for c in range(16):
                                    S.op("pe", lambda E, wt=wt, off=off, c=c, bank=bank: E.matmul(
                                        ps[bank][:, :], lhsT=wt[:, c, off:off + 128], rhs=hTx(c, tok0, 512),
                                        start=(c == 0), stop=(c == 15)),
                                        reads=[wres] + HT_ALL, writes=[("ps", bank)], inc=(c == 15))
                                S.op("act", lambda E, k=k, bank=bank: E.activation(out=cf[:, k, :], in_=ps[bank][:, :], func=AF.Copy),
                                     reads=[("ps", bank)], writes=[("cf", k)])
                                S.op("act", lambda E, k=k, bank=bank: E.activation(out=csq[:, k, :], in_=ps[bank][:, :], func=AF.Square),
                                     reads=[("ps", bank)], writes=[("csq", k)])
                            for k in range(nchunk):
                                S.op("pe", lambda E, k=k: E.matmul(ps[2][:, :], lhsT=ones_b, rhs=csq[:, k, :], start=(k == 0), stop=(k == nchunk - 1)),
                                     reads=[("csq", k), "cstb"], writes=[("ps", 2)], inc=(k == nchunk - 1))
                            S.op("act", lambda E: E.activation(out=rbc[:], in_=ps[2][:, :], func=AF.Ln, bias=EPS, scale=1.0 / width),
                                 reads=[("ps", 2)], writes=["rbc"])
                            S.op("act", lambda E: E.activation(out=rbc[:], in_=rbc[:], func=AF.Exp, scale=-0.5),
                                 reads=["rbc"], writes=["rbc"])
                            for k in range(nchunk):
                                S.op("dve", lambda E, k=k: E.scalar_tensor_tensor(
                                    out=dst(k), in0=cf[:, k, :], scalar=gvec[:, k:k + 1], in1=rbc[:], op0=ALU.mult, op1=ALU.mult),
                                    reads=[("cf", k), "rbc", "cstf"], writes=[dst_res])

                        S.op("pool", lambda E: E.dma_start(out=ch(fl(wmA), 2048), in_=ch(wmA_d, 2048)),
                             writes=["wmA"], dsem="d_wmA")
                        S.op("pool", lambda E: E.dma_start(out=ch(fl(wmB), 2048), in_=ch(wmB_d, 2048)),
                             writes=["wmB"], dsem="d_wmB")
                        precast(3)
                        cq_w = [(wmA, 0, "wmA"), (wmA, 128, "wmA"), (wmB, 0, "wmB"), (wmB, 128, "wmB")]
                        for tg in range(2):
                            latent_norm(4, cq_w, 1024 + tg * 512, gq, lambda k, tg=tg: cqn[:, k, tg * 512:(tg + 1) * 512], "cqn", 512.0)
                        S.op("pool", lambda E: E.dma_start(out=ch(fl(wmA), 2048), in_=ch(wmC_d, 2048)),
                             writes=["wmA"], dsem="d_wmA")
                        S.op("pool", lambda E: E.dma_start(out=wmB[:, :, 0:128], in_=wmD_d.rearrange("p (c n) -> p c n", n=128)),
                             writes=["wmB"], dsem="d_wmB")
                        ckv_w = [(wmA, 0, "wmA"), (wmA, 128, "wmA")]
                        for tg in range(4):
                            latent_norm(2, ckv_w, tg * 512, gkv, lambda k, tg=tg: ckvn[:, k, tg * 512:(tg + 1) * 512], "ckvn", 256.0)
                        for tg in range(4):
                            bank = tg % 2
                            for c in range(16):
                                S.op("pe", lambda E, c=c, bank=bank, tg=tg: E.matmul(
                                    ps[bank][:, :], lhsT=wmB[:, c, 0:128], rhs=hTx(c, tg * 512, 512),
                                    start=(c == 0), stop=(c == 15)),
                                    reads=["wmB"] + HT_ALL, writes=[("ps", bank)], inc=(c == 15))
                            pipe.push(rope_evac(bank, 128, krT[:, tg * 512:(tg + 1) * 512], tg * 512, cos_m, sin_m, R_m, "krT", "tab_m"))
                        pipe.flush()
                    S.barrier()
                    for h in range(8):
                        precast(3, after=([("obT", h - 1)] if h >= 1 else []))
                        for tg in range(2):
                            bank = tg % 2
                            for c in range(4):
                                S.op("pe", lambda E, c=c, bank=bank, tg=tg, h=h: E.matmul(
                                    ps[bank][0:96, :], lhsT=wuq[:, c, h * 96:(h + 1) * 96], rhs=cqn[:, c, tg * 512:(tg + 1) * 512],
                                    start=(c == 0), stop=(c == 3)),
                                    reads=["wuq", "cqn"], writes=[("ps", bank)], inc=(c == 3))
                            pipe.push(rope_evac(bank, 96, qT[0:96, tg * 512:(tg + 1) * 512], 1024 + tg * 512, cos_m, sin_m, R_m, "qT", "tab_m"))
                        pipe.flush()
                        for tg in range(4):
                            bank = tg % 2
                            for c in range(2):
                                S.op("pe", lambda E, c=c, bank=bank, tg=tg, h=h: E.matmul(
                                    ps[bank][0:64, :], lhsT=wukv[:, c, h * 192:h * 192 + 64], rhs=ckvn[:, c, tg * 512:(tg + 1) * 512],
                                    start=(c == 0), stop=(c == 1)),
                                    reads=["wukv", "ckvn"], writes=[("ps", bank)], inc=(c == 1))
                            S.op("act", lambda E, bank=bank, tg=tg: E.activation(out=kT[0:64, tg * 512:(tg + 1) * 512], in_=ps[bank][0:64, :], func=AF.Copy),
                                 reads=[("ps", bank)], writes=["kT"])
                        S.op("dve", lambda E: E.tensor_copy(out=kT[64:96, :], in_=krT[64:96, :]), reads=["krT"], writes=["kT"])
                        for tq in range(4):
                            bank = tq % 2
                            for ti in range(4):
                                tt = tq * 4 + ti
                                for c in range(2):
                                    S.op("pe", lambda E, bank=bank, ti=ti, tt=tt, c=c, h=h: E.matmul(
                                        ps[bank][:, ti * 128:(ti + 1) * 128], lhsT=ckvn[:, c, tt * 128:(tt + 1) * 128],
                                        rhs=wukv[:, c, h * 192 + 64:h * 192 + 192], start=(c == 0), stop=(c == 1)),
                                        reads=["wukv", "ckvn"], writes=[("ps", bank)], inc=(c == 1))
                            v_evac(bank, tq)
                        for g in range(2):
                            def donem(qtile, ob, h=h):
                                i = qtile % 4
                                sm = small[:, 28 + i:29 + i]
                                S.op("dve", lambda E: E.reciprocal(out=sm, in_=ps[ob][:, 128:129]), reads=[("ps", ob)], writes=[("r3", i)])
                                ob_i = state["onb"] % 2
                                state["onb"] += 1
                                S.op("dve", lambda E: E.tensor_scalar(out=onb[ob_i][:], in0=ps[ob][:, 0:128], scalar1=sm, scalar2=None, op0=ALU.mult),
                                     reads=[("ps", ob), ("r3", i)], writes=[("onb", ob_i)])
                                S.op("pe", lambda E: E.transpose(out=psb(3)[:, 0:128], in_=onb[ob_i][:], identity=ident_b),
                                     reads=[("onb", ob_i), "cstb"], writes=[("ps", 3)])
                                S.op("act", lambda E: E.activation(out=obT[:, h, qtile * 128:(qtile + 1) * 128], in_=psb(3)[:, 0:128], func=AF.Copy),
                                     reads=[("ps", 3)], writes=[("obT", h)])
                            attn_unit(96, 0, float(96.0 ** -0.5), g, donem)
                S.barrier()
        S.barrier()

        OAT = [("oaT", h) for h in range(8)]
        OBT = [("obT", h) for h in range(8)]
        if debug:
            S.op("sp", lambda E: E.dma_start(out=dbg["oaT"], in_=bufO[:, 0:8192]), reads=OAT, dsem="d_dbg2")
            S.op("sp", lambda E: E.dma_start(out=dbg["obT"], in_=bufO[:, 8192:16384]), reads=OBT, dsem="d_dbg3")

        MIX = [("hT", t) for t in range(8)]
        if stage >= 4:
            with contextlib.ExitStack() as es4:
                def sb4(name, shape, dt):
                    return es4.enter_context(nc.sbuf_tensor(name, list(shape), dt))
                wgt = [sb4(f"wgt{i}", [128, 16, 256], BF16) for i in range(2)]
                wat = [sb4(f"wat{i}", [128, 8, 128], BF16) for i in range(2)]
                wbt = [sb4(f"wbt{i}", [128, 8, 128], BF16) for i in range(2)]
                sga = [sb4(f"sga{i}", [128, 512], F32) for i in range(2)]
                sgb = [sb4(f"sgb{i}", [128, 512], F32) for i in range(2)]
                m1 = [sb4(f"m1_{i}", [128, 512], F32) for i in range(2)]
                m2 = [sb4(f"m2_{i}", [128, 512], F32) for i in range(2)]
                it = 0
                for j in range(16):
                    wb = j % 2
                    S.op("pool", lambda E, wb=wb, j=j: E.dma_start(out=ch(fl(wgt[wb]), 2048), in_=ch(wg_d[j], 2048)),
                         writes=[("wgt", wb)], dsem=f"d_wg{wb}")
                    S.op("pool", lambda E, wb=wb, j=j: E.dma_start(out=fl(wat[wb]), in_=wba_d[j]),
                         writes=[("wat", wb)], dsem=f"d_wa{wb}")
                    S.op("pool", lambda E, wb=wb, j=j: E.dma_start(out=fl(wbt[wb]), in_=wbb_d[j]),
                         writes=[("wbt", wb)], dsem=f"d_wb{wb}")
                    for tg in range(2):
                        pb = 4 * (it % 2)
                        ib = it % 2
                        it += 1
                        tsl = slice(tg * 512, (tg + 1) * 512)
                        for c in range(16):
                            S.op("pe", lambda E, c=c, pb=pb, wb=wb, tsl=tsl: E.matmul(
                                ps[pb][:, :], lhsT=wgt[wb][:, c, 0:128], rhs=hTo[:, c, tsl], start=(c == 0), stop=(c == 15)),
                                reads=[("wgt", wb)] + HT_OWN, writes=[("ps", pb)], inc=(c == 15))
                        for c in range(16):
                            S.op("pe", lambda E, c=c, pb=pb, wb=wb, tsl=tsl: E.matmul(
                                ps[pb + 1][:, :], lhsT=wgt[wb][:, c, 128:256], rhs=hTo[:, c, tsl], start=(c == 0), stop=(c == 15)),
                                reads=[("wgt", wb)] + HT_OWN, writes=[("ps", pb + 1)], inc=(c == 15))
                        for hh in range(8):
                            S.op("pe", lambda E, hh=hh, pb=pb, wb=wb, tg=tg: E.matmul(
                                ps[pb + 2][:, :], lhsT=wat[wb][:, hh, :], rhs=oaT[:, hh, tg * 512:(tg + 1) * 512], start=(hh == 0), stop=(hh == 7)),
                                reads=[("wat", wb)] + OAT, writes=[("ps", pb + 2)], inc=(hh == 7))
                        for hh in range(8):
                            S.op("pe", lambda E, hh=hh, pb=pb, wb=wb, tg=tg: E.matmul(
                                ps[pb + 3][:, :], lhsT=wbt[wb][:, hh, :], rhs=obT[:, hh, tg * 512:(tg + 1) * 512], start=(hh == 0), stop=(hh == 7)),
                                reads=[("wbt", wb)] + OBT, writes=[("ps", pb + 3)], inc=(hh == 7))
                        S.op("act", lambda E, pb=pb, ib=ib: E.activation(out=sga[ib][:], in_=ps[pb][:, :], func=AF.Sigmoid),
                             reads=[("ps", pb)], writes=[("sga", ib)])
                        S.op("act", lambda E, pb=pb, ib=ib: E.activation(out=sgb[ib][:], in_=ps[pb + 1][:, :], func=AF.Sigmoid),
                             reads=[("ps", pb + 1)], writes=[("sgb", ib)])
                        S.op("dve", lambda E, pb=pb, ib=ib: E.tensor_tensor(out=m1[ib][:], in0=ps[pb + 2][:, :], in1=sga[ib][:], op=ALU.mult),
                             reads=[("ps", pb + 2), ("sga", ib)], writes=[("m1", ib)])
                        S.op("dve", lambda E, pb=pb, ib=ib: E.tensor_tensor(out=m2[ib][:], in0=ps[pb + 3][:, :], in1=sgb[ib][:], op=ALU.mult),
                             reads=[("ps", pb + 3), ("sgb", ib)], writes=[("m2", ib)])
                        S.op("dve", lambda E, ib=ib, j=j, tg=tg: E.tensor_tensor(out=hTc[:, j, tg * 512:(tg + 1) * 512], in0=m1[ib][:], in1=m2[ib][:], op=ALU.add),
                             reads=[("m1", ib), ("m2", ib)], writes=[("hT", 4 * tg + k) for k in range(4)])
            S.barrier()

        if stage >= 5:
            with contextlib.ExitStack() as es5:
                def sb5(name, shape, dt):
                    return es5.enter_context(nc.sbuf_tensor(name, list(shape), dt))
                XM = [("xm", t) for t in range(8)]
                for t in range(8):
                    S.op("sp", lambda E, t=t: E.dma_start(out=xm(t)[:, :], in_=x_own[t * 128:(t + 1) * 128, :]),
                         writes=[("xm", t)], dsem=f"d_xm{t}")
                with contextlib.ExitStack() as es5a:
                    wo = [es5a.enter_context(nc.sbuf_tensor(f"wo{i}", [128, 16, 512], BF16)) for i in range(2)]
                    it = 0
                    for n in range(4):
                        wb = n % 2
                        S.op("pool", lambda E, wb=wb, n=n: E.dma_start(out=ch(fl(wo[wb]), 2048), in_=ch(wout_d[n], 2048)),
                             writes=[("wo", wb)], dsem=f"d_wo{wb}")
                        for t in range(8):
                            bank = it % 4
                            it += 1
                            for c in range(16):
                                S.op("pe", lambda E, c=c, bank=bank, t=t, wb=wb: E.matmul(
                                    ps[bank][:, :], lhsT=hTc[:, c, t * 128:(t + 1) * 128], rhs=wo[wb][:, c, :], start=(c == 0), stop=(c == 15)),
                                    reads=[("wo", wb)] + MIX, writes=[("ps", bank)], inc=(c == 15))
                            S.op("dve", lambda E, bank=bank, t=t, n=n: E.tensor_tensor(
                                out=xm(t)[:, n * 512:(n + 1) * 512], in0=ps[bank][:, :], in1=xm(t)[:, n * 512:(n + 1) * 512], op=ALU.add),
                                reads=[("ps", bank), ("xm", t)], writes=[("xm", t)])
                S.barrier()
                if debug:
                    S.op("sp", lambda E: E.dma_start(out=dbg["xmid"][:, 0:8192], in_=xmA.rearrange("p a b -> p (a b)")), reads=XM, dsem="d_dbg4")
                    S.op("sp", lambda E: E.dma_start(out=dbg["xmid"][:, 8192:16384], in_=xmB.rearrange("p a b -> p (a b)")), reads=XM, dsem="d_dbg5")

                if stage >= 6:
                    Wc = sb5("Wc", [128, 8, 32], F32)
                    Ab = sb5("Ab", [128, 8, 32], BF16)
                    Af = sb5("Af", [128, 8, 32], F32)
                    A1 = sb5("A1", [128, 8, 32], F32)
                    A2 = sb5("A2", [128, 8, 32], F32)
                    sidx = sb5("sidx", [128, 8, 2], I32)
                    sidf = sb5("sidf", [128, 8, 2], F32)
                    rankf = sb5("rankf", [128, 8, 32], F32)
                    with contextlib.ExitStack() as es6:
                        def sb6(name, shape, dt):
                            return es6.enter_context(nc.sbuf_tensor(name, list(shape), dt))
                        gbc = sb6("gbc", [128, 2048], F32)
                        hff2 = [sb6(f"hff{i}", [128, 2048], F32) for i in range(2)]
                        hfT2 = [sb6(f"hfT{i}", [128, 16, 128], F32) for i in range(2)]
                        wr = sb6("wr", [128, 16, 36], F32)
                        rbias = sb6("rbias", [128, 36], F32)
                        lg2 = [sb6(f"lg{i}", [128, 36], F32) for i in range(2)]
                        rt2 = [sb6(f"rt{i}", [128, 64], F32) for i in range(2)]
                        elm2 = [sb6(f"elm{i}", [128, 32], F32) for i in range(2)]
                        rt = rt2[0]
                        RB = sb6("RB", [128, 8, 32], F32)
                        RT = sb6("RT", [128, 8, 32], F32)
                        junk2 = [sb6(f"junk{i}", [128, 2048], BF16) for i in range(2)]
                        S.op("sp", lambda E: E.dma_start(out=gbc[:], in_=ffn_d.partition_broadcast(128)), writes=["gbc"], dsem="d_gbc")
                        S.op("sp", lambda E: E.dma_start(out=fl(wr), in_=wr_d), writes=["wr"], dsem="d_wr")
                        S.op("sp", lambda E: E.dma_start(out=rbias[:], in_=rb_d.partition_broadcast(128)), writes=["rbias"], dsem="d_rb")
                        for t in range(8):
                            a1 = A1[:, t, :]
                            a2 = A2[:, t, :]
                            par = t % 2
                            hff, hfT, lg, rt, elm, junk = hff2[par], hfT2[par], lg2[par], rt2[par], elm2[par], junk2[par]
                            ss = small[:, 40 + par:41 + par]
                            S.op("act", lambda E, t=t, hff=hff, hfT=hfT, lg=lg, rt=rt, elm=elm, junk=junk, ss=ss: E.activation(out=junk[:], in_=xm(t)[:, :], func=AF.Square, accum_out=ss),
                                 reads=[("xm", t)], writes=[("junk", par), ("ss6", par)])
                            S.op("act", lambda E, hff=hff, hfT=hfT, lg=lg, rt=rt, elm=elm, junk=junk, ss=ss: E.activation(out=ss, in_=ss, func=AF.Ln, bias=EPS, scale=1.0 / 2048.0), reads=[("ss6", par)], writes=[("ss6", par)])
                            S.op("act", lambda E, hff=hff, hfT=hfT, lg=lg, rt=rt, elm=elm, junk=junk, ss=ss: E.activation(out=ss, in_=ss, func=AF.Exp, scale=-0.5), reads=[("ss6", par)], writes=[("ss6", par)])
                            S.op("dve", lambda E, t=t, hff=hff, hfT=hfT, lg=lg, rt=rt, elm=elm, junk=junk, ss=ss: E.scalar_tensor_tensor(out=hff[:], in0=xm(t)[:, :], scalar=ss, in1=gbc[:], op0=ALU.mult, op1=ALU.mult),
                                 reads=[("xm", t), ("ss6", par), "gbc"], writes=[("hff", par)])
                            S.op("act", lambda E, t=t, hff=hff, hfT=hfT, lg=lg, rt=rt, elm=elm, junk=junk, ss=ss: E.activation(out=hfb[:, t, :], in_=hff[:], func=AF.Copy), reads=[("hff", par)], writes=[("hfb", t)])
                            if stage >= 7 and t < 6:
                                precast(2, after=[("hfb", t)])
                            for q4 in range(4):
                                bank = q4
                                for j in range(4):
                                    c = q4 * 4 + j
                                    S.op("pe", lambda E, bank=bank, j=j, c=c, hff=hff, hfT=hfT, lg=lg, rt=rt, elm=elm, junk=junk, ss=ss: E.transpose(
                                        out=ps[bank][:, j * 128:(j + 1) * 128], in_=hff[:, c * 128:(c + 1) * 128], identity=ident_f),
                                        reads=[("hff", par), "cstf"], writes=[("ps", bank)], inc=(j == 3))
                                S.op("act" if q4 % 2 else "dve",
                                     (lambda E, bank=bank, q4=q4, hff=hff, hfT=hfT, lg=lg, rt=rt, elm=elm, junk=junk, ss=ss: E.activation(out=hfT[:, q4 * 4:(q4 + 1) * 4, :], in_=ps[bank][:, :].rearrange("p (a b) -> p a b", b=128), func=AF.Copy))
                                     if q4 % 2 else
                                     (lambda E, bank=bank, q4=q4, hff=hff, hfT=hfT, lg=lg, rt=rt, elm=elm, junk=junk, ss=ss: E.tensor_copy(out=hfT[:, q4 * 4:(q4 + 1) * 4, :], in_=ps[bank][:, :].rearrange("p (a b) -> p a b", b=128))),
                                     reads=[("ps", bank)], writes=[("hfT", par, q4)])
                            for c in range(16):
                                S.op("pe", lambda E, c=c, hff=hff, hfT=hfT, lg=lg, rt=rt, elm=elm, junk=junk, ss=ss: E.matmul(ps[4][:, 0:36], lhsT=hfT[:, c, :], rhs=wr[:, c, :], start=(c == 0), stop=(c == 15)),
                                     reads=[("hfT", par, c // 4), "wr"], writes=[("ps", 4)], inc=(c == 15))
                            S.op("dve", lambda E, hff=hff, hfT=hfT, lg=lg, rt=rt, elm=elm, junk=junk, ss=ss: E.tensor_tensor(out=lg[:], in0=ps[4][:, 0:36], in1=rbias[:], op=ALU.add),
                                 reads=[("ps", 4), "rbias"], writes=[("lg", par)])
                            S.op("dve", lambda E, hff=hff, hfT=hfT, lg=lg, rt=rt, elm=elm, junk=junk, ss=ss: E.tensor_reduce(out=rt[:, 0:1], in_=lg[:, 0:4], axis=AX.X, op=ALU.max), reads=[("lg", par)], writes=[("rt0", par)])
                            S.op("dve", lambda E, hff=hff, hfT=hfT, lg=lg, rt=rt, elm=elm, junk=junk, ss=ss: E.tensor_scalar(out=rt[:, 4:8], in0=lg[:, 0:4], scalar1=rt[:, 0:1], scalar2=None, op0=ALU.is_equal),
                                 reads=[("lg", par), ("rt0", par)], writes=[("gm", par)])
                            S.op("dve", lambda E, hff=hff, hfT=hfT, lg=lg, rt=rt, elm=elm, junk=junk, ss=ss: E.tensor_scalar(out=rt[:, 1:2], in0=rt[:, 0:1], scalar1=-1.0, scalar2=None, op0=ALU.mult),
                                 reads=[("rt0", par)], writes=[("rt1", par)])
                            S.op("act", lambda E, hff=hff, hfT=hfT, lg=lg, rt=rt, elm=elm, junk=junk, ss=ss: E.activation(out=rt[:, 8:12], in_=lg[:, 0:4], func=AF.Exp, bias=rt[:, 1:2], scale=1.0, accum_out=rt[:, 2:3]),
                                 reads=[("lg", par), ("rt1", par)], writes=[("rt2", par), ("rt8", par)])
                            S.op("dve", lambda E, hff=hff, hfT=hfT, lg=lg, rt=rt, elm=elm, junk=junk, ss=ss: E.reciprocal(out=rt[:, 3:4], in_=rt[:, 2:3]), reads=[("rt2", par)], writes=[("pg", par)])
                            S.op("dve", lambda E, hff=hff, hfT=hfT, lg=lg, rt=rt, elm=elm, junk=junk, ss=ss: E.tensor_scalar(out=rt[:, 4:8], in0=rt[:, 4:8], scalar1=-1.0, scalar2=1e30, op0=ALU.add, op1=ALU.mult),
                                 reads=[("gm", par)], writes=[("gm", par)])
                            S.op("dve", lambda E, hff=hff, hfT=hfT, lg=lg, rt=rt, elm=elm, junk=junk, ss=ss: E.tensor_tensor(out=elm[:].rearrange("p (a b) -> p a b", b=8), in0=lg[:, 4:36].rearrange("p (a b) -> p a b", b=8),
                                                                  in1=rt[:, 4:8].unsqueeze(2).broadcast_to([128, 4, 8]), op=ALU.add),
                                 reads=[("lg", par), ("gm", par)], writes=[("elm", par)])
                            S.op("dve", lambda E, hff=hff, hfT=hfT, lg=lg, rt=rt, elm=elm, junk=junk, ss=ss: E.max(out=rt[:, 16:24], in_=elm[:]), reads=[("elm", par)], writes=[("top8", par)])
                            S.op("dve", lambda E, a1=a1, hff=hff, hfT=hfT, lg=lg, rt=rt, elm=elm, junk=junk, ss=ss: E.tensor_scalar(out=a1, in0=elm[:], scalar1=rt[:, 16:17], scalar2=None, op0=ALU.is_equal),
                                 reads=[("elm", par), ("top8", par)], writes=[("a1", par)])
                            S.op("dve", lambda E, a2=a2, hff=hff, hfT=hfT, lg=lg, rt=rt, elm=elm, junk=junk, ss=ss: E.tensor_scalar(out=a2, in0=elm[:], scalar1=rt[:, 17:18], scalar2=None, op0=ALU.is_equal),
                                 reads=[("elm", par), ("top8", par)], writes=[("a2", par)])
                            S.op("dve", lambda E, hff=hff, hfT=hfT, lg=lg, rt=rt, elm=elm, junk=junk, ss=ss: E.tensor_tensor(out=rt[:, 24:25], in0=rt[:, 16:17], in1=rt[:, 17:18], op=ALU.subtract),
                                 reads=[("top8", par)], writes=[("dd", par)])
                            S.op("act", lambda E, hff=hff, hfT=hfT, lg=lg, rt=rt, elm=elm, junk=junk, ss=ss: E.activation(out=rt[:, 25:26], in_=rt[:, 24:25], func=AF.Sigmoid), reads=[("dd", par)], writes=[("w1", par)])
                            S.op("dve", lambda E, hff=hff, hfT=hfT, lg=lg, rt=rt, elm=elm, junk=junk, ss=ss: E.tensor_tensor(out=rt[:, 26:27], in0=rt[:, 25:26], in1=rt[:, 3:4], op=ALU.mult),
                                 reads=[("w1", par), ("pg", par)], writes=[("cw1", par)])
                            S.op("dve", lambda E, hff=hff, hfT=hfT, lg=lg, rt=rt, elm=elm, junk=junk, ss=ss: E.tensor_tensor(out=rt[:, 27:28], in0=rt[:, 3:4], in1=rt[:, 26:27], op=ALU.subtract),
                                 reads=[("cw1", par), ("pg", par)], writes=[("cw2", par)])
                            S.op("dve", lambda E, t=t, a1=a1, hff=hff, hfT=hfT, lg=lg, rt=rt, elm=elm, junk=junk, ss=ss: E.tensor_scalar(out=Wc[:, t, :], in0=a1, scalar1=rt[:, 26:27], scalar2=None, op0=ALU.mult),
                                 reads=[("a1", par), ("cw1", par)], writes=[("Wc", t)])
                            S.op("dve", lambda E, t=t, a2=a2, hff=hff, hfT=hfT, lg=lg, rt=rt, elm=elm, junk=junk, ss=ss: E.scalar_tensor_tensor(out=Wc[:, t, :], in0=a2, scalar=rt[:, 27:28], in1=Wc[:, t, :], op0=ALU.mult, op1=ALU.add),
                                 reads=[("a2", par), ("cw2", par), ("Wc", t)], writes=[("Wc", t)])
                            S.op("dve", lambda E, t=t, a1=a1, a2=a2, hff=hff, hfT=hfT, lg=lg, rt=rt, elm=elm, junk=junk, ss=ss: E.tensor_tensor(out=Ab[:, t, :], in0=a1, in1=a2, op=ALU.add),
                                 reads=[("a1", par), ("a2", par)], writes=[("Ab", t)])
                            S.op("dve", lambda E, t=t, a1=a1, a2=a2, hff=hff, hfT=hfT, lg=lg, rt=rt, elm=elm, junk=junk, ss=ss: E.tensor_tensor(out=Af[:, t, :], in0=a1, in1=a2, op=ALU.add),
                                 reads=[("a1", par), ("a2", par)], writes=[("Af", t)])
                        for t in range(8):
                            for tp in range(t + 1):
                                S.op("pe", lambda E, t=t, tp=tp: E.matmul(ps[5][:, 0:32], lhsT=(ustrict if tp == t else ones_b), rhs=Ab[:, tp, :],
                                                                          start=(tp == 0), stop=(tp == t)),
                                     reads=[("Ab", tp), "cstb"], writes=[("ps", 5)], inc=(tp == t))
                            S.op("dve", lambda E, t=t: E.tensor_copy(out=rankf[:, t, :], in_=ps[5][:, 0:32]), reads=[("ps", 5)], writes=[("rank", t)])
                        RANKS = [("rank", t) for t in range(8)]
                        S.op("dve", lambda E: E.tensor_tensor(out=RB[:], in0=rankf[:], in1=ebase.unsqueeze(1).broadcast_to([128, 8, 32]), op=ALU.add),
                             reads=RANKS + ["cstf"], writes=["RB"])
                        for k, Ak in ((0, A1), (1, A2)):
                            S.op("dve", lambda E, Ak=Ak: E.tensor_tensor(out=RT[:], in0=RB[:], in1=Ak[:], op=ALU.mult), reads=["RB", "a1", "a2"], writes=["RT"])
                            S.op("dve", lambda E, k=k: E.tensor_reduce(out=sidf[:, :, k], in_=RT[:], axis=AX.X, op=ALU.add), reads=["RT"], writes=[("sidf", k)])
                            S.op("dve", lambda E, Ak=Ak: E.tensor_tensor(out=RT[:], in0=rankf[:], in1=Ak[:], op=ALU.mult), reads=RANKS + ["a1", "a2"], writes=["RT"])
                            S.op("dve", lambda E, k=k: E.tensor_reduce(out=rt[:, 32 + 8 * k:40 + 8 * k], in_=RT[:], axis=AX.X, op=ALU.add), reads=["RT"], writes=[("rk", k)])
                            S.op("dve", lambda E, k=k: E.tensor_scalar(out=rt[:, 32 + 8 * k:40 + 8 * k], in0=rt[:, 32 + 8 * k:40 + 8 * k], scalar1=127.5, scalar2=1e6,
                                                                      op0=ALU.is_gt, op1=ALU.mult), reads=[("rk", k)], writes=[("rk", k)])
                            S.op("dve", lambda E, k=k: E.tensor_tensor(out=sidf[:, :, k], in0=sidf[:, :, k], in1=rt[:, 32 + 8 * k:40 + 8 * k], op=ALU.add),
                                 reads=[("sidf", k), ("rk", k)], writes=[("sidf", k)])
                        S.op("dve", lambda E: E.tensor_scalar(out=sidf[:], in0=sidf[:], scalar1=4096.0, scalar2=None, op0=ALU.min),
                             reads=[("sidf", 0), ("sidf", 1)], writes=["sidf2"])
                        S.op("dve", lambda E: E.tensor_copy(out=sidx[:], in_=sidf[:]), reads=["sidf2"], writes=["sidx"])

                if stage >= 6:
                    S.barrier()
                if stage >= 7:
                    with contextlib.ExitStack() as es7:
                        def sb7(name, shape, dt):
                            return es7.enter_context(nc.sbuf_tensor(name, list(shape), dt))
                        wring = [sb7(f"wring{i}", [128, 16, 512], BF16) for i in range(4)]
                        hTc_flat = hTc[:].rearrange("p a b -> p (a b)")
                        wring.append(hTc_flat[:, 0:8192].rearrange("p (c n) -> p c n", n=512))
                        wring.append(hTc_flat[:, 8192:16384].rearrange("p (c n) -> p c n", n=512))
                        NRING = 6
                        HFB_ALL = [("hfb", t) for t in range(8)]

                        def ring_res(s):
                            return [("wring", s)] + (HFB_ALL if s >= 4 else [])

                        def fl2(t3):
                            return (t3[:] if hasattr(t3, "alloc_name") else t3).rearrange("p c n -> p (c n)")
                        Pd = [sb7("Pd0", [128, 8, 128], BF16)] * 2
                        NXG = 3
                        xg = [sb7(f"xg{i}", [128, 2048], BF16) for i in range(NXG)]
                        ye = [sb7(f"ye{i}", [128, 2048], BF16) for i in range(2)]
                        Rfull = sb7("Rfull", [128, 8, 32, 4], BF16)
                        idx_i = sb7("idx_i", [128, 32], I32)
                        idx_f = sb7("idx_f", [128, 32], F32)
                        wsl = sb7("wsl", [128, 32], F32)
                        hfe = sb7("hfe", [128, 16, 128], BF16)
                        sg = sb7("sg", [128, 512], F32)
                        ae = sb7("ae", [128, 512], BF16)
                        aT = sb7("aT", [128, 4, 128], BF16)
                        precast(96)
                        WCS = [("Wc", t) for t in range(8)]
                        Rc3 = Rc.rearrange("p (t k) -> p t k", k=4)
                        for k in range(3):
                            S.op("dve", lambda E, k=k: E.tensor_copy(out=Rfull[:, :, :, k], in_=Rc3[:, :, k].unsqueeze(2).broadcast_to([128, 8, 32])),
                                 reads=["cstf"], writes=["Rfull"])
                        S.op("dve", lambda E: E.tensor_copy(out=Rfull[:, :, :, 3], in_=Wc[:]), reads=WCS, writes=["Rfull"])
                        S.op("dve", lambda E: E.memset(xg[0][:], 0.0), writes=[("xg", 0)])
                        S.op("sp", lambda E: E.dma_start(out=hf_s[1024:1025, :], in_=xg[0][0:1, :]), reads=[("xg", 0)], writes=["hfz"], dsem="d_hfz")
                        S.op("sp", lambda E: E.dma_start(out=y_s[4096:4097, :], in_=xg[0][0:1, :]), reads=[("xg", 0)], writes=["yz"], dsem="d_yz")
                        S.op("sp", lambda E: E.dma_start(out=hf_s[0:1024, :].rearrange("(t p) f -> p t f", p=128), in_=hfb),
                             reads=[("hfb", t) for t in range(8)], writes=["hfd"], dsem="d_hfd")

                        def prep_a(e):
                            pb_ = 0
                            for t in range(8):
                                S.op("dve", lambda E, t=t: E.tensor_scalar(
                                    out=Pd[pb_][:, t, :], in0=iota_f, scalar1=rankf[:, t, e:e + 1], scalar2=Af[:, t, e:e + 1],
                                    op0=ALU.is_equal, op1=ALU.mult),
                                    reads=[("rank", t), ("Af", t), "cstf"], writes=[("Pd", pb_)])

                        def prep_b(e):
                            pb_ = 0
                            xb_ = e % NXG
                            for t in range(8):
                                S.op("pe", lambda E, t=t: E.matmul(ps[6][:, 0:4], lhsT=Pd[pb_][:, t, :], rhs=Rfull[:, t, e, :], start=(t == 0), stop=(t == 7)),
                                     reads=[("Pd", pb_), "Rfull"], writes=[("ps", 6)], inc=(t == 7))
                            S.op("dve", lambda E: E.tensor_copy(out=small[:, 48:52], in_=ps[6][:, 0:4]),
                                 reads=[("ps", 6)], writes=["ixr"])
                            S.op("dve", lambda E: E.scalar_tensor_tensor(out=idx_f[:, e:e + 1], in0=small[:, 49:50], scalar=128.0, in1=small[:, 48:49],
                                                                         op0=ALU.mult, op1=ALU.add), reads=["ixr"], writes=[("idxf", e)])
                            S.op("dve", lambda E: E.tensor_scalar(out=small[:, 52:53], in0=small[:, 50:51], scalar1=-1024.0, scalar2=1024.0, op0=ALU.mult, op1=ALU.add),
                                 reads=["ixr"], writes=["ixo"])
                            S.op("dve", lambda E: E.tensor_tensor(out=idx_f[:, e:e + 1], in0=idx_f[:, e:e + 1], in1=small[:, 52:53], op=ALU.add),
                                 reads=[("idxf", e), "ixo"], writes=[("idxf", e)])
                            S.op("dve", lambda E: E.tensor_copy(out=idx_i[:, e:e + 1], in_=idx_f[:, e:e + 1]), reads=[("idxf", e)], writes=[("idxi", e)])
                            S.op("dve", lambda E: E.tensor_copy(out=wsl[:, e:e + 1], in_=small[:, 51:52]), reads=["ixr"], writes=[("wsl", e)])
                            S.op("pool", lambda E: E.indirect_dma_start(out=xg[xb_][:], out_offset=None, in_=hf_s,
                                                                        in_offset=bass.IndirectOffsetOnAxis(ap=idx_i[:, e:e + 1], axis=0)),
                                 reads=[("idxi", e), "hfd", "hfz"], writes=[("xg", xb_)], dsem=f"d_xg{xb_}")

                        def wslots(e):
                            sg_, su_, sd_ = (3 * e) % NRING, (3 * e + 1) % NRING, (3 * e + 2) % NRING
                            wdt_ = fl2(wring[sd_]).rearrange("p (f n) -> p f n", n=2048)
                            return sg_, su_, sd_, wring[sg_], wring[su_], wdt_

                        def loads(e):
                            pcr = [("pc", e, "g"), ("pc", e, "u"), ("pc", e, "d")]
                            sg_, su_, sd_, wgt_, wut_, wdt_ = wslots(e)
                            if e % 6 == 5:
                                S.op("pool", lambda E: E.dma_start(out=ch(fl2(wgt_), 2048), in_=ch(weg_d[e], 2048)),
                                     writes=ring_res(sg_), dsem=f"d_wrp{sg_}")
                                S.op("pool", lambda E: E.dma_start(out=ch(fl2(wut_), 2048), in_=ch(weu_d[e], 2048)),
                                     writes=ring_res(su_), dsem=f"d_wrp{su_}")
                                S.op("pool", lambda E: E.dma_start(out=ch(fl2(wring[sd_]), 2048), in_=ch(wed_d[e], 2048)),
                                     writes=ring_res(sd_), dsem=f"d_wrp{sd_}")
                                return
                            S.op("sp", lambda E: E.dma_start(out=fl2(wgt_), in_=weg_s[e]),
                                 reads=pcr, writes=ring_res(sg_), dsem=f"d_wr{sg_}")
                            S.op("sp", lambda E: E.dma_start(out=fl2(wut_), in_=weu_s[e]),
                                 reads=pcr, writes=ring_res(su_), dsem=f"d_wr{su_}")
                            S.op("sp", lambda E: E.dma_start(out=fl2(wring[sd_]), in_=wed_s[e]),
                                 reads=pcr, writes=ring_res(sd_), dsem=f"d_wr{sd_}")

                        def moe_t(e):
                            pb_ = e % NXG
                            for hb in range(2):
                                for j in range(8):
                                    c = hb * 8 + j
                                    S.op("pe", lambda E, hb=hb, j=j, c=c: E.transpose(out=psb(hb)[:, j * 128:(j + 1) * 128], in_=xg[pb_][:, c * 128:(c + 1) * 128], identity=ident_b),
                                         reads=[("xg", pb_), "cstb"], writes=[("ps", hb)], inc=(j == 7))
                                if hb == 0:
                                    S.op("act", lambda E: E.activation(out=hfe[:, 0:8, :], in_=psb(0).rearrange("p (a b) -> p a b", b=128), func=AF.Copy),
                                         reads=[("ps", 0)], writes=[("hfe", 0)])
                                else:
                                    S.op("act", lambda E: E.activation(out=hfe[:, 8:16, :], in_=psb(1).rearrange("p (a b) -> p a b", b=128), func=AF.Copy),
                                         reads=[("ps", 1)], writes=[("hfe", 1)])

                        def moe_gu(e):
                            sg_, su_, sd_, wgt_, wut_, wdt_ = wslots(e)
                            for c in range(16):
                                S.op("pe", lambda E, c=c: E.matmul(ps[2][:, :], lhsT=hfe[:, c, :], rhs=wgt_[:, c, :], start=(c == 0), stop=(c == 15)),
                                     reads=[("hfe", c // 8), ("wring", sg_)], writes=[("ps", 2)], inc=(c == 15))
                            for c in range(16):
                                S.op("pe", lambda E, c=c: E.matmul(ps[3][:, :], lhsT=hfe[:, c, :], rhs=wut_[:, c, :], start=(c == 0), stop=(c == 15)),
                                     reads=[("hfe", c // 8), ("wring", su_)], writes=[("ps", 3)], inc=(c == 15))
                            S.op("act", lambda E: E.activation(out=sg[:], in_=ps[2][:, :], func=AF.Silu), reads=[("ps", 2)], writes=["sg"])
                            S.op("dve", lambda E: E.tensor_tensor(out=ae[:], in0=ps[3][:, :], in1=sg[:], op=ALU.mult), reads=[("ps", 3), "sg"], writes=["ae"])

                        def moe_at(e):
                            for j in range(4):
                                S.op("pe", lambda E, j=j: E.transpose(out=psb(7)[:, j * 128:(j + 1) * 128], in_=ae[:, j * 128:(j + 1) * 128], identity=ident_b),
                                     reads=["ae", "cstb"], writes=[("ps", 7)], inc=(j == 3))
                            S.op("act", lambda E: E.activation(out=aT[:], in_=psb(7)[:, 0:512].rearrange("p (a b) -> p a b", b=128), func=AF.Copy),
                                 reads=[("ps", 7)], writes=["aT"])

                        def moe_dn(e):
                            pb_ = e % 2
                            sg_, su_, sd_, wgt_, wut_, wdt_ = wslots(e)
                            for n in range(4):
                                bank = 4 + (n % 2)
                                for f in range(4):
                                    S.op("pe", lambda E, bank=bank, f=f, n=n: E.matmul(
                                        ps[bank][:, :], lhsT=aT[:, f, :], rhs=wdt_[:, f, n * 512:(n + 1) * 512], start=(f == 0), stop=(f == 3)),
                                        reads=["aT", ("wring", sd_)], writes=[("ps", bank)], inc=(f == 3))
                                if n % 2 == 0:
                                    S.op("act", lambda E, bank=bank, n=n: E.activation(out=ye[pb_][:, n * 512:(n + 1) * 512], in_=ps[bank][:, :], func=AF.Copy, scale=wsl[:, e:e + 1]),
                                         reads=[("ps", bank), ("wsl", e)], writes=[("ye", pb_)])
                                else:
                                    S.op("dve", lambda E, bank=bank, n=n: E.tensor_scalar(out=ye[pb_][:, n * 512:(n + 1) * 512], in0=ps[bank][:, :], scalar1=wsl[:, e:e + 1], scalar2=None, op0=ALU.mult),
                                         reads=[("ps", bank), ("wsl", e)], writes=[("ye", pb_)])
                            S.op("act", lambda E: E.dma_start(out=y_s[e * 128:(e + 1) * 128, :], in_=ye[pb_][:]),
                                 reads=[("ye", pb_)], writes=[("Yd", e)], dsem=f"d_y{pb_}")

                        loads(0)
                        loads(1)
                        for e0 in range(3):
                            prep_a(e0)
                            prep_b(e0)
                        moe_t(0)
                        moe_gu(0)
                        for e in range(NEXP):
                            if e + 1 < NEXP:
                                moe_t(e + 1)
                            if e + 3 < NEXP:
                                prep_a(e + 3)
                            moe_at(e)
                            if e + 1 < NEXP:
                                moe_gu(e + 1)
                            moe_dn(e)
                            if e + 2 < NEXP:
                                loads(e + 2)
                            if e + 3 < NEXP:
                                prep_b(e + 3)

                S.barrier()
                with contextlib.ExitStack() as es8:
                    fbc = es8.enter_context(nc.sbuf_tensor("fbc", [128, 2048], F32))
                    ot = [es8.enter_context(nc.sbuf_tensor(f"ot{i}", [128, 2048], F32)) for i in range(2)]
                    jk = es8.enter_context(nc.sbuf_tensor("jk", [128, 2048], BF16))
                    if stage >= 7:
                        yg = [[es8.enter_context(nc.sbuf_tensor(f"yg{i}_{k}", [128, 2048], BF16)) for k in range(2)] for i in range(2)]
                        S.op("sp", lambda E: E.dma_start(out=fbc[:], in_=fin_d.partition_broadcast(128)), writes=["fbc"], dsem="d_fbc")
                        YD = [("Yd", e) for e in range(NEXP)] + ["yz"]
                    for t in range(8):
                        ob_ = t % 2
                        if stage >= 7:
                            for k in range(2):
                                S.op("pool", lambda E, t=t, k=k, ob_=ob_: E.indirect_dma_start(
                                    out=yg[ob_][k][:], out_offset=None, in_=y_s, in_offset=bass.IndirectOffsetOnAxis(ap=sidx[:, t, k:k + 1], axis=0)),
                                    reads=YD + ["sidx"], writes=[("yg", ob_, k)], dsem=f"d_yg{ob_}{k}")
                                S.op("dve", lambda E, t=t, k=k, ob_=ob_: E.tensor_tensor(out=xm(t)[:, :], in0=xm(t)[:, :], in1=yg[ob_][k][:], op=ALU.add),
                                     reads=[("xm", t), ("yg", ob_, k)], writes=[("xm", t)])
                            ss = small[:, 44 + ob_:45 + ob_]
                            S.op("act", lambda E, t=t, ss=ss: E.activation(out=jk[:], in_=xm(t)[:, :], func=AF.Square, accum_out=ss),
                                 reads=[("xm", t)], writes=["jk", ("ss8", ob_)])
                            S.op("act", lambda E, ss=ss: E.activation(out=ss, in_=ss, func=AF.Ln, bias=EPS, scale=1.0 / 2048.0), reads=[("ss8", ob_)], writes=[("ss8", ob_)])
                            S.op("act", lambda E, ss=ss: E.activation(out=ss, in_=ss, func=AF.Exp, scale=-0.5), reads=[("ss8", ob_)], writes=[("ss8", ob_)])
                            S.op("dve", lambda E, t=t, ob_=ob_, ss=ss: E.scalar_tensor_tensor(out=ot[ob_][:], in0=xm(t)[:, :], scalar=ss, in1=fbc[:], op0=ALU.mult, op1=ALU.mult),
                                 reads=[("xm", t), ("ss8", ob_), "fbc"], writes=[("ot", ob_)])
                        else:
                            S.op("dve", lambda E, t=t, ob_=ob_: E.tensor_copy(out=ot[ob_][:], in_=xm(t)[:, :]), reads=[("xm", t)], writes=[("ot", ob_)])
                        S.op("sp", lambda E, t=t, ob_=ob_: E.dma_start(out=out_d[t * 128:(t + 1) * 128, :], in_=ot[ob_][:]),
                             reads=[("ot", ob_)], dsem=f"d_out{ob_}")
        for name in list(S.cnt.keys()):
            if name.startswith("d_"):
                S.wait_tok("sp", (name, S.cnt[name]))

        with contextlib.ExitStack() as esem:
            for name in S.sem_names:
                S.sems[name] = esem.enter_context(nc.semaphore(name))
            with nc.Block() as block:
                @block.tensor
                def _(E):
                    S.emit("pe", E)

                @block.scalar
                def _(E):
                    S.emit("act", E)

                @block.vector
                def _(E):
                    S.emit("dve", E)

                @block.gpsimd
                def _(E):
                    S.emit("pool", E)

                @block.sync
                def _(E):
                    S.emit("sp", E)
    return nc


GA = [0, 3, 4, 7, 8, 11, 12, 15]
GB = [1, 2, 5, 6, 9, 10, 13, 14]


def _consts():
    p = np.arange(128)
    cf = np.zeros((128, 360), np.float32)
    cf[:, 0:128] = np.arange(128, dtype=np.float32)[None, :]
    cf[:, 128] = (10000.0 ** (-(2.0 * (p % 32)) / 64.0)).astype(np.float32)
    invm = np.zeros(128, np.float32)
    for q in range(64, 96):
        invm[q] = 10000.0 ** (-(2.0 * ((q - 64) % 16)) / 32.0)
    cf[:, 129] = invm
    cf[:, 160:288] = np.eye(128, dtype=np.float32)
    cf[:, 288:320] = (128.0 * np.arange(32, dtype=np.float32))[None, :]
    rc = np.zeros((128, 8, 4), np.float32)
    rc[:, :, 0] = p[:, None]
    rc[:, :, 1] = np.arange(8, dtype=np.float32)[None, :]
    rc[:, :, 2] = 1.0
    cf[:, 320:352] = rc.reshape(128, 32)
    cb = np.zeros((128, 768), np.float32)
    cb[:, 0:128] = np.eye(128)
    R = np.zeros((128, 128), np.float32)
    for m in range(128):
        if (m % 64) < 32:
            R[m + 32, m] = -1.0
        else:
            R[m - 32, m] = 1.0
    cb[:, 128:256] = R
    Rm = np.zeros((128, 128), np.float32)
    for m in range(64, 80):
        Rm[m + 16, m] = -1.0
    for m in range(80, 96):
        Rm[m - 16, m] = 1.0
    cb[:, 256:384] = Rm
    cb[:, 384:512] = (p[:, None] <= p[None, :]).astype(np.float32)
    cb[:, 512:640] = (p[:, None] < p[None, :]).astype(np.float32)
    cb[:, 640:768] = 1.0
    return cf, cb


def make_in_maps(inp, stage=STAGE_ALL, cores=range(8)):
    f32 = np.float32
    x = np.asarray(inp["x"], f32)
    pos = np.asarray(inp["positions"]).astype(np.int32)
    w_in = np.asarray(inp["w_in"], f32)[0]
    cf0, cb = _consts()
    def pm(w):
        K, N = w.shape
        return np.ascontiguousarray(w.reshape(K // 128, 128, N).transpose(1, 0, 2).reshape(128, (K // 128) * N))

    w_da = np.ascontiguousarray(np.stack([
        pm(np.concatenate([w_in[:, h * 128:(h + 1) * 128], w_in[:, 1024 + h * 128:1024 + (h + 1) * 128],
                           w_in[:, 2048 + h * 128:2048 + (h + 1) * 128]], axis=1)) for h in range(8)]))
    w_kr = np.zeros((2048, 128), f32)
    w_kr[:, 64:96] = w_in[:, 3840:3872]
    shared = {
        "cst_b": cb, "w_da": w_da,
        "w_mA": pm(w_in[:, 3072:3328]), "w_mB": pm(w_in[:, 3328:3584]), "w_mC": pm(w_in[:, 3584:3840]), "w_mD": pm(w_kr),
        "w_uq": pm(np.asarray(inp["mla_w_uq"], f32)[0]),
        "w_ukv": pm(np.asarray(inp["mla_w_ukv"], f32)[0]),
        "subln": np.ascontiguousarray(np.asarray(inp["da_subln"], f32)[0]),
        "lam": np.ascontiguousarray(np.asarray(inp["da_lambda"], f32)[0].reshape(256)),
    }
    if stage >= 4:
        ga = w_in[:, 3872:5920]
        gb = w_in[:, 5920:7968]
        shared["w_g"] = np.ascontiguousarray(np.stack([
            pm(np.concatenate([ga[:, j * 128:(j + 1) * 128], gb[:, j * 128:(j + 1) * 128]], axis=1)) for j in range(16)]))
        wba = np.asarray(inp["w_branch_a"], f32)[0]
        wbb = np.asarray(inp["w_branch_b"], f32)[0]
        shared["w_ba"] = np.ascontiguousarray(np.stack([pm(wba[:, j * 128:(j + 1) * 128]) for j in range(16)]))
        shared["w_bb"] = np.ascontiguousarray(np.stack([pm(wbb[:, j * 128:(j + 1) * 128]) for j in range(16)]))
    if stage >= 5:
        wout = np.asarray(inp["w_out"], f32)[0]
        shared["w_out"] = np.ascontiguousarray(np.stack([pm(wout[:, n * 512:(n + 1) * 512]) for n in range(4)]))
    if stage >= 6:
        shared["ffn_norm"] = np.ascontiguousarray(np.asarray(inp["ffn_norm"], f32)[0])
        shared["w_r"] = pm(np.concatenate([np.asarray(inp["w_group"], f32)[0], np.asarray(inp["w_router"], f32)[0]], axis=1))
        shared["r_bias"] = np.ascontiguousarray(np.concatenate([np.asarray(inp["b_group"], f32)[0], np.asarray(inp["b_router"], f32)[0]]))
    if stage >= 7:
        def pm3(w):
            E_, K, N = w.shape
            return np.ascontiguousarray(w.reshape(E_, K // 128, 128, N).transpose(0, 2, 1, 3).reshape(E_, 128, (K // 128) * N))
        shared["w_eg"] = pm3(np.asarray(inp["w_exp_gate"], f32)[0])
        shared["w_eu"] = pm3(np.asarray(inp["w_exp_up"], f32)[0])
        shared["w_ed"] = pm3(np.asarray(inp["w_exp_down"], f32)[0])
        shared["final_norm"] = np.ascontiguousarray(np.asarray(inp["final_norm"], f32))
    an = np.asarray(inp["attn_norm"], f32)[0].reshape(16, 128).T
    qn = np.asarray(inp["mla_q_norm"], f32)[0].reshape(4, 128).T
    kvn = np.asarray(inp["mla_kv_norm"], f32)[0].reshape(2, 128).T
    maps = []
    for c in cores:
        b, hf = divmod(c, 2)
        cf = cf0.copy()
        own = GA if hf == 0 else GB
        ctx = GB if hf == 0 else GA
        cf[:, 352:360] = np.array([1.0 if own[j] > ctx[j] else 0.0 for j in range(8)], np.float32)[None, :]
        cf[:, 132:148] = an
        cf[:, 148:152] = qn
        cf[:, 152:154] = kvn
        m = dict(shared)
        m["cst_f"] = cf
        xb = x[b].reshape(16, 128, 2048)
        pb = pos[b].reshape(16, 128)
        m["x_own"] = np.ascontiguousarray(xb[own].reshape(1024, 2048))
        m["x_ctx"] = np.ascontiguousarray(xb[ctx].reshape(1024, 2048))
        m["pos"] = np.ascontiguousarray(np.concatenate([pb[ctx].reshape(-1), pb[own].reshape(-1)]))
        maps.append(m)
    return maps


def kernel(**inp):
    nc = build_nc()
    maps = make_in_maps(inp)
    res = run_bass_kernel_spmd(nc, maps, core_ids=list(range(8)))
    out = np.zeros((4, 2048, 2048), np.float32)
    for c in range(8):
        b, hf = divmod(c, 2)
        own = GA if hf == 0 else GB
        o = res.results[c]["out"].reshape(8, 128, 2048)
        for j in range(8):
            out[b, own[j] * 128:(own[j] + 1) * 128] = o[j]
    return out
```

```python
import contextlib
import numpy as np
import concourse.bass as bass
import concourse.mybir as mybir
from concourse.bass_utils import run_bass_kernel_spmd

F32 = mybir.dt.float32
BF16 = mybir.dt.bfloat16
I32 = mybir.dt.int32
AF = mybir.ActivationFunctionType
ALU = mybir.AluOpType
AX = mybir.AxisListType

EPS = 1e-6
NEXP = 32
STAGE_ALL = 99


class Sched:
    ENG = ("pe", "act", "dve", "pool", "sp")

    def __init__(self, nc):
        self.nc = nc
        self.q = {e: [] for e in self.ENG}
        self.cnt = {}
        self.seen = {e: {} for e in self.ENG}
        self.lastw = {}
        self.readers = {}
        self.sems = {}
        self.sem_names = []

    def sem(self, name):
        if name not in self.cnt:
            self.cnt[name] = 0
            self.sem_names.append(name)
        return name

    def op(self, eng, fn, reads=(), writes=(), inc=True, dsem=None):
        psr = [r for r in reads if isinstance(r, tuple) and r[0] == "ps"]
        if psr:
            reads = [r for r in reads if r not in psr]
            writes = list(writes) + psr
        deps = {}

        def add(tok):
            if tok is None:
                return
            s, v = tok
            if deps.get(s, 0) < v:
                deps[s] = v

        for r in reads:
            add(self.lastw.get(r))
        for w in writes:
            add(self.lastw.get(w))
            for t in self.readers.get(w, ()):
                add(t)
        for s, v in deps.items():
            if s == "pe" and eng == "pe":
                continue
            if self.seen[eng].get(s, 0) >= v:
                continue
            self.seen[eng][s] = v
            self.q[eng].append(("wait", s, v))
        if dsem is not None:
            self.sem(dsem)
            self.cnt[dsem] += 16
            tok = (dsem, self.cnt[dsem])
            self.q[eng].append(("dma", fn, dsem))
        else:
            self.sem(eng)
            if inc:
                self.cnt[eng] += 1
                tok = (eng, self.cnt[eng])
                self.q[eng].append(("inc", fn, eng))
            else:
                tok = (eng, self.cnt[eng] + 1)
                self.q[eng].append(("noinc", fn, None))
        for r in reads:
            self.readers.setdefault(r, []).append(tok)
        for w in writes:
            self.lastw[w] = tok
            self.readers[w] = []
        return tok

    def barrier(self):
        for e in self.ENG:
            for s in list(self.cnt.keys()):
                if s.startswith("d_pc"):
                    continue
                if s != e and self.cnt[s] > 0:
                    self.wait_tok(e, (s, self.cnt[s]))

    def wait_tok(self, eng, tok):
        s, v = tok
        if self.seen[eng].get(s, 0) >= v:
            return
        self.seen[eng][s] = v
        self.q[eng].append(("wait", s, v))

    def emit(self, eng, E):
        for item in self.q[eng]:
            if item[0] == "wait":
                E.wait_ge(self.sems[item[1]], item[2])
            elif item[0] == "dma":
                item[1](E).then_inc(self.sems[item[2]], 16)
            elif item[0] == "inc":
                item[1](E).then_inc(self.sems[item[2]], 1)
            else:
                item[1](E)


def build_nc(stage=STAGE_ALL, debug=False):
    nc = bass.Bass("TRN2", target_bir_lowering=False)
    S = Sched(nc)

    def din(name, shape, dt=F32):
        return nc.dram_tensor(name, list(shape), dt, kind="ExternalInput").ap()

    def dout(name, shape, dt=F32):
        return nc.dram_tensor(name, list(shape), dt, kind="ExternalOutput").ap()

    x_own = din("x_own", [1024, 2048])
    x_ctx = din("x_ctx", [1024, 2048])
    pos_d = din("pos", [2048], I32)
    cstf_d = din("cst_f", [128, 360])
    cstb_d = din("cst_b", [128, 768])
    wda_d = din("w_da", [8, 128, 6144])
    wmA_d = din("w_mA", [128, 4096])
    wmB_d = din("w_mB", [128, 4096])
    wmC_d = din("w_mC", [128, 4096])
    wmD_d = din("w_mD", [128, 2048])
    wuq_d = din("w_uq", [128, 3072])
    wukv_d = din("w_ukv", [128, 3072])
    subln_d = din("subln", [128])
    lam_d = din("lam", [256])
    if stage >= 4:
        wg_d = din("w_g", [16, 128, 4096])
        wba_d = din("w_ba", [16, 128, 1024])
        wbb_d = din("w_bb", [16, 128, 1024])
    if stage >= 5:
        wout_d = din("w_out", [4, 128, 8192])
    if stage >= 6:
        ffn_d = din("ffn_norm", [2048])
        wr_d = din("w_r", [128, 576])
        rb_d = din("r_bias", [36])
    if stage >= 7:
        weg_d = din("w_eg", [NEXP, 128, 8192])
        weu_d = din("w_eu", [NEXP, 128, 8192])
        wed_d = din("w_ed", [NEXP, 128, 8192])
        fin_d = din("final_norm", [2048])
    out_d = dout("out", [1024, 2048])
    def ch(ap2, b):
        return ap2.rearrange("p (a b) -> p a b", b=b)

    def fl(t3):
        return t3[:].rearrange("p c n -> p (c n)")

    PC = {"i": 0, "list": []}
    if stage >= 7:
        hf_s = nc.dram_tensor("hf_s", [1025, 2048], BF16, kind="Internal").ap()
        y_s = nc.dram_tensor("y_s", [4097, 2048], BF16, kind="Internal").ap()
        weg_s = nc.dram_tensor("weg_s", [NEXP, 128, 8192], BF16, kind="Internal").ap()
        weu_s = nc.dram_tensor("weu_s", [NEXP, 128, 8192], BF16, kind="Internal").ap()
        wed_s = nc.dram_tensor("wed_s", [NEXP, 128, 8192], BF16, kind="Internal").ap()
        for e in range(NEXP):
            if e % 6 == 5:
                continue
            PC["list"].append((e, "g", ch(weg_d[e], 2048), ch(weg_s[e], 2048)))
            PC["list"].append((e, "u", ch(weu_d[e], 2048), ch(weu_s[e], 2048)))
            PC["list"].append((e, "d", ch(wed_d[e], 2048), ch(wed_s[e], 2048)))

    def precast(n, after=()):
        for _ in range(n):
            if PC["i"] >= len(PC["list"]):
                return
            e, kind, src_ap, dst_ap = PC["list"][PC["i"]]
            PC["i"] += 1
            S.op("pool", lambda E, src_ap=src_ap, dst_ap=dst_ap: E.dma_start(out=dst_ap, in_=src_ap),
                 reads=list(after), writes=[("pc", e, kind)], dsem=f"d_pc{e}")
    dbg = {}
    if debug:
        dbg["oaT"] = dout("dbg_oaT", [128, 8 * 1024], BF16)
        dbg["obT"] = dout("dbg_obT", [128, 8 * 1024], BF16)
        dbg["xmid"] = dout("dbg_xmid", [128, 8 * 2048])

    es = contextlib.ExitStack()
    with es:
        def sb(name, shape, dt):
            return es.enter_context(nc.sbuf_tensor(name, list(shape), dt))

        ps = [es.enter_context(nc.psum_tensor(f"ps{i}", [128, 512], F32)) for i in range(8)]

        def psb(i):
            return ps[i][:].bitcast(BF16)

        cstf = sb("cstf", [128, 360], F32)
        cstb = sb("cstb", [128, 768], BF16)
        iota_f = cstf[:, 0:128]
        invf_da = cstf[:, 128:129]
        invf_m = cstf[:, 129:130]
        vis = cstf[:, 352:360]
        gattn = cstf[:, 132:148]
        gq = cstf[:, 148:152]
        gkv = cstf[:, 152:154]
        ident_f = cstf[:, 160:288]
        ebase = cstf[:, 288:320]
        Rc = cstf[:, 320:352]
        ident_b = cstb[:, 0:128]
        R_da = cstb[:, 128:256]
        R_m = cstb[:, 256:384]
        cmask = cstb[:, 384:512]
        ustrict = cstb[:, 512:640]
        ones_b = cstb[:, 640:768]

        hTc = sb("hTc", [128, 16, 1024], BF16)
        hTo = sb("hTo", [128, 16, 1024], BF16)
        bufO = sb("bufO", [128, 16384], BF16)
        oaT = bufO[:, 0:8192].rearrange("p (h t) -> p h t", t=1024)
        obT = bufO[:, 8192:16384].rearrange("p (h t) -> p h t", t=1024)
        xmA = bufO[:].bitcast(F32).rearrange("p (a b) -> p a b", b=2048)
        xmB = hTo[:].rearrange("p a b -> p (a b)").bitcast(F32).rearrange("p (a b) -> p a b", b=2048)
        hfb = hTc[:].rearrange("p a b -> p (a b)").rearrange("p (t f) -> p t f", f=2048)

        def hTx(c, t0, n):
            if t0 < 1024:
                return hTc[:, c, t0:t0 + n]
            return hTo[:, c, t0 - 1024:t0 - 1024 + n]

        def xm(t):
            return (xmA if t < 4 else xmB)[:, t % 4, :]
        small = sb("small", [128, 64], F32)
        lamv = small[:, 0:1]
        neglam = small[:, 1:2]

        S.op("sp", lambda E: E.dma_start(out=cstf[:], in_=cstf_d), writes=["cstf"], dsem="d_cf")
        S.op("pool", lambda E: E.dma_start(out=cstb[:], in_=cstb_d), writes=["cstb"], dsem="d_cb")

        HT_ALL = [("hT", t) for t in range(16)]
        HT_OWN = [("hT", t) for t in range(8, 16)]
        AB = {}
        state = {"sbank": 0, "pt": 0, "rope": 0, "onb": 0}
        NPT = 4

        def alloc_attn_bufs(es_, sfx):
            def a(name, shape, dt):
                return es_.enter_context(nc.sbuf_tensor(name + sfx, list(shape), dt))
            AB["qT"] = a("qT", [128, 1024], BF16)
            AB["kT"] = a("kT", [128, 2048], BF16)
            AB["Vt"] = a("Vt", [128, 16, 130], BF16)
            AB["xq"] = [a(f"xq{i}", [128, 512], BF16) for i in range(2)]
            AB["t1"] = [a(f"t1_{i}", [128, 512], F32) for i in range(2)]
            AB["t2"] = [a(f"t2_{i}", [128, 512], F32) for i in range(2)]
            AB["pt"] = [a(f"pt{i}", [128, 512], BF16) for i in range(NPT)]
            AB["onb"] = [a(f"onb{i}", [128, 128], BF16) for i in range(2)]
            Vt_ = AB["Vt"]
            S.op("dve", lambda E: E.memset(Vt_[:, :, 128:130], 1.0), writes=["Vones"])

        def rope_evac(psrc_bank, nrows, dst_ap, tok0, ctab, stab, Rm, dst_res, tabres):
            i = state["rope"] % 2
            state["rope"] += 1
            xq, t1, t2 = AB["xq"][i], AB["t1"][i], AB["t2"][i]
            S.op("act", lambda E: E.activation(out=xq[0:nrows, :], in_=ps[psrc_bank][0:nrows, :], func=AF.Copy),
                 reads=[("ps", psrc_bank)], writes=[("xq", i)])
            S.op("dve", lambda E: E.tensor_tensor(out=t1[0:nrows, :], in0=ps[psrc_bank][0:nrows, :],
                                                  in1=ctab[0:nrows, tok0:tok0 + 512], op=ALU.mult),
                 reads=[("ps", psrc_bank), tabres], writes=[("t1", i)])

            def part_b():
                S.op("pe", lambda E: E.matmul(ps[3][0:nrows, :], lhsT=Rm[0:nrows, 0:nrows], rhs=xq[0:nrows, :], start=True, stop=True),
                     reads=[("xq", i), "cstb"], writes=[("ps", 3)])
                S.op("dve", lambda E: E.tensor_tensor(out=t2[0:nrows, :], in0=ps[3][0:nrows, :],
                                                      in1=stab[0:nrows, tok0:tok0 + 512], op=ALU.mult),
                     reads=[("ps", 3), tabres], writes=[("t2", i)])
                S.op("dve", lambda E: E.tensor_tensor(out=dst_ap, in0=t1[0:nrows, :], in1=t2[0:nrows, :], op=ALU.add),
                     reads=[("t1", i), ("t2", i)], writes=[dst_res])
            return part_b

        class Pipe:
            def __init__(self):
                self.pend = None

            def push(self, fn):
                if self.pend is not None:
                    self.pend()
                self.pend = fn

            def flush(self):
                if self.pend is not None:
                    self.pend()
                self.pend = None

        pipe = Pipe()

        def attn_unit(K, p0, scale, g, on_done):
            qT, kT, Vt, pt = AB["qT"], AB["kT"], AB["Vt"], AB["pt"]
            qt0 = 4 * g
            ktiles = list(range(4 * g + 4)) + [8 + j for j in range(4 * g + 4)]
            DEPTH = 2
            info = {}

            def emit_s(kt):
                visk = None
                if kt < 8:
                    first_q, diag, bias = max(qt0, kt), False, 0.0
                    if kt >= qt0:
                        visk = vis[:, kt:kt + 1]
                else:
                    j = kt - 8
                    first_q, diag, bias = max(qt0, j), (j >= qt0), 0.0
                ncol = (qt0 + 4 - first_q) * 128
                sbk = state["sbank"] % 3
                state["sbank"] += 1
                S.op("pe", lambda E: E.matmul(
                    ps[sbk][:, 0:ncol], lhsT=kT[p0:p0 + K, kt * 128:(kt + 1) * 128],
                    rhs=qT[p0:p0 + K, first_q * 128:first_q * 128 + ncol], start=True, stop=True),
                    reads=["kT", "qT"], writes=[("ps", sbk)])
                sl = state["pt"] % NPT
                state["pt"] += 1
                S.op("act", lambda E: E.activation(
                    out=pt[sl][:, 0:ncol], in_=ps[sbk][:, 0:ncol], func=AF.Exp, bias=bias, scale=float(scale)),
                    reads=[("ps", sbk), "cstf"], writes=[("pt", sl)])
                if diag:
                    S.op("dve", lambda E: E.tensor_tensor(out=pt[sl][:, 0:128], in0=pt[sl][:, 0:128], in1=cmask, op=ALU.mult),
                         reads=[("pt", sl), "cstb"], writes=[("pt", sl)])
                if visk is not None:
                    S.op("dve", lambda E: E.tensor_scalar(out=pt[sl][:, 0:128], in0=pt[sl][:, 0:128], scalar1=visk, scalar2=None, op0=ALU.mult),
                         reads=[("pt", sl), "cstf"], writes=[("pt", sl)])
                info[kt] = (first_q, sl)

            def emit_pv(kt):
                first_q, sl = info[kt]
                for i in range(first_q, qt0 + 4):
                    ob = 4 + (i - qt0)
                    start = (kt == 0)
                    stop = (kt == 8 + i)
                    last = (i == qt0 + 3)
                    S.op("pe", lambda E, ob=ob, i=i, start=start, stop=stop: E.matmul(
                        ps[ob][:, 0:129], lhsT=pt[sl][:, (i - first_q) * 128:(i - first_q + 1) * 128],
                        rhs=Vt[:, kt, 0:129], start=start, stop=stop),
                        reads=[("pt", sl), "Vt", "Vones"], writes=[("ps", ob)], inc=(stop or last))

            n = len(ktiles)
            for idx in range(n + DEPTH):
                if idx < n:
                    emit_s(ktiles[idx])
                if idx - DEPTH >= 0:
                    emit_pv(ktiles[idx - DEPTH])
            for i in range(4):
                on_done(qt0 + i, 4 + i)

        def v_evac(bank, tq):
            Vt = AB["Vt"]
            S.op("act", lambda E: E.activation(
                out=Vt[:, tq * 4:(tq + 1) * 4, 0:128], in_=ps[bank][:, :].rearrange("p (a b) -> p a b", b=128), func=AF.Copy),
                reads=[("ps", bank)], writes=["Vt"])

        with contextlib.ExitStack() as es1:
            def sb1(name, shape, dt):
                return es1.enter_context(nc.sbuf_tensor(name, list(shape), dt))

            cos_m = sb1("cos_m", [128, 2048], BF16)
            sin_m = sb1("sin_m", [128, 2048], BF16)
            subw = sb1("subw", [128, 128], F32)
            S.op("sp", lambda E: E.dma_start(out=subw[:], in_=subln_d.partition_broadcast(128)),
                 writes=["subw"], dsem="d_sub")
            S.op("dve", lambda E: E.tensor_scalar(out=subw[:], in0=subw[:], scalar1=0.8, scalar2=None, op0=ALU.mult),
                 reads=["subw"], writes=["subw"])

            with contextlib.ExitStack() as esda:
                cos_da = esda.enter_context(nc.sbuf_tensor("cos_da", [128, 2048], BF16))
                sin_da = esda.enter_context(nc.sbuf_tensor("sin_da", [128, 2048], BF16))
                lamt = esda.enter_context(nc.sbuf_tensor("lamt", [128, 256], F32))
                S.op("sp", lambda E: E.dma_start(out=lamt[:], in_=lam_d.partition_broadcast(128)),
                     writes=["lamt"], dsem="d_lam")
                S.op("dve", lambda E: E.tensor_tensor(out=lamt[:, 0:64], in0=lamt[:, 0:64], in1=lamt[:, 64:128], op=ALU.mult),
                     reads=["lamt"], writes=["lamt"])
                S.op("dve", lambda E: E.tensor_tensor(out=lamt[:, 128:192], in0=lamt[:, 128:192], in1=lamt[:, 192:256], op=ALU.mult),
                     reads=["lamt"], writes=["lamt"])
                S.op("dve", lambda E: E.tensor_reduce(out=small[:, 2:3], in_=lamt[:, 0:64], axis=AX.X, op=ALU.add),
                     reads=["lamt"], writes=["sm2"])
                S.op("dve", lambda E: E.tensor_reduce(out=small[:, 3:4], in_=lamt[:, 128:192], axis=AX.X, op=ALU.add),
                     reads=["lamt"], writes=["sm3"])
                S.op("act", lambda E: E.activation(out=small[:, 4:6], in_=small[:, 2:4], func=AF.Exp),
                     reads=["sm2", "sm3"], writes=["sm4"])
                S.op("dve", lambda E: E.tensor_tensor(out=small[:, 6:7], in0=small[:, 4:5], in1=small[:, 5:6], op=ALU.subtract),
                     reads=["sm4"], writes=["sm6"])
                S.op("dve", lambda E: E.tensor_scalar(out=lamv, in0=small[:, 6:7], scalar1=0.2, scalar2=None, op0=ALU.add),
                     reads=["sm6"], writes=["lamv"])
                S.op("dve", lambda E: E.tensor_scalar(out=neglam, in0=lamv, scalar1=-1.0, scalar2=None, op0=ALU.mult),
                     reads=["lamv"], writes=["neglam"])

                with contextlib.ExitStack() as es0:
                    posi = es0.enter_context(nc.sbuf_tensor("posi", [128, 2048], I32))
                    posf = es0.enter_context(nc.sbuf_tensor("posf", [128, 2048], F32))
                    ang = es0.enter_context(nc.sbuf_tensor("ang", [128, 2048], F32))
                    kk = es0.enter_context(nc.sbuf_tensor("kk", [128, 2048], F32))
                    ki = es0.enter_context(nc.sbuf_tensor("ki", [128, 2048], I32))
                    S.op("sp", lambda E: E.dma_start(out=posi[:], in_=pos_d.partition_broadcast(128)),
                         writes=["posi"], dsem="d_pos")
                    S.op("dve", lambda E: E.tensor_copy(out=posf[:], in_=posi[:]), reads=["posi"], writes=["posf"])
                    TWO_PI = 2.0 * np.pi
                    for (invf, ctab, stab, nm) in ((invf_da, cos_da, sin_da, "da"), (invf_m, cos_m, sin_m, "m")):
                        for (shift, tab) in ((0.0, stab), (np.pi / 2.0, ctab)):
                            S.op("dve", lambda E, invf=invf, shift=shift: E.tensor_scalar(
                                out=ang[:], in0=posf[:], scalar1=invf, scalar2=float(shift), op0=ALU.mult, op1=ALU.add),
                                reads=["posf", "cstf"], writes=["ang"])
                            S.op("dve", lambda E: E.tensor_scalar(
                                out=kk[:], in0=ang[:], scalar1=float(1.0 / TWO_PI), scalar2=None, op0=ALU.mult),
                                reads=["ang"], writes=["kk"])
                            S.op("dve", lambda E: E.tensor_copy(out=ki[:], in_=kk[:]), reads=["kk"], writes=["ki"])
                            S.op("dve", lambda E: E.tensor_copy(out=kk[:], in_=ki[:]), reads=["ki"], writes=["kk"])
                            S.op("dve", lambda E: E.scalar_tensor_tensor(
                                out=ang[:], in0=kk[:], scalar=float(-TWO_PI), in1=ang[:], op0=ALU.mult, op1=ALU.add),
                                reads=["kk", "ang"], writes=["ang"])
                            S.op("dve", lambda E: E.tensor_scalar(
                                out=kk[:], in0=ang[:], scalar1=float(np.pi), scalar2=float(TWO_PI), op0=ALU.is_gt, op1=ALU.mult),
                                reads=["ang"], writes=["kk"])
                            S.op("dve", lambda E: E.tensor_tensor(out=ang[:], in0=ang[:], in1=kk[:], op=ALU.subtract),
                                 reads=["ang", "kk"], writes=["ang"])
                            S.op("dve", lambda E: E.tensor_scalar(
                                out=kk[:], in0=ang[:], scalar1=float(-np.pi), scalar2=float(TWO_PI), op0=ALU.is_lt, op1=ALU.mult),
                                reads=["ang"], writes=["kk"])
                            S.op("dve", lambda E: E.tensor_tensor(out=ang[:], in0=ang[:], in1=kk[:], op=ALU.add),
                                 reads=["ang", "kk"], writes=["ang"])
                            S.op("dve", lambda E: E.tensor_scalar(
                                out=ang[:], in0=ang[:], scalar1=3.141592, scalar2=-3.141592, op0=ALU.min, op1=ALU.max),
                                reads=["ang"], writes=["ang"])
                            S.op("act", lambda E, tab=tab: E.activation(out=tab[:], in_=ang[:], func=AF.Sin),
                                 reads=["ang"], writes=["tab_" + nm])

                    xst = [es0.enter_context(nc.sbuf_tensor(f"xst{i}", [128, 2048], F32)) for i in range(2)]
                    xsb = [es0.enter_context(nc.sbuf_tensor(f"xsb{i}", [128, 2048], BF16)) for i in range(2)]
                    for t in range(16):
                        b = t % 2
                        src = x_ctx if t < 8 else x_own
                        r0 = (t % 8) * 128
                        S.op("sp", lambda E, b=b, src=src, r0=r0: E.dma_start(out=xst[b][:], in_=src[r0:r0 + 128, :]),
                             writes=[("xst", b)], dsem=f"d_x{b}")
                        ss = small[:, 8 + b:9 + b]
                        S.op("act", lambda E, b=b, ss=ss: E.activation(out=xsb[b][:], in_=xst[b][:], func=AF.Square, accum_out=ss),
                             reads=[("xst", b)], writes=[("xsb", b), ("ss", b)])
                        S.op("act", lambda E, ss=ss: E.activation(out=ss, in_=ss, func=AF.Ln, bias=EPS, scale=1.0 / 2048.0),
                             reads=[("ss", b)], writes=[("ss", b)])
                        S.op("act", lambda E, ss=ss: E.activation(out=ss, in_=ss, func=AF.Exp, scale=-0.5),
                             reads=[("ss", b)], writes=[("ss", b)])
                        S.op("dve", lambda E, b=b, ss=ss: E.tensor_scalar(out=xsb[b][:], in0=xst[b][:], scalar1=ss, scalar2=None, op0=ALU.mult),
                             reads=[("xst", b), ("ss", b)], writes=[("xsb", b)])
                        for hb in range(2):
                            bank = 2 * b + hb
                            for j in range(8):
                                c = hb * 8 + j
                                S.op("pe", lambda E, bank=bank, j=j, c=c, b=b: E.transpose(
                                    out=psb(bank)[:, j * 128:(j + 1) * 128], in_=xsb[b][:, c * 128:(c + 1) * 128], identity=ident_b),
                                    reads=[("xsb", b), "cstb"], writes=[("ps", bank)], inc=(j == 7))
                            S.op("dve", lambda E, bank=bank, hb=hb, t=t: E.tensor_tensor(
                                out=(hTc if t < 8 else hTo)[:, hb * 8:(hb + 1) * 8, (t % 8) * 128:(t % 8 + 1) * 128],
                                in0=psb(bank).rearrange("p (a b) -> p a b", b=128),
                                in1=gattn[:, hb * 8:(hb + 1) * 8].unsqueeze(2).broadcast_to([128, 8, 128]),
                                op=ALU.mult),
                                reads=[("ps", bank), "cstf"], writes=[("hT", t)])
                S.barrier()

                with contextlib.ExitStack() as es2:
                    alloc_attn_bufs(es2, "_a")
                    qT, kT = AB["qT"], AB["kT"]
                    acc = es2.enter_context(nc.sbuf_tensor("acc", [128, 4, 128], F32))
                    ocomb = es2.enter_context(nc.sbuf_tensor("ocomb", [128, 128], F32))
                    osq = es2.enter_context(nc.sbuf_tensor("osq", [128, 128], F32))
                    wda = [es2.enter_context(nc.sbuf_tensor(f"wda{i}", [128, 16, 384], BF16)) for i in range(2)]
                    onb = AB["onb"]
                    for h in range(8 if stage >= 2 else 0):
                        wb = h % 2
                        S.op("pool", lambda E, wb=wb, h=h: E.dma_start(out=ch(fl(wda[wb]), 2048), in_=ch(wda_d[h], 2048)),
                             writes=[("wda", wb)], dsem=f"d_wda{wb}")
                        precast(6)
                        for tg in range(2):
                            bank = tg % 2
                            for c in range(16):
                                S.op("pe", lambda E, bank=bank, c=c, wb=wb, tg=tg: E.matmul(
                                    ps[bank][:, :], lhsT=wda[wb][:, c, 0:128], rhs=hTo[:, c, tg * 512:(tg + 1) * 512],
                                    start=(c == 0), stop=(c == 15)),
                                    reads=[("wda", wb)] + HT_OWN, writes=[("ps", bank)], inc=(c == 15))
                            pipe.push(rope_evac(bank, 128, qT[:, tg * 512:(tg + 1) * 512], 1024 + tg * 512, cos_da, sin_da, R_da, "qT", "tab_da"))
                        for tg in range(4):
                            bank = tg % 2
                            for c in range(16):
                                S.op("pe", lambda E, bank=bank, c=c, wb=wb, tg=tg: E.matmul(
                                    ps[bank][:, :], lhsT=wda[wb][:, c, 128:256], rhs=hTx(c, tg * 512, 512),
                                    start=(c == 0), stop=(c == 15)),
                                    reads=[("wda", wb)] + HT_ALL, writes=[("ps", bank)], inc=(c == 15))
                            pipe.push(rope_evac(bank, 128, kT[:, tg * 512:(tg + 1) * 512], tg * 512, cos_da, sin_da, R_da, "kT", "tab_da"))
                        pipe.flush()
                        for tq in range(4):
                            bank = tq % 2
                            for ti in range(4):
                                tt = tq * 4 + ti
                                for c in range(16):
                                    S.op("pe", lambda E, bank=bank, ti=ti, tt=tt, c=c, wb=wb: E.matmul(
                                        ps[bank][:, ti * 128:(ti + 1) * 128], lhsT=hTx(c, tt * 128, 128),
                                        rhs=wda[wb][:, c, 256:384], start=(c == 0), stop=(c == 15)),
                                        reads=[("wda", wb)] + HT_ALL, writes=[("ps", bank)], inc=(c == 15))
                            v_evac(bank, tq)
                        for g in range(2):
                            def done0(qtile, ob):
                                i = qtile % 4
                                sm = small[:, 16 + i:17 + i]
                                S.op("dve", lambda E: E.reciprocal(out=sm, in_=ps[ob][:, 128:129]), reads=[("ps", ob)], writes=[("r1", i)])
                                S.op("dve", lambda E: E.tensor_scalar(out=acc[:, i, :], in0=ps[ob][:, 0:128], scalar1=sm, scalar2=None, op0=ALU.mult),
                                     reads=[("ps", ob), ("r1", i)], writes=[("acc", i)])

                            def done1(qtile, ob, h=h):
                                i = qtile % 4
                                sm = small[:, 20 + i:21 + i]
                                sm2 = small[:, 24 + i:25 + i]
                                S.op("dve", lambda E: E.reciprocal(out=sm, in_=ps[ob][:, 128:129]), reads=[("ps", ob)], writes=[("r2", i)])
                                S.op("dve", lambda E: E.tensor_tensor(out=sm, in0=sm, in1=neglam, op=ALU.mult),
                                     reads=[("r2", i), "neglam"], writes=[("r2", i)])
                                S.op("dve", lambda E: E.scalar_tensor_tensor(out=ocomb[:], in0=ps[ob][:, 0:128], scalar=sm, in1=acc[:, i, :],
                                                                             op0=ALU.mult, op1=ALU.add),
                                     reads=[("ps", ob), ("r2", i), ("acc", i)], writes=["ocomb"])
                                S.op("dve", lambda E: E.tensor_tensor(out=osq[:], in0=ocomb[:], in1=ocomb[:], op=ALU.mult),
                                     reads=["ocomb"], writes=["osq"])
                                S.op("dve", lambda E: E.tensor_reduce(out=sm2, in_=osq[:], axis=AX.X, op=ALU.add),
                                     reads=["osq"], writes=[("ssq", i)])
                                S.op("act", lambda E: E.activation(out=sm2, in_=sm2, func=AF.Ln, bias=EPS, scale=1.0 / 128.0),
                                     reads=[("ssq", i)], writes=[("ssq", i)])
                                S.op("act", lambda E: E.activation(out=sm2, in_=sm2, func=AF.Exp, scale=-0.5),
                                     reads=[("ssq", i)], writes=[("ssq", i)])
                                ob_i = state["onb"] % 2
                                state["onb"] += 1
                                S.op("dve", lambda E: E.scalar_tensor_tensor(out=onb[ob_i][:], in0=ocomb[:], scalar=sm2, in1=subw[:],
                                                                             op0=ALU.mult, op1=ALU.mult),
                                     reads=["ocomb", ("ssq", i), "subw"], writes=[("onb", ob_i)])
                                S.op("pe", lambda E: E.transpose(out=psb(3)[:, 0:128], in_=onb[ob_i][:], identity=ident_b),
                                     reads=[("onb", ob_i), "cstb"], writes=[("ps", 3)])
                                S.op("act", lambda E: E.activation(out=oaT[:, h, qtile * 128:(qtile + 1) * 128], in_=psb(3)[:, 0:128], func=AF.Copy),
                                     reads=[("ps", 3)], writes=[("oaT", h)])

                            attn_unit(64, 0, 0.125, g, done0)
                            attn_unit(64, 64, 0.125, g, done1)
                S.barrier()
            S.barrier()

            if stage >= 3:
                with contextlib.ExitStack() as es3:
                    def sb3(name, shape, dt):
                        return es3.enter_context(nc.sbuf_tensor(name, list(shape), dt))
                    alloc_attn_bufs(es3, "_b")
                    qT, kT = AB["qT"], AB["kT"]
                    onb = AB["onb"]
                    cqn = sb3("cqn", [128, 4, 1024], BF16)
                    ckvn = sb3("ckvn", [128, 2, 2048], BF16)
                    krT = sb3("krT", [128, 2048], BF16)
                    wuq = sb3("wuq", [128, 4, 768], BF16)
                    wukv = sb3("wukv", [128, 2, 1536], BF16)
                    cf = sb3("cf", [128, 4, 512], BF16)
                    csq = sb3("csq", [128, 4, 512], BF16)
                    rbc = sb3("rbc", [128, 512], F32)
                    S.op("pool", lambda E: E.dma_start(out=ch(fl(wuq), 1024), in_=ch(wuq_d, 1024)),
                         writes=["wuq"], dsem="d_wuq")
                    S.op("pool", lambda E: E.dma_start(out=ch(fl(wukv), 1024), in_=ch(wukv_d, 1024)),
                         writes=["wukv"], dsem="d_wukv")
                    with contextlib.ExitStack() as es3b:
                        wmA = es3b.enter_context(nc.sbuf_tensor("wmA", [128, 16, 256], BF16))
                        wmB = es3b.enter_context(nc.sbuf_tensor("wmB", [128, 16, 256], BF16))

                        def latent_norm(nchunk, wts, tok0, gvec, dst, dst_res, width):
                            for k in range(nchunk):
                                wt, off, wres = wts[k]
                                bank = k % 2
                                for c in range(16):
                                    S.op("pe", lambda E, wt=wt, off=off, c=c, bank=bank: E.matmul(
                                        ps[bank][:, :], lhsT=wt[:, c, off:off + 128], rhs=hTx(c, tok0, 512),
                                        start=(c == 0), stop=(c == 15)),
                                        reads=[wres] + HT_ALL, writes=[("ps", bank)], inc=(c == 15))
                                S.op("act", lambda E, k=k, bank=bank: E.activation(out=cf[:, k, :], in_=ps[bank][:, :], func=AF.Copy),
                                     reads=[("ps", bank)], writes=[("cf", k)])
                                S.op("act", lambda E, k=k, bank=bank: E.activation(out=csq[:, k, :], in_=ps[bank][:, :], func=AF.Square),
                                     reads=[("ps", bank)], writes=[("csq", k)])
                            for k in range(nchunk):
                                S.op("pe", lambda E, k=k: E.matmul(ps[2][:, :], lhsT=ones_b, rhs=csq[:, k, :], start=(k == 0), stop=(k == nchunk - 1)),
                                     reads=[("csq", k), "cstb"], writes=[("ps", 2)], inc=(k == nchunk - 1))
                            S.op("act", lambda E: E.activation(out=rbc[:], in_=ps[2][:, :], func=AF.Ln, bias=EPS, scale=1.0 / width),
                                 reads=[("ps", 2)], writes=["rbc"])
                            S.op("act", lambda E: E.activation(out=rbc[:], in_=rbc[:], func=AF.Exp, scale=-0.5),
                                 reads=["rbc"], writes=["rbc"])
                            for k in range(nchunk):
                                S.op("dve", lambda E, k=k: E.scalar_tensor_tensor(
                                    out=dst(k), in0=cf[:, k, :], scalar=gvec[:, k:k + 1], in1=rbc[:], op0=ALU.mult, op1=ALU.mult),
                                    reads=[("cf", k), "rbc", "cstf"], writes=[dst_res])

                        S.op("pool", lambda E: E.dma_start(out=ch(fl(wmA), 2048), in_=ch(wmA_d, 2048)),
                             writes=["wmA"], dsem="d_wmA")
                        S.op("pool", lambda E: E.dma_start(out=ch(fl(wmB), 2048), in_=ch(wmB_d, 2048)),
                             writes=["wmB"], dsem="d_wmB")
                        precast(3)
                        cq_w = [(wmA, 0, "wmA"), (wmA, 128, "wmA"), (wmB, 0, "wmB"), (wmB, 128, "wmB")]
                        for tg in range(2):
                            latent_norm(4, cq_w, 1024 + tg * 512, gq, lambda k, tg=tg: cqn[:, k, tg * 512:(tg + 1) * 512], "cqn", 512.0)
                        S.op("pool", lambda E: E.dma_start(out=ch(fl(wmA), 2048), in_=ch(wmC_d, 2048)),
                             writes=["wmA"], dsem="d_wmA")
                        S.op("pool", lambda E: E.dma_start(out=wmB[:, :, 0:128], in_=wmD_d.rearrange("p (c n) -> p c n", n=128)),
                             writes=["wmB"], dsem="d_wmB")
                        ckv_w = [(wmA, 0, "wmA"), (wmA, 128, "wmA")]
                        for tg in range(4):
                            latent_norm(2, ckv_w, tg * 512, gkv, lambda k, tg=tg: ckvn[:, k, tg * 512:(tg + 1) * 512], "ckvn", 256.0)
                        for tg in range(4):
                            bank = tg % 2
                            for c in range(16):
                                S.op("pe", lambda E, c=c, bank=bank, tg=tg: E.matmul(
                                    ps[bank][:, :], lhsT=wmB[:, c, 0:128], rhs=hTx(c, tg * 512, 512),
                                    start=(c == 0), stop=(c == 15)),
                                    reads=["wmB"] + HT_ALL, writes=[("ps", bank)], inc=(c == 15))
                            pipe.push(rope_evac(bank, 128, krT[:, tg * 512:(tg + 1) * 512], tg * 512, cos_m, sin_m, R_m, "krT", "tab_m"))
                        pipe.flush()
                    S.barrier()
                    for h in range(8):
                        if h < 6:
                            precast(3, after=([("obT", h - 1)] if h >= 1 else []))
                        for tg in range(2):
                            bank = tg % 2
                            for c in range(4):
                                S.op("pe", lambda E, c=c, bank=bank, tg=tg, h=h: E.matmul(
                                    ps[bank][0:96, :], lhsT=wuq[:, c, h * 96:(h + 1) * 96], rhs=cqn[:, c, tg * 512:(tg + 1) * 512],
                                    start=(c == 0), stop=(c == 3)),
                                    reads=["wuq", "cqn"], writes=[("ps", bank)], inc=(c == 3))
                            pipe.push(rope_evac(bank, 96, qT[0:96, tg * 512:(tg + 1) * 512], 1024 + tg * 512, cos_m, sin_m, R_m, "qT", "tab_m"))
                        pipe.flush()
                        for tg in range(4):
                            bank = tg % 2
                            for c in range(2):
                                S.op("pe", lambda E, c=c, bank=bank, tg=tg, h=h: E.matmul(
                                    ps[bank][0:64, :], lhsT=wukv[:, c, h * 192:h * 192 + 64], rhs=ckvn[:, c, tg * 512:(tg + 1) * 512],
                                    start=(c == 0), stop=(c == 1)),
                                    reads=["wukv", "ckvn"], writes=[("ps", bank)], inc=(c == 1))
                            S.op("act", lambda E, bank=bank, tg=tg: E.activation(out=kT[0:64, tg * 512:(tg + 1) * 512], in_=ps[bank][0:64, :], func=AF.Copy),
                                 reads=[("ps", bank)], writes=["kT"])
                        S.op("dve", lambda E: E.tensor_copy(out=kT[64:96, :], in_=krT[64:96, :]), reads=["krT"], writes=["kT"])
                        for tq in range(4):
                            bank = tq % 2
                            for ti in range(4):
                                tt = tq * 4 + ti
                                for c in range(2):
                                    S.op("pe", lambda E, bank=bank, ti=ti, tt=tt, c=c, h=h: E.matmul(
                                        ps[bank][:, ti * 128:(ti + 1) * 128], lhsT=ckvn[:, c, tt * 128:(tt + 1) * 128],
                                        rhs=wukv[:, c, h * 192 + 64:h * 192 + 192], start=(c == 0), stop=(c == 1)),
                                        reads=["wukv", "ckvn"], writes=[("ps", bank)], inc=(c == 1))
                            v_evac(bank, tq)
                        for g in range(2):
                            def donem(qtile, ob, h=h):
                                i = qtile % 4
                                sm = small[:, 28 + i:29 + i]
                                S.op("dve", lambda E: E.reciprocal(out=sm, in_=ps[ob][:, 128:129]), reads=[("ps", ob)], writes=[("r3", i)])
                                ob_i = state["onb"] % 2
                                state["onb"] += 1
                                S.op("dve", lambda E: E.tensor_scalar(out=onb[ob_i][:], in0=ps[ob][:, 0:128], scalar1=sm, scalar2=None, op0=ALU.mult),
                                     reads=[("ps", ob), ("r3", i)], writes=[("onb", ob_i)])
                                S.op("pe", lambda E: E.transpose(out=psb(3)[:, 0:128], in_=onb[ob_i][:], identity=ident_b),
                                     reads=[("onb", ob_i), "cstb"], writes=[("ps", 3)])
                                S.op("act", lambda E: E.activation(out=obT[:, h, qtile * 128:(qtile + 1) * 128], in_=psb(3)[:, 0:128], func=AF.Copy),
                                     reads=[("ps", 3)], writes=[("obT", h)])
                            attn_unit(96, 0, float(96.0 ** -0.5), g, donem)
                S.barrier()
        S.barrier()

        OAT = [("oaT", h) for h in range(8)]
        OBT = [("obT", h) for h in range(8)]
        if debug:
            S.op("sp", lambda E: E.dma_start(out=dbg["oaT"], in_=bufO[:, 0:8192]), reads=OAT, dsem="d_dbg2")
            S.op("sp", lambda E: E.dma_start(out=dbg["obT"], in_=bufO[:, 8192:16384]), reads=OBT, dsem="d_dbg3")

        MIX = [("hT", t) for t in range(8)]
        if stage >= 4:
            with contextlib.ExitStack() as es4:
                def sb4(name, shape, dt):
                    return es4.enter_context(nc.sbuf_tensor(name, list(shape), dt))
                wgt = [sb4(f"wgt{i}", [128, 16, 256], BF16) for i in range(2)]
                wat = [sb4(f"wat{i}", [128, 8, 128], BF16) for i in range(2)]
                wbt = [sb4(f"wbt{i}", [128, 8, 128], BF16) for i in range(2)]
                sga = [sb4(f"sga{i}", [128, 512], F32) for i in range(2)]
                sgb = [sb4(f"sgb{i}", [128, 512], F32) for i in range(2)]
                m1 = [sb4(f"m1_{i}", [128, 512], F32) for i in range(2)]
                m2 = [sb4(f"m2_{i}", [128, 512], F32) for i in range(2)]
                it = 0
                for j in range(16):
                    wb = j % 2
                    S.op("pool", lambda E, wb=wb, j=j: E.dma_start(out=ch(fl(wgt[wb]), 2048), in_=ch(wg_d[j], 2048)),
                         writes=[("wgt", wb)], dsem=f"d_wg{wb}")
                    S.op("pool", lambda E, wb=wb, j=j: E.dma_start(out=fl(wat[wb]), in_=wba_d[j]),
                         writes=[("wat", wb)], dsem=f"d_wa{wb}")
                    S.op("pool", lambda E, wb=wb, j=j: E.dma_start(out=fl(wbt[wb]), in_=wbb_d[j]),
                         writes=[("wbt", wb)], dsem=f"d_wb{wb}")
                    for tg in range(2):
                        pb = 4 * (it % 2)
                        ib = it % 2
                        it += 1
                        tsl = slice(tg * 512, (tg + 1) * 512)
                        for c in range(16):
                            S.op("pe", lambda E, c=c, pb=pb, wb=wb, tsl=tsl: E.matmul(
                                ps[pb][:, :], lhsT=wgt[wb][:, c, 0:128], rhs=hTo[:, c, tsl], start=(c == 0), stop=(c == 15)),
                                reads=[("wgt", wb)] + HT_OWN, writes=[("ps", pb)], inc=(c == 15))
                        for c in range(16):
                            S.op("pe", lambda E, c=c, pb=pb, wb=wb, tsl=tsl: E.matmul(
                                ps[pb + 1][:, :], lhsT=wgt[wb][:, c, 128:256], rhs=hTo[:, c, tsl], start=(c == 0), stop=(c == 15)),
                                reads=[("wgt", wb)] + HT_OWN, writes=[("ps", pb + 1)], inc=(c == 15))
                        for hh in range(8):
                            S.op("pe", lambda E, hh=hh, pb=pb, wb=wb, tg=tg: E.matmul(
                                ps[pb + 2][:, :], lhsT=wat[wb][:, hh, :], rhs=oaT[:, hh, tg * 512:(tg + 1) * 512], start=(hh == 0), stop=(hh == 7)),
                                reads=[("wat", wb)] + OAT, writes=[("ps", pb + 2)], inc=(hh == 7))
                        for hh in range(8):
                            S.op("pe", lambda E, hh=hh, pb=pb, wb=wb, tg=tg: E.matmul(
                                ps[pb + 3][:, :], lhsT=wbt[wb][:, hh, :], rhs=obT[:, hh, tg * 512:(tg + 1) * 512], start=(hh == 0), stop=(hh == 7)),
                                reads=[("wbt", wb)] + OBT, writes=[("ps", pb + 3)], inc=(hh == 7))
                        S.op("act", lambda E, pb=pb, ib=ib: E.activation(out=sga[ib][:], in_=ps[pb][:, :], func=AF.Sigmoid),
                             reads=[("ps", pb)], writes=[("sga", ib)])
                        S.op("act", lambda E, pb=pb, ib=ib: E.activation(out=sgb[ib][:], in_=ps[pb + 1][:, :], func=AF.Sigmoid),
                             reads=[("ps", pb + 1)], writes=[("sgb", ib)])
                        S.op("dve", lambda E, pb=pb, ib=ib: E.tensor_tensor(out=m1[ib][:], in0=ps[pb + 2][:, :], in1=sga[ib][:], op=ALU.mult),
                             reads=[("ps", pb + 2), ("sga", ib)], writes=[("m1", ib)])
                        S.op("dve", lambda E, pb=pb, ib=ib: E.tensor_tensor(out=m2[ib][:], in0=ps[pb + 3][:, :], in1=sgb[ib][:], op=ALU.mult),
                             reads=[("ps", pb + 3), ("sgb", ib)], writes=[("m2", ib)])
                        S.op("dve", lambda E, ib=ib, j=j, tg=tg: E.tensor_tensor(out=hTc[:, j, tg * 512:(tg + 1) * 512], in0=m1[ib][:], in1=m2[ib][:], op=ALU.add),
                             reads=[("m1", ib), ("m2", ib)], writes=[("hT", 4 * tg + k) for k in range(4)])
            S.barrier()

        if stage >= 5:
            with contextlib.ExitStack() as es5:
                def sb5(name, shape, dt):
                    return es5.enter_context(nc.sbuf_tensor(name, list(shape), dt))
                XM = [("xm", t) for t in range(8)]
                for t in range(8):
                    S.op("sp", lambda E, t=t: E.dma_start(out=xm(t)[:, :], in_=x_own[t * 128:(t + 1) * 128, :]),
                         writes=[("xm", t)], dsem=f"d_xm{t}")
                with contextlib.ExitStack() as es5a:
                    wo = [es5a.enter_context(nc.sbuf_tensor(f"wo{i}", [128, 16, 512], BF16)) for i in range(2)]
                    it = 0
                    for n in range(4):
                        wb = n % 2
                        S.op("pool", lambda E, wb=wb, n=n: E.dma_start(out=ch(fl(wo[wb]), 2048), in_=ch(wout_d[n], 2048)),
                             writes=[("wo", wb)], dsem=f"d_wo{wb}")
                        for t in range(8):
                            bank = it % 4
                            it += 1
                            for c in range(16):
                                S.op("pe", lambda E, c=c, bank=bank, t=t, wb=wb: E.matmul(
                                    ps[bank][:, :], lhsT=hTc[:, c, t * 128:(t + 1) * 128], rhs=wo[wb][:, c, :], start=(c == 0), stop=(c == 15)),
                                    reads=[("wo", wb)] + MIX, writes=[("ps", bank)], inc=(c == 15))
                            S.op("dve", lambda E, bank=bank, t=t, n=n: E.tensor_tensor(
                                out=xm(t)[:, n * 512:(n + 1) * 512], in0=ps[bank][:, :], in1=xm(t)[:, n * 512:(n + 1) * 512], op=ALU.add),
                                reads=[("ps", bank), ("xm", t)], writes=[("xm", t)])
                S.barrier()
                if debug:
                    S.op("sp", lambda E: E.dma_start(out=dbg["xmid"][:, 0:8192], in_=xmA.rearrange("p a b -> p (a b)")), reads=XM, dsem="d_dbg4")
                    S.op("sp", lambda E: E.dma_start(out=dbg["xmid"][:, 8192:16384], in_=xmB.rearrange("p a b -> p (a b)")), reads=XM, dsem="d_dbg5")

                if stage >= 6:
                    Wc = sb5("Wc", [128, 8, 32], F32)
                    Ab = sb5("Ab", [128, 8, 32], BF16)
                    Af = sb5("Af", [128, 8, 32], F32)
                    A1 = sb5("A1", [128, 8, 32], F32)
                    A2 = sb5("A2", [128, 8, 32], F32)
                    sidx = sb5("sidx", [128, 8, 2], I32)
                    sidf = sb5("sidf", [128, 8, 2], F32)
                    rankf = sb5("rankf", [128, 8, 32], F32)
                    with contextlib.ExitStack() as es6:
                        def sb6(name, shape, dt):
                            return es6.enter_context(nc.sbuf_tensor(name, list(shape), dt))
                        gbc = sb6("gbc", [128, 2048], F32)
                        hff2 = [sb6(f"hff{i}", [128, 2048], F32) for i in range(2)]
                        hfT2 = [sb6(f"hfT{i}", [128, 16, 128], F32) for i in range(2)]
                        wr = sb6("wr", [128, 16, 36], F32)
                        rbias = sb6("rbias", [128, 36], F32)
                        lg2 = [sb6(f"lg{i}", [128, 36], F32) for i in range(2)]
                        rt2 = [sb6(f"rt{i}", [128, 64], F32) for i in range(2)]
                        elm2 = [sb6(f"elm{i}", [128, 32], F32) for i in range(2)]
                        rt = rt2[0]
                        RB = sb6("RB", [128, 8, 32], F32)
                        RT = sb6("RT", [128, 8, 32], F32)
                        junk2 = [sb6(f"junk{i}", [128, 2048], BF16) for i in range(2)]
                        S.op("sp", lambda E: E.dma_start(out=gbc[:], in_=ffn_d.partition_broadcast(128)), writes=["gbc"], dsem="d_gbc")
                        S.op("sp", lambda E: E.dma_start(out=fl(wr), in_=wr_d), writes=["wr"], dsem="d_wr")
                        S.op("sp", lambda E: E.dma_start(out=rbias[:], in_=rb_d.partition_broadcast(128)), writes=["rbias"], dsem="d_rb")
                        for t in range(8):
                            a1 = A1[:, t, :]
                            a2 = A2[:, t, :]
                            par = t % 2
                            hff, hfT, lg, rt, elm, junk = hff2[par], hfT2[par], lg2[par], rt2[par], elm2[par], junk2[par]
                            ss = small[:, 40 + par:41 + par]
                            S.op("act", lambda E, t=t, hff=hff, hfT=hfT, lg=lg, rt=rt, elm=elm, junk=junk, ss=ss: E.activation(out=junk[:], in_=xm(t)[:, :], func=AF.Square, accum_out=ss),
                                 reads=[("xm", t)], writes=[("junk", par), ("ss6", par)])
                            S.op("act", lambda E, hff=hff, hfT=hfT, lg=lg, rt=rt, elm=elm, junk=junk, ss=ss: E.activation(out=ss, in_=ss, func=AF.Ln, bias=EPS, scale=1.0 / 2048.0), reads=[("ss6", par)], writes=[("ss6", par)])
                            S.op("act", lambda E, hff=hff, hfT=hfT, lg=lg, rt=rt, elm=elm, junk=junk, ss=ss: E.activation(out=ss, in_=ss, func=AF.Exp, scale=-0.5), reads=[("ss6", par)], writes=[("ss6", par)])
                            S.op("dve", lambda E, t=t, hff=hff, hfT=hfT, lg=lg, rt=rt, elm=elm, junk=junk, ss=ss: E.scalar_tensor_tensor(out=hff[:], in0=xm(t)[:, :], scalar=ss, in1=gbc[:], op0=ALU.mult, op1=ALU.mult),
                                 reads=[("xm", t), ("ss6", par), "gbc"], writes=[("hff", par)])
                            S.op("act", lambda E, t=t, hff=hff, hfT=hfT, lg=lg, rt=rt, elm=elm, junk=junk, ss=ss: E.activation(out=hfb[:, t, :], in_=hff[:], func=AF.Copy), reads=[("hff", par)], writes=[("hfb", t)])
                            if stage >= 7 and t < 6:
                                precast(2, after=[("hfb", t)])
                            for q4 in range(4):
                                bank = q4
                                for j in range(4):
                                    c = q4 * 4 + j
                                    S.op("pe", lambda E, bank=bank, j=j, c=c, hff=hff, hfT=hfT, lg=lg, rt=rt, elm=elm, junk=junk, ss=ss: E.transpose(
                                        out=ps[bank][:, j * 128:(j + 1) * 128], in_=hff[:, c * 128:(c + 1) * 128], identity=ident_f),
                                        reads=[("hff", par), "cstf"], writes=[("ps", bank)], inc=(j == 3))
                                S.op("act" if q4 % 2 else "dve",
                                     (lambda E, bank=bank, q4=q4, hff=hff, hfT=hfT, lg=lg, rt=rt, elm=elm, junk=junk, ss=ss: E.activation(out=hfT[:, q4 * 4:(q4 + 1) * 4, :], in_=ps[bank][:, :].rearrange("p (a b) -> p a b", b=128), func=AF.Copy))
                                     if q4 % 2 else
                                     (lambda E, bank=bank, q4=q4, hff=hff, hfT=hfT, lg=lg, rt=rt, elm=elm, junk=junk, ss=ss: E.tensor_copy(out=hfT[:, q4 * 4:(q4 + 1) * 4, :], in_=ps[bank][:, :].rearrange("p (a b) -> p a b", b=128))),
                                     reads=[("ps", bank)], writes=[("hfT", par, q4)])
                            for c in range(16):
                                S.op("pe", lambda E, c=c, hff=hff, hfT=hfT, lg=lg, rt=rt, elm=elm, junk=junk, ss=ss: E.matmul(ps[4][:, 0:36], lhsT=hfT[:, c, :], rhs=wr[:, c, :], start=(c == 0), stop=(c == 15)),
                                     reads=[("hfT", par, c // 4), "wr"], writes=[("ps", 4)], inc=(c == 15))
                            S.op("dve", lambda E, hff=hff, hfT=hfT, lg=lg, rt=rt, elm=elm, junk=junk, ss=ss: E.tensor_tensor(out=lg[:], in0=ps[4][:, 0:36], in1=rbias[:], op=ALU.add),
                                 reads=[("ps", 4), "rbias"], writes=[("lg", par)])
                            S.op("dve", lambda E, hff=hff, hfT=hfT, lg=lg, rt=rt, elm=elm, junk=junk, ss=ss: E.tensor_reduce(out=rt[:, 0:1], in_=lg[:, 0:4], axis=AX.X, op=ALU.max), reads=[("lg", par)], writes=[("rt0", par)])
                            S.op("dve", lambda E, hff=hff, hfT=hfT, lg=lg, rt=rt, elm=elm, junk=junk, ss=ss: E.tensor_scalar(out=rt[:, 4:8], in0=lg[:, 0:4], scalar1=rt[:, 0:1], scalar2=None, op0=ALU.is_equal),
                                 reads=[("lg", par), ("rt0", par)], writes=[("gm", par)])
                            S.op("dve", lambda E, hff=hff, hfT=hfT, lg=lg, rt=rt, elm=elm, junk=junk, ss=ss: E.tensor_scalar(out=rt[:, 1:2], in0=rt[:, 0:1], scalar1=-1.0, scalar2=None, op0=ALU.mult),
                                 reads=[("rt0", par)], writes=[("rt1", par)])
                            S.op("act", lambda E, hff=hff, hfT=hfT, lg=lg, rt=rt, elm=elm, junk=junk, ss=ss: E.activation(out=rt[:, 8:12], in_=lg[:, 0:4], func=AF.Exp, bias=rt[:, 1:2], scale=1.0, accum_out=rt[:, 2:3]),
                                 reads=[("lg", par), ("rt1", par)], writes=[("rt2", par), ("rt8", par)])
                            S.op("dve", lambda E, hff=hff, hfT=hfT, lg=lg, rt=rt, elm=elm, junk=junk, ss=ss: E.reciprocal(out=rt[:, 3:4], in_=rt[:, 2:3]), reads=[("rt2", par)], writes=[("pg", par)])
                            S.op("dve", lambda E, hff=hff, hfT=hfT, lg=lg, rt=rt, elm=elm, junk=junk, ss=ss: E.tensor_scalar(out=rt[:, 4:8], in0=rt[:, 4:8], scalar1=-1.0, scalar2=1e30, op0=ALU.add, op1=ALU.mult),
                                 reads=[("gm", par)], writes=[("gm", par)])
                            S.op("dve", lambda E, hff=hff, hfT=hfT, lg=lg, rt=rt, elm=elm, junk=junk, ss=ss: E.tensor_tensor(out=elm[:].rearrange("p (a b) -> p a b", b=8), in0=lg[:, 4:36].rearrange("p (a b) -> p a b", b=8),
                                                                  in1=rt[:, 4:8].unsqueeze(2).broadcast_to([128, 4, 8]), op=ALU.add),
                                 reads=[("lg", par), ("gm", par)], writes=[("elm", par)])
                            S.op("dve", lambda E, hff=hff, hfT=hfT, lg=lg, rt=rt, elm=elm, junk=junk, ss=ss: E.max(out=rt[:, 16:24], in_=elm[:]), reads=[("elm", par)], writes=[("top8", par)])
                            S.op("dve", lambda E, a1=a1, hff=hff, hfT=hfT, lg=lg, rt=rt, elm=elm, junk=junk, ss=ss: E.tensor_scalar(out=a1, in0=elm[:], scalar1=rt[:, 16:17], scalar2=None, op0=ALU.is_equal),
                                 reads=[("elm", par), ("top8", par)], writes=[("a1", par)])
                            S.op("dve", lambda E, a2=a2, hff=hff, hfT=hfT, lg=lg, rt=rt, elm=elm, junk=junk, ss=ss: E.tensor_scalar(out=a2, in0=elm[:], scalar1=rt[:, 17:18], scalar2=None, op0=ALU.is_equal),
                                 reads=[("elm", par), ("top8", par)], writes=[("a2", par)])
                            S.op("dve", lambda E, hff=hff, hfT=hfT, lg=lg, rt=rt, elm=elm, junk=junk, ss=ss: E.tensor_tensor(out=rt[:, 24:25], in0=rt[:, 16:17], in1=rt[:, 17:18], op=ALU.subtract),
                                 reads=[("top8", par)], writes=[("dd", par)])
                            S.op("act", lambda E, hff=hff, hfT=hfT, lg=lg, rt=rt, elm=elm, junk=junk, ss=ss: E.activation(out=rt[:, 25:26], in_=rt[:, 24:25], func=AF.Sigmoid), reads=[("dd", par)], writes=[("w1", par)])
                            S.op("dve", lambda E, hff=hff, hfT=hfT, lg=lg, rt=rt, elm=elm, junk=junk, ss=ss: E.tensor_tensor(out=rt[:, 26:27], in0=rt[:, 25:26], in1=rt[:, 3:4], op=ALU.mult),
                                 reads=[("w1", par), ("pg", par)], writes=[("cw1", par)])
                            S.op("dve", lambda E, hff=hff, hfT=hfT, lg=lg, rt=rt, elm=elm, junk=junk, ss=ss: E.tensor_tensor(out=rt[:, 27:28], in0=rt[:, 3:4], in1=rt[:, 26:27], op=ALU.subtract),
                                 reads=[("cw1", par), ("pg", par)], writes=[("cw2", par)])
                            S.op("dve", lambda E, t=t, a1=a1, hff=hff, hfT=hfT, lg=lg, rt=rt, elm=elm, junk=junk, ss=ss: E.tensor_scalar(out=Wc[:, t, :], in0=a1, scalar1=rt[:, 26:27], scalar2=None, op0=ALU.mult),
                                 reads=[("a1", par), ("cw1", par)], writes=[("Wc", t)])
                            S.op("dve", lambda E, t=t, a2=a2, hff=hff, hfT=hfT, lg=lg, rt=rt, elm=elm, junk=junk, ss=ss: E.scalar_tensor_tensor(out=Wc[:, t, :], in0=a2, scalar=rt[:, 27:28], in1=Wc[:, t, :], op0=ALU.mult, op1=ALU.add),
                                 reads=[("a2", par), ("cw2", par), ("Wc", t)], writes=[("Wc", t)])
                            S.op("dve", lambda E, t=t, a1=a1, a2=a2, hff=hff, hfT=hfT, lg=lg, rt=rt, elm=elm, junk=junk, ss=ss: E.tensor_tensor(out=Ab[:, t, :], in0=a1, in1=a2, op=ALU.add),
                                 reads=[("a1", par), ("a2", par)], writes=[("Ab", t)])
                            S.op("dve", lambda E, t=t, a1=a1, a2=a2, hff=hff, hfT=hfT, lg=lg, rt=rt, elm=elm, junk=junk, ss=ss: E.tensor_tensor(out=Af[:, t, :], in0=a1, in1=a2, op=ALU.add),
                                 reads=[("a1", par), ("a2", par)], writes=[("Af", t)])
                        for t in range(8):
                            for tp in range(t + 1):
                                S.op("pe", lambda E, t=t, tp=tp: E.matmul(ps[5][:, 0:32], lhsT=(ustrict if tp == t else ones_b), rhs=Ab[:, tp, :],
                                                                          start=(tp == 0), stop=(tp == t)),
                                     reads=[("Ab", tp), "cstb"], writes=[("ps", 5)], inc=(tp == t))
                            S.op("dve", lambda E, t=t: E.tensor_copy(out=rankf[:, t, :], in_=ps[5][:, 0:32]), reads=[("ps", 5)], writes=[("rank", t)])
                        RANKS = [("rank", t) for t in range(8)]
                        S.op("dve", lambda E: E.tensor_tensor(out=RB[:], in0=rankf[:], in1=ebase.unsqueeze(1).broadcast_to([128, 8, 32]), op=ALU.add),
                             reads=RANKS + ["cstf"], writes=["RB"])
                        for k, Ak in ((0, A1), (1, A2)):
                            S.op("dve", lambda E, Ak=Ak: E.tensor_tensor(out=RT[:], in0=RB[:], in1=Ak[:], op=ALU.mult), reads=["RB", "a1", "a2"], writes=["RT"])
                            S.op("dve", lambda E, k=k: E.tensor_reduce(out=sidf[:, :, k], in_=RT[:], axis=AX.X, op=ALU.add), reads=["RT"], writes=[("sidf", k)])
                            S.op("dve", lambda E, Ak=Ak: E.tensor_tensor(out=RT[:], in0=rankf[:], in1=Ak[:], op=ALU.mult), reads=RANKS + ["a1", "a2"], writes=["RT"])
                            S.op("dve", lambda E, k=k: E.tensor_reduce(out=rt[:, 32 + 8 * k:40 + 8 * k], in_=RT[:], axis=AX.X, op=ALU.add), reads=["RT"], writes=[("rk", k)])
                            S.op("dve", lambda E, k=k: E.tensor_scalar(out=rt[:, 32 + 8 * k:40 + 8 * k], in0=rt[:, 32 + 8 * k:40 + 8 * k], scalar1=127.5, scalar2=1e6,
                                                                      op0=ALU.is_gt, op1=ALU.mult), reads=[("rk", k)], writes=[("rk", k)])
                            S.op("dve", lambda E, k=k: E.tensor_tensor(out=sidf[:, :, k], in0=sidf[:, :, k], in1=rt[:, 32 + 8 * k:40 + 8 * k], op=ALU.add),
                                 reads=[("sidf", k), ("rk", k)], writes=[("sidf", k)])
                        S.op("dve", lambda E: E.tensor_scalar(out=sidf[:], in0=sidf[:], scalar1=4096.0, scalar2=None, op0=ALU.min),
                             reads=[("sidf", 0), ("sidf", 1)], writes=["sidf2"])
                        S.op("dve", lambda E: E.tensor_copy(out=sidx[:], in_=sidf[:]), reads=["sidf2"], writes=["sidx"])

                if stage >= 6:
                    S.barrier()
                if stage >= 7:
                    with contextlib.ExitStack() as es7:
                        def sb7(name, shape, dt):
                            return es7.enter_context(nc.sbuf_tensor(name, list(shape), dt))
                        wring = [sb7(f"wring{i}", [128, 16, 512], BF16) for i in range(4)]
                        hTc_flat = hTc[:].rearrange("p a b -> p (a b)")
                        wring.append(hTc_flat[:, 0:8192].rearrange("p (c n) -> p c n", n=512))
                        wring.append(hTc_flat[:, 8192:16384].rearrange("p (c n) -> p c n", n=512))
                        NRING = 6
                        HFB_ALL = [("hfb", t) for t in range(8)]

                        def ring_res(s):
                            return [("wring", s)] + (HFB_ALL if s >= 4 else [])

                        def fl2(t3):
                            return (t3[:] if hasattr(t3, "alloc_name") else t3).rearrange("p c n -> p (c n)")
                        Pd = [sb7("Pd0", [128, 8, 128], BF16)] * 2
                        NXG = 3
                        xg = [sb7(f"xg{i}", [128, 2048], BF16) for i in range(NXG)]
                        ye = [sb7(f"ye{i}", [128, 2048], BF16) for i in range(2)]
                        Rfull = sb7("Rfull", [128, 8, 32, 4], BF16)
                        idx_i = sb7("idx_i", [128, 32], I32)
                        idx_f = sb7("idx_f", [128, 32], F32)
                        wsl = sb7("wsl", [128, 32], F32)
                        hfe = sb7("hfe", [128, 16, 128], BF16)
                        sg = sb7("sg", [128, 512], F32)
                        ae = sb7("ae", [128, 512], BF16)
                        aT = sb7("aT", [128, 4, 128], BF16)
                        precast(96)
                        WCS = [("Wc", t) for t in range(8)]
                        Rc3 = Rc.rearrange("p (t k) -> p t k", k=4)
                        for k in range(3):
                            S.op("dve", lambda E, k=k: E.tensor_copy(out=Rfull[:, :, :, k], in_=Rc3[:, :, k].unsqueeze(2).broadcast_to([128, 8, 32])),
                                 reads=["cstf"], writes=["Rfull"])
                        S.op("dve", lambda E: E.tensor_copy(out=Rfull[:, :, :, 3], in_=Wc[:]), reads=WCS, writes=["Rfull"])
                        S.op("dve", lambda E: E.memset(xg[0][:], 0.0), writes=[("xg", 0)])
                        S.op("sp", lambda E: E.dma_start(out=hf_s[1024:1025, :], in_=xg[0][0:1, :]), reads=[("xg", 0)], writes=["hfz"], dsem="d_hfz")
                        S.op("sp", lambda E: E.dma_start(out=y_s[4096:4097, :], in_=xg[0][0:1, :]), reads=[("xg", 0)], writes=["yz"], dsem="d_yz")
                        S.op("sp", lambda E: E.dma_start(out=hf_s[0:1024, :].rearrange("(t p) f -> p t f", p=128), in_=hfb),
                             reads=[("hfb", t) for t in range(8)], writes=["hfd"], dsem="d_hfd")

                        def prep_a(e):
                            pb_ = 0
                            for t in range(8):
                                S.op("dve", lambda E, t=t: E.tensor_scalar(
                                    out=Pd[pb_][:, t, :], in0=iota_f, scalar1=rankf[:, t, e:e + 1], scalar2=Af[:, t, e:e + 1],
                                    op0=ALU.is_equal, op1=ALU.mult),
                                    reads=[("rank", t), ("Af", t), "cstf"], writes=[("Pd", pb_)])

                        def prep_b(e):
                            pb_ = 0
                            xb_ = e % NXG
                            for t in range(8):
                                S.op("pe", lambda E, t=t: E.matmul(ps[6][:, 0:4], lhsT=Pd[pb_][:, t, :], rhs=Rfull[:, t, e, :], start=(t == 0), stop=(t == 7)),
                                     reads=[("Pd", pb_), "Rfull"], writes=[("ps", 6)], inc=(t == 7))
                            S.op("dve", lambda E: E.tensor_copy(out=small[:, 48:52], in_=ps[6][:, 0:4]),
                                 reads=[("ps", 6)], writes=["ixr"])
                            S.op("dve", lambda E: E.scalar_tensor_tensor(out=idx_f[:, e:e + 1], in0=small[:, 49:50], scalar=128.0, in1=small[:, 48:49],
                                                                         op0=ALU.mult, op1=ALU.add), reads=["ixr"], writes=[("idxf", e)])
                            S.op("dve", lambda E: E.tensor_scalar(out=small[:, 52:53], in0=small[:, 50:51], scalar1=-1024.0, scalar2=1024.0, op0=ALU.mult, op1=ALU.add),
                                 reads=["ixr"], writes=["ixo"])
                            S.op("dve", lambda E: E.tensor_tensor(out=idx_f[:, e:e + 1], in0=idx_f[:, e:e + 1], in1=small[:, 52:53], op=ALU.add),
                                 reads=[("idxf", e), "ixo"], writes=[("idxf", e)])
                            S.op("dve", lambda E: E.tensor_copy(out=idx_i[:, e:e + 1], in_=idx_f[:, e:e + 1]), reads=[("idxf", e)], writes=[("idxi", e)])
                            S.op("dve", lambda E: E.tensor_copy(out=wsl[:, e:e + 1], in_=small[:, 51:52]), reads=["ixr"], writes=[("wsl", e)])
                            S.op("pool", lambda E: E.indirect_dma_start(out=xg[xb_][:], out_offset=None, in_=hf_s,
                                                                        in_offset=bass.IndirectOffsetOnAxis(ap=idx_i[:, e:e + 1], axis=0)),
                                 reads=[("idxi", e), "hfd", "hfz"], writes=[("xg", xb_)], dsem=f"d_xg{xb_}")

                        def wslots(e):
                            sg_, su_, sd_ = (3 * e) % NRING, (3 * e + 1) % NRING, (3 * e + 2) % NRING
                            wdt_ = fl2(wring[sd_]).rearrange("p (f n) -> p f n", n=2048)
                            return sg_, su_, sd_, wring[sg_], wring[su_], wdt_

                        def loads(e):
                            pcr = [("pc", e, "g"), ("pc", e, "u"), ("pc", e, "d")]
                            sg_, su_, sd_, wgt_, wut_, wdt_ = wslots(e)
                            if e % 6 == 5:
                                S.op("pool", lambda E: E.dma_start(out=ch(fl2(wgt_), 2048), in_=ch(weg_d[e], 2048)),
                                     writes=ring_res(sg_), dsem=f"d_wrp{sg_}")
                                S.op("pool", lambda E: E.dma_start(out=ch(fl2(wut_), 2048), in_=ch(weu_d[e], 2048)),
                                     writes=ring_res(su_), dsem=f"d_wrp{su_}")
                                S.op("pool", lambda E: E.dma_start(out=ch(fl2(wring[sd_]), 2048), in_=ch(wed_d[e], 2048)),
                                     writes=ring_res(sd_), dsem=f"d_wrp{sd_}")
                                return
                            S.op("sp", lambda E: E.dma_start(out=fl2(wgt_), in_=weg_s[e]),
                                 reads=pcr, writes=ring_res(sg_), dsem=f"d_wr{sg_}")
                            S.op("sp", lambda E: E.dma_start(out=fl2(wut_), in_=weu_s[e]),
                                 reads=pcr, writes=ring_res(su_), dsem=f"d_wr{su_}")
                            S.op("sp", lambda E: E.dma_start(out=fl2(wring[sd_]), in_=wed_s[e]),
                                 reads=pcr, writes=ring_res(sd_), dsem=f"d_wr{sd_}")

                        def moe_t(e):
                            pb_ = e % NXG
                            for hb in range(2):
                                for j in range(8):
                                    c = hb * 8 + j
                                    S.op("pe", lambda E, hb=hb, j=j, c=c: E.transpose(out=psb(hb)[:, j * 128:(j + 1) * 128], in_=xg[pb_][:, c * 128:(c + 1) * 128], identity=ident_b),
                                         reads=[("xg", pb_), "cstb"], writes=[("ps", hb)], inc=(j == 7))
                                if hb == 0:
                                    S.op("act", lambda E: E.activation(out=hfe[:, 0:8, :], in_=psb(0).rearrange("p (a b) -> p a b", b=128), func=AF.Copy),
                                         reads=[("ps", 0)], writes=[("hfe", 0)])
                                else:
                                    S.op("act", lambda E: E.activation(out=hfe[:, 8:16, :], in_=psb(1).rearrange("p (a b) -> p a b", b=128), func=AF.Copy),
                                         reads=[("ps", 1)], writes=[("hfe", 1)])

                        def moe_gu(e):
                            sg_, su_, sd_, wgt_, wut_, wdt_ = wslots(e)
                            for c in range(16):
                                S.op("pe", lambda E, c=c: E.matmul(ps[2][:, :], lhsT=hfe[:, c, :], rhs=wgt_[:, c, :], start=(c == 0), stop=(c == 15)),
                                     reads=[("hfe", c // 8), ("wring", sg_)], writes=[("ps", 2)], inc=(c == 15))
                            for c in range(16):
                                S.op("pe", lambda E, c=c: E.matmul(ps[3][:, :], lhsT=hfe[:, c, :], rhs=wut_[:, c, :], start=(c == 0), stop=(c == 15)),
                                     reads=[("hfe", c // 8), ("wring", su_)], writes=[("ps", 3)], inc=(c == 15))
                            S.op("act", lambda E: E.activation(out=sg[:], in_=ps[2][:, :], func=AF.Silu), reads=[("ps", 2)], writes=["sg"])
                            S.op("dve", lambda E: E.tensor_tensor(out=ae[:], in0=ps[3][:, :], in1=sg[:], op=ALU.mult), reads=[("ps", 3), "sg"], writes=["ae"])

                        def moe_at(e):
                            for j in range(4):
                                S.op("pe", lambda E, j=j: E.transpose(out=psb(7)[:, j * 128:(j + 1) * 128], in_=ae[:, j * 128:(j + 1) * 128], identity=ident_b),
                                     reads=["ae", "cstb"], writes=[("ps", 7)], inc=(j == 3))
                            S.op("act", lambda E: E.activation(out=aT[:], in_=psb(7)[:, 0:512].rearrange("p (a b) -> p a b", b=128), func=AF.Copy),
                                 reads=[("ps", 7)], writes=["aT"])

                        def moe_dn(e):
                            pb_ = e % 2
                            sg_, su_, sd_, wgt_, wut_, wdt_ = wslots(e)
                            for n in range(4):
                                bank = 4 + (n % 2)
                                for f in range(4):
                                    S.op("pe", lambda E, bank=bank, f=f, n=n: E.matmul(
                                        ps[bank][:, :], lhsT=aT[:, f, :], rhs=wdt_[:, f, n * 512:(n + 1) * 512], start=(f == 0), stop=(f == 3)),
                                        reads=["aT", ("wring", sd_)], writes=[("ps", bank)], inc=(f == 3))
                                if n % 2 == 0:
                                    S.op("act", lambda E, bank=bank, n=n: E.activation(out=ye[pb_][:, n * 512:(n + 1) * 512], in_=ps[bank][:, :], func=AF.Copy, scale=wsl[:, e:e + 1]),
                                         reads=[("ps", bank), ("wsl", e)], writes=[("ye", pb_)])
                                else:
                                    S.op("dve", lambda E, bank=bank, n=n: E.tensor_scalar(out=ye[pb_][:, n * 512:(n + 1) * 512], in0=ps[bank][:, :], scalar1=wsl[:, e:e + 1], scalar2=None, op0=ALU.mult),
                                         reads=[("ps", bank), ("wsl", e)], writes=[("ye", pb_)])
                            S.op("act", lambda E: E.dma_start(out=y_s[e * 128:(e + 1) * 128, :], in_=ye[pb_][:]),
                                 reads=[("ye", pb_)], writes=[("Yd", e)], dsem=f"d_y{pb_}")

                        loads(0)
                        loads(1)
                        for e0 in range(3):
                            prep_a(e0)
                            prep_b(e0)
                        moe_t(0)
                        moe_gu(0)
                        for e in range(NEXP):
                            if e + 1 < NEXP:
                                moe_t(e + 1)
                            if e + 3 < NEXP:
                                prep_a(e + 3)
                            moe_at(e)
                            if e + 1 < NEXP:
                                moe_gu(e + 1)
                            moe_dn(e)
                            if e + 2 < NEXP:
                                loads(e + 2)
                            if e + 3 < NEXP:
                                prep_b(e + 3)

                S.barrier()
                with contextlib.ExitStack() as es8:
                    fbc = es8.enter_context(nc.sbuf_tensor("fbc", [128, 2048], F32))
                    ot = [es8.enter_context(nc.sbuf_tensor(f"ot{i}", [128, 2048], F32)) for i in range(2)]
                    jk = es8.enter_context(nc.sbuf_tensor("jk", [128, 2048], BF16))
                    if stage >= 7:
                        yg = [[es8.enter_context(nc.sbuf_tensor(f"yg{i}_{k}", [128, 2048], BF16)) for k in range(2)] for i in range(2)]
                        S.op("sp", lambda E: E.dma_start(out=fbc[:], in_=fin_d.partition_broadcast(128)), writes=["fbc"], dsem="d_fbc")
                        YD = [("Yd", e) for e in range(NEXP)] + ["yz"]
                    for t in range(8):
                        ob_ = t % 2
                        if stage >= 7:
                            for k in range(2):
                                S.op("pool", lambda E, t=t, k=k, ob_=ob_: E.indirect_dma_start(
                                    out=yg[ob_][k][:], out_offset=None, in_=y_s, in_offset=bass.IndirectOffsetOnAxis(ap=sidx[:, t, k:k + 1], axis=0)),
                                    reads=YD + ["sidx"], writes=[("yg", ob_, k)], dsem=f"d_yg{ob_}{k}")
                                S.op("dve", lambda E, t=t, k=k, ob_=ob_: E.tensor_tensor(out=xm(t)[:, :], in0=xm(t)[:, :], in1=yg[ob_][k][:], op=ALU.add),
                                     reads=[("xm", t), ("yg", ob_, k)], writes=[("xm", t)])
                            ss = small[:, 44 + ob_:45 + ob_]
                            S.op("act", lambda E, t=t, ss=ss: E.activation(out=jk[:], in_=xm(t)[:, :], func=AF.Square, accum_out=ss),
                                 reads=[("xm", t)], writes=["jk", ("ss8", ob_)])
                            S.op("act", lambda E, ss=ss: E.activation(out=ss, in_=ss, func=AF.Ln, bias=EPS, scale=1.0 / 2048.0), reads=[("ss8", ob_)], writes=[("ss8", ob_)])
                            S.op("act", lambda E, ss=ss: E.activation(out=ss, in_=ss, func=AF.Exp, scale=-0.5), reads=[("ss8", ob_)], writes=[("ss8", ob_)])
                            S.op("dve", lambda E, t=t, ob_=ob_, ss=ss: E.scalar_tensor_tensor(out=ot[ob_][:], in0=xm(t)[:, :], scalar=ss, in1=fbc[:], op0=ALU.mult, op1=ALU.mult),
                                 reads=[("xm", t), ("ss8", ob_), "fbc"], writes=[("ot", ob_)])
                        else:
                            S.op("dve", lambda E, t=t, ob_=ob_: E.tensor_copy(out=ot[ob_][:], in_=xm(t)[:, :]), reads=[("xm", t)], writes=[("ot", ob_)])
                        S.op("sp", lambda E, t=t, ob_=ob_: E.dma_start(out=out_d[t * 128:(t + 1) * 128, :], in_=ot[ob_][:]),
                             reads=[("ot", ob_)], dsem=f"d_out{ob_}")
        for name in list(S.cnt.keys()):
            if name.startswith("d_"):
                S.wait_tok("sp", (name, S.cnt[name]))

        with contextlib.ExitStack() as esem:
            for name in S.sem_names:
                S.sems[name] = esem.enter_context(nc.semaphore(name))
            with nc.Block() as block:
                @block.tensor
                def _(E):
                    S.emit("pe", E)

                @block.scalar
                def _(E):
                    S.emit("act", E)

                @block.vector
                def _(E):
                    S.emit("dve", E)

                @block.gpsimd
                def _(E):
                    S.emit("pool", E)

                @block.sync
                def _(E):
                    S.emit("sp", E)
    return nc


GA = [0, 3, 4, 7, 8, 11, 12, 15]
GB = [1, 2, 5, 6, 9, 10, 13, 14]


def _consts():
    p = np.arange(128)
    cf = np.zeros((128, 360), np.float32)
    cf[:, 0:128] = np.arange(128, dtype=np.float32)[None, :]
    cf[:, 128] = (10000.0 ** (-(2.0 * (p % 32)) / 64.0)).astype(np.float32)
    invm = np.zeros(128, np.float32)
    for q in range(64, 96):
        invm[q] = 10000.0 ** (-(2.0 * ((q - 64) % 16)) / 32.0)
    cf[:, 129] = invm
    cf[:, 160:288] = np.eye(128, dtype=np.float32)
    cf[:, 288:320] = (128.0 * np.arange(32, dtype=np.float32))[None, :]
    rc = np.zeros((128, 8, 4), np.float32)
    rc[:, :, 0] = p[:, None]
    rc[:, :, 1] = np.arange(8, dtype=np.float32)[None, :]
    rc[:, :, 2] = 1.0
    cf[:, 320:352] = rc.reshape(128, 32)
    cb = np.zeros((128, 768), np.float32)
    cb[:, 0:128] = np.eye(128)
    R = np.zeros((128, 128), np.float32)
    for m in range(128):
        if (m % 64) < 32:
            R[m + 32, m] = -1.0
        else:
            R[m - 32, m] = 1.0
    cb[:, 128:256] = R
    Rm = np.zeros((128, 128), np.float32)
    for m in range(64, 80):
        Rm[m + 16, m] = -1.0
    for m in range(80, 96):
        Rm[m - 16, m] = 1.0
    cb[:, 256:384] = Rm
    cb[:, 384:512] = (p[:, None] <= p[None, :]).astype(np.float32)
    cb[:, 512:640] = (p[:, None] < p[None, :]).astype(np.float32)
    cb[:, 640:768] = 1.0
    return cf, cb


def make_in_maps(inp, stage=STAGE_ALL, cores=range(8)):
    f32 = np.float32
    x = np.asarray(inp["x"], f32)
    pos = np.asarray(inp["positions"]).astype(np.int32)
    w_in = np.asarray(inp["w_in"], f32)[0]
    cf0, cb = _consts()
    def pm(w):
        K, N = w.shape
        return np.ascontiguousarray(w.reshape(K // 128, 128, N).transpose(1, 0, 2).reshape(128, (K // 128) * N))

    w_da = np.ascontiguousarray(np.stack([
        pm(np.concatenate([w_in[:, h * 128:(h + 1) * 128], w_in[:, 1024 + h * 128:1024 + (h + 1) * 128],
                           w_in[:, 2048 + h * 128:2048 + (h + 1) * 128]], axis=1)) for h in range(8)]))
    w_kr = np.zeros((2048, 128), f32)
    w_kr[:, 64:96] = w_in[:, 3840:3872]
    shared = {
        "cst_b": cb, "w_da": w_da,
        "w_mA": pm(w_in[:, 3072:3328]), "w_mB": pm(w_in[:, 3328:3584]), "w_mC": pm(w_in[:, 3584:3840]), "w_mD": pm(w_kr),
        "w_uq": pm(np.asarray(inp["mla_w_uq"], f32)[0]),
        "w_ukv": pm(np.asarray(inp["mla_w_ukv"], f32)[0]),
        "subln": np.ascontiguousarray(np.asarray(inp["da_subln"], f32)[0]),
        "lam": np.ascontiguousarray(np.asarray(inp["da_lambda"], f32)[0].reshape(256)),
    }
    if stage >= 4:
        ga = w_in[:, 3872:5920]
        gb = w_in[:, 5920:7968]
        shared["w_g"] = np.ascontiguousarray(np.stack([
            pm(np.concatenate([ga[:, j * 128:(j + 1) * 128], gb[:, j * 128:(j + 1) * 128]], axis=1)) for j in range(16)]))
        wba = np.asarray(inp["w_branch_a"], f32)[0]
        wbb = np.asarray(inp["w_branch_b"], f32)[0]
        shared["w_ba"] = np.ascontiguousarray(np.stack([pm(wba[:, j * 128:(j + 1) * 128]) for j in range(16)]))
        shared["w_bb"] = np.ascontiguousarray(np.stack([pm(wbb[:, j * 128:(j + 1) * 128]) for j in range(16)]))
    if stage >= 5:
        wout = np.asarray(inp["w_out"], f32)[0]
        shared["w_out"] = np.ascontiguousarray(np.stack([pm(wout[:, n * 512:(n + 1) * 512]) for n in range(4)]))
    if stage >= 6:
        shared["ffn_norm"] = np.ascontiguousarray(np.asarray(inp["ffn_norm"], f32)[0])
        shared["w_r"] = pm(np.concatenate([np.asarray(inp["w_group"], f32)[0], np.asarray(inp["w_router"], f32)[0]], axis=1))
        shared["r_bias"] = np.ascontiguousarray(np.concatenate([np.asarray(inp["b_group"], f32)[0], np.asarray(inp["b_router"], f32)[0]]))
    if stage >= 7:
        def pm3(w):
            E_, K, N = w.shape
            return np.ascontiguousarray(w.reshape(E_, K // 128, 128, N).transpose(0, 2, 1, 3).reshape(E_, 128, (K // 128) * N))
        shared["w_eg"] = pm3(np.asarray(inp["w_exp_gate"], f32)[0])
        shared["w_eu"] = pm3(np.asarray(inp["w_exp_up"], f32)[0])
        shared["w_ed"] = pm3(np.asarray(inp["w_exp_down"], f32)[0])
        shared["final_norm"] = np.ascontiguousarray(np.asarray(inp["final_norm"], f32))
    an = np.asarray(inp["attn_norm"], f32)[0].reshape(16, 128).T
    qn = np.asarray(inp["mla_q_norm"], f32)[0].reshape(4, 128).T
    kvn = np.asarray(inp["mla_kv_norm"], f32)[0].reshape(2, 128).T
    maps = []
    for c in cores:
        b, hf = divmod(c, 2)
        cf = cf0.copy()
        own = GA if hf == 0 else GB
        ctx = GB if hf == 0 else GA
        cf[:, 352:360] = np.array([1.0 if own[j] > ctx[j] else 0.0 for j in range(8)], np.float32)[None, :]
        cf[:, 132:148] = an
        cf[:, 148:152] = qn
        cf[:, 152:154] = kvn
        m = dict(shared)
        m["cst_f"] = cf
        xb = x[b].reshape(16, 128, 2048)
        pb = pos[b].reshape(16, 128)
        m["x_own"] = np.ascontiguousarray(xb[own].reshape(1024, 2048))
        m["x_ctx"] = np.ascontiguousarray(xb[ctx].reshape(1024, 2048))
        m["pos"] = np.ascontiguousarray(np.concatenate([pb[ctx].reshape(-1), pb[own].reshape(-1)]))
        maps.append(m)
    return maps


def kernel(**inp):
    nc = build_nc()
    maps = make_in_maps(inp)
    res = run_bass_kernel_spmd(nc, maps, core_ids=list(range(8)))
    out = np.zeros((4, 2048, 2048), np.float32)
    for c in range(8):
        b, hf = divmod(c, 2)
        own = GA if hf == 0 else GB
        o = res.results[c]["out"].reshape(8, 128, 2048)
        for j in range(8):
            out[b, own[j] * 128:(own[j] + 1) * 128] = o[j]
    return out
```
